# Optimizing a Trainium2 kernel written in Bass

```python
import math
import jax, jax.numpy as jnp
from jax import lax
import numpy as np

D_MODEL = 2048
BATCH = 2
SEQ = 16384
DEPTH = 2

D_MIX = D_MODEL
POOL_WIDTH = D_MIX // 4
POOL_WINDOWS = (2, 4, 8, 16)
POOL_GROUPS = len(POOL_WINDOWS)
POOL_GROUP_DIM = POOL_WIDTH // POOL_GROUPS
SSM_WIDTH = D_MIX // 4
SSM_GROUP = 16
SSM_N_GROUPS = SSM_WIDTH // SSM_GROUP
SSM_STATE = 64
DT_MIN = 0.001
DT_MAX = 0.1
MLA_HEADS = 8
QK_NOPE = 128
QK_ROPE = 64
QK_HEAD = QK_NOPE + QK_ROPE
V_HEAD = 128
MLA_WIDTH = MLA_HEADS * V_HEAD
Q_LORA = 512
KV_LORA = 256
ROPE_THETA = 10000.0
Q_BLOCK = 128
IN_COLS = POOL_WIDTH + SSM_WIDTH + Q_LORA + KV_LORA + QK_ROPE
MOE_GROUPS = 4
EXPERTS_PER_GROUP = 8
N_EXPERTS = MOE_GROUPS * EXPERTS_PER_GROUP
TOP_K = 2
D_EXPERT = 512
MOE_BLOCK = 128
EPS = 1e-6

kernel_name = 'hybrid_pool_s5_mla_hmoe'


def rms_norm(x, g):
    xf = x.astype(jnp.float32)
    y = xf * lax.rsqrt(jnp.mean(xf * xf, axis=-1, keepdims=True) + EPS)
    return (y * g.astype(jnp.float32)).astype(x.dtype)


def apply_rope(x, cos, sin):
    xf = x.astype(jnp.float32)
    x1, x2 = jnp.split(xf, 2, axis=-1)
    return jnp.concatenate([x1 * cos - x2 * sin, x2 * cos + x1 * sin], axis=-1).astype(x.dtype)


def pool_mixer(u, w_lin, scale):
    B_, L, _ = u.shape
    uf = u.astype(jnp.float32).reshape(B_, L, POOL_GROUPS, POOL_GROUP_DIM)
    cs = jnp.cumsum(uf, axis=1)
    t = jnp.arange(L)
    outs = []
    for gi, w in enumerate(POOL_WINDOWS):
        c_g = cs[:, :, gi]
        lag = jnp.pad(c_g, ((0, 0), (w, 0), (0, 0)))[:, :L]
        cnt = jnp.minimum(t + 1, w).astype(jnp.float32)[None, :, None]
        outs.append((c_g - lag) / cnt - uf[:, :, gi])
    d = jnp.stack(outs, axis=2).astype(u.dtype)
    y = jnp.einsum('blgc,gcd->blgd', d, w_lin)
    return y.reshape(B_, L, POOL_WIDTH) * scale


def _complex_scan_op(e1, e2):
    a1r, a1i, b1r, b1i = e1
    a2r, a2i, b2r, b2i = e2
    return (a2r * a1r - a2i * a1i,
            a2r * a1i + a2i * a1r,
            a2r * b1r - a2i * b1i + b2r,
            a2r * b1i + a2i * b1r + b2i)


def s5_mixer(u, lam_re, lam_im, log_dt, b_re, b_im, c_re, c_im, d_skip, w_glu):
    B_, L, _ = u.shape
    f32 = jnp.float32
    uf = u.astype(f32).reshape(B_, L, SSM_N_GROUPS, SSM_GROUP)
    dt = jnp.exp(log_dt.astype(f32))[:, None]
    lr, li = lam_re.astype(f32), lam_im.astype(f32)
    mag = jnp.exp(lr * dt)
    ab_re, ab_im = mag * jnp.cos(li * dt), mag * jnp.sin(li * dt)
    den = lr * lr + li * li
    f_re = ((ab_re - 1.0) * lr + ab_im * li) / den
    f_im = (ab_im * lr - (ab_re - 1.0) * li) / den
    br, bi = b_re.astype(f32), b_im.astype(f32)
    bb_re = f_re[..., None] * br - f_im[..., None] * bi
    bb_im = f_re[..., None] * bi + f_im[..., None] * br
    bu_re = jnp.einsum('blgh,gph->blgp', uf, bb_re)
    bu_im = jnp.einsum('blgh,gph->blgp', uf, bb_im)
    a_re = jnp.broadcast_to(ab_re, bu_re.shape)
    a_im = jnp.broadcast_to(ab_im, bu_im.shape)
    _, _, s_re, s_im = lax.associative_scan(_complex_scan_op, (a_re, a_im, bu_re, bu_im), axis=1)
    y = (jnp.einsum('blgp,ghp->blgh', s_re, c_re.astype(f32))
         - jnp.einsum('blgp,ghp->blgh', s_im, c_im.astype(f32)))
    y = y.reshape(B_, L, SSM_WIDTH) + d_skip.astype(f32) * uf.reshape(B_, L, SSM_WIDTH)
    y = jax.nn.gelu(y).astype(u.dtype)
    return y * jax.nn.sigmoid(y @ w_glu)


def mla_mixer(q_c, kv_c, k_pe_in, q_norm_g, kv_norm_g, w_uq, w_ukv, cos, sin):
    B_, L, _ = q_c.shape
    q = jnp.einsum('blr,rhd->blhd', rms_norm(q_c, q_norm_g), w_uq)
    q_nope = q[..., :QK_NOPE]
    q_pe = apply_rope(q[..., QK_NOPE:], cos[:, :, None, :], sin[:, :, None, :])
    kv = jnp.einsum('blr,rhd->blhd', rms_norm(kv_c, kv_norm_g), w_ukv)
    k_nope, v = kv[..., :QK_NOPE], kv[..., QK_NOPE:]
    k_pe = apply_rope(k_pe_in, cos, sin)
    scale = QK_HEAD ** -0.5
    n_blocks = L // Q_BLOCK
    key_idx = jnp.arange(L)

    def attend_block(i):
        s = i * Q_BLOCK
        qn = lax.dynamic_slice_in_dim(q_nope, s, Q_BLOCK, axis=1)
        qp = lax.dynamic_slice_in_dim(q_pe, s, Q_BLOCK, axis=1)
        sc = (jnp.einsum('bqhd,bkhd->bhqk', qn, k_nope)
              + jnp.einsum('bqhd,bkd->bhqk', qp, k_pe)).astype(jnp.float32) * scale
        causal = key_idx[None, :] <= (s + jnp.arange(Q_BLOCK))[:, None]
        sc = jnp.where(causal[None, None], sc, -jnp.inf)
        p = jax.nn.softmax(sc, axis=-1).astype(v.dtype)
        return jnp.einsum('bhqk,bkhd->bqhd', p, v)

    o = lax.map(attend_block, jnp.arange(n_blocks))
    return o.transpose(1, 0, 2, 3, 4).reshape(B_, L, MLA_WIDTH)


def hier_moe(h, w_rg, b_rg, w_re, b_re, w1, w3, w2):
    B_, L, D = h.shape
    N = B_ * L
    hf = h.reshape(N, D)
    g_logits = (hf @ w_rg).astype(jnp.float32) + b_rg
    g_prob = jax.nn.softmax(g_logits, axis=-1)
    g_top = jnp.argmax(g_logits, axis=-1)
    g_w = jnp.take_along_axis(g_prob, g_top[:, None], axis=-1)
    e_logits = ((hf @ w_re).astype(jnp.float32) + b_re).reshape(N, MOE_GROUPS, EXPERTS_PER_GROUP)
    e_logits = jnp.take_along_axis(e_logits, g_top[:, None, None], axis=1)[:, 0]
    e_prob = jax.nn.softmax(e_logits, axis=-1)
    top_p, top_i = lax.top_k(e_prob, TOP_K)
    top_p = top_p / jnp.sum(top_p, axis=-1, keepdims=True)
    weights = (g_w * top_p).reshape(-1)
    expert = (g_top[:, None] * EXPERTS_PER_GROUP + top_i).reshape(-1).astype(jnp.int32)
    token = jnp.repeat(jnp.arange(N, dtype=jnp.int32), TOP_K)
    A = N * TOP_K
    order = jnp.argsort(expert)
    e_s, tok_s, w_s = expert[order], token[order], weights[order]
    counts = jnp.bincount(expert, length=N_EXPERTS)
    padded = (counts + MOE_BLOCK - 1) // MOE_BLOCK * MOE_BLOCK
    pad_end = jnp.cumsum(padded)
    pad_start = pad_end - padded
    start = jnp.cumsum(counts) - counts
    dest = pad_start[e_s] + (jnp.arange(A, dtype=jnp.int32) - start[e_s])
    P = (-(-A // MOE_BLOCK) + N_EXPERTS) * MOE_BLOCK
    n_blocks = P // MOE_BLOCK
    row_tok = jnp.zeros((P,), jnp.int32).at[dest].set(tok_s)
    row_w = jnp.zeros((P,), jnp.float32).at[dest].set(w_s)
    block_start = jnp.arange(n_blocks, dtype=jnp.int32) * MOE_BLOCK
    block_expert = jnp.minimum(jnp.searchsorted(pad_end, block_start, side='right'), N_EXPERTS - 1)

    def expert_block(args):
        tok, e = args
        xb = hf[tok]
        a = jax.nn.silu(xb @ w1[e]) * (xb @ w3[e])
        return a @ w2[e]

    out = lax.map(expert_block, (row_tok.reshape(n_blocks, MOE_BLOCK), block_expert))
    out = out.reshape(P, D) * row_w[:, None].astype(out.dtype)
    y = jax.ops.segment_sum(out, row_tok, num_segments=N)
    return y.reshape(B_, L, D).astype(h.dtype)


def setup_inputs(seed: int = 0) -> dict:
    key = jax.random.key(seed)
    ks = iter(jax.random.split(key, 64))
    f32 = jnp.float32

    def nrm(shape, scale):
        return jax.random.normal(next(ks), shape, f32) * scale

    def gain(shape):
        return 1.0 + nrm(shape, 0.02)

    Lr = DEPTH
    x = nrm((BATCH, SEQ, D_MODEL), 1.0)
    c = nrm((BATCH, D_MODEL), 1.0)
    offset = jax.random.randint(next(ks), (BATCH, 1), 0, 4096, jnp.int32)
    positions = (jnp.arange(SEQ, dtype=jnp.int32)[None, :] + offset).astype(jnp.int32)
    w_ada = nrm((Lr, D_MODEL, 6 * D_MODEL), 0.3 * D_MODEL ** -0.5)
    b_ada = nrm((Lr, 6 * D_MODEL), 0.02)
    norm1_g = gain((Lr, D_MODEL))
    w_in = nrm((Lr, D_MODEL, IN_COLS), D_MODEL ** -0.5)
    pool_w = nrm((Lr, POOL_GROUPS, POOL_GROUP_DIM, POOL_GROUP_DIM), POOL_GROUP_DIM ** -0.5)
    pool_scale = 1.0 + nrm((Lr, POOL_WIDTH), 0.1)
    n = jnp.arange(SSM_STATE, dtype=f32)
    ssm_lam_re = -0.5 + nrm((Lr, SSM_N_GROUPS, SSM_STATE), 1e-3)
    ssm_lam_im = math.pi * n + nrm((Lr, SSM_N_GROUPS, SSM_STATE), 1e-3)
    ssm_log_dt = jax.random.uniform(next(ks), (Lr, SSM_N_GROUPS), f32, math.log(DT_MIN), math.log(DT_MAX))
    ssm_b_re = nrm((Lr, SSM_N_GROUPS, SSM_STATE, SSM_GROUP), (2 * SSM_GROUP) ** -0.5)
    ssm_b_im = nrm((Lr, SSM_N_GROUPS, SSM_STATE, SSM_GROUP), (2 * SSM_GROUP) ** -0.5)
    ssm_c_re = nrm((Lr, SSM_N_GROUPS, SSM_GROUP, SSM_STATE), (2 * SSM_STATE) ** -0.5)
    ssm_c_im = nrm((Lr, SSM_N_GROUPS, SSM_GROUP, SSM_STATE), (2 * SSM_STATE) ** -0.5)
    ssm_d = nrm((Lr, SSM_WIDTH), 1.0)
    ssm_w_glu = nrm((Lr, SSM_WIDTH, SSM_WIDTH), SSM_WIDTH ** -0.5)
    q_norm_g = gain((Lr, Q_LORA))
    kv_norm_g = gain((Lr, KV_LORA))
    w_uq = nrm((Lr, Q_LORA, MLA_HEADS, QK_HEAD), Q_LORA ** -0.5)
    w_ukv = nrm((Lr, KV_LORA, MLA_HEADS, QK_NOPE + V_HEAD), KV_LORA ** -0.5)
    out_norm_g = gain((Lr, D_MIX))
    w_out = nrm((Lr, D_MIX, D_MODEL), D_MIX ** -0.5)
    norm2_g = gain((Lr, D_MODEL))
    router_w_group = nrm((Lr, D_MODEL, MOE_GROUPS), D_MODEL ** -0.5)
    router_b_group = nrm((Lr, MOE_GROUPS), 0.01)
    router_w_expert = nrm((Lr, D_MODEL, N_EXPERTS), D_MODEL ** -0.5)
    router_b_expert = nrm((Lr, N_EXPERTS), 0.01)
    w_gate = nrm((Lr, N_EXPERTS, D_MODEL, D_EXPERT), D_MODEL ** -0.5)
    w_up = nrm((Lr, N_EXPERTS, D_MODEL, D_EXPERT), D_MODEL ** -0.5)
    w_down = nrm((Lr, N_EXPERTS, D_EXPERT, D_MODEL), D_EXPERT ** -0.5)
    final_g = gain((D_MODEL,))
    return {'x': x, 'c': c, 'positions': positions, 'w_ada': w_ada, 'b_ada': b_ada,
            'norm1_g': norm1_g, 'w_in': w_in, 'pool_w': pool_w, 'pool_scale': pool_scale,
            'ssm_lam_re': ssm_lam_re, 'ssm_lam_im': ssm_lam_im, 'ssm_log_dt': ssm_log_dt,
            'ssm_b_re': ssm_b_re, 'ssm_b_im': ssm_b_im, 'ssm_c_re': ssm_c_re, 'ssm_c_im': ssm_c_im,
            'ssm_d': ssm_d, 'ssm_w_glu': ssm_w_glu, 'q_norm_g': q_norm_g, 'kv_norm_g': kv_norm_g,
            'w_uq': w_uq, 'w_ukv': w_ukv, 'out_norm_g': out_norm_g, 'w_out': w_out,
            'norm2_g': norm2_g, 'router_w_group': router_w_group, 'router_b_group': router_b_group,
            'router_w_expert': router_w_expert, 'router_b_expert': router_b_expert,
            'w_gate': w_gate, 'w_up': w_up, 'w_down': w_down, 'final_g': final_g}


def reference(x, c, positions, w_ada, b_ada, norm1_g, w_in, pool_w, pool_scale,
              ssm_lam_re, ssm_lam_im, ssm_log_dt, ssm_b_re, ssm_b_im, ssm_c_re, ssm_c_im,
              ssm_d, ssm_w_glu, q_norm_g, kv_norm_g, w_uq, w_ukv, out_norm_g, w_out,
              norm2_g, router_w_group, router_b_group, router_w_expert, router_b_expert,
              w_gate, w_up, w_down, final_g):
    inv_freq = jnp.power(ROPE_THETA, -jnp.arange(0, QK_ROPE, 2, dtype=jnp.float32) / QK_ROPE)
    ang = positions.astype(jnp.float32)[..., None] * inv_freq
    cos, sin = jnp.cos(ang), jnp.sin(ang)
    in_splits = (POOL_WIDTH, POOL_WIDTH + SSM_WIDTH, POOL_WIDTH + SSM_WIDTH + Q_LORA,
                 POOL_WIDTH + SSM_WIDTH + Q_LORA + KV_LORA)
    for l in range(DEPTH):
        mod = c @ w_ada[l] + b_ada[l]
        sh_a, sc_a, g_a, sh_f, sc_f, g_f = [m[:, None, :] for m in jnp.split(mod, 6, axis=-1)]
        h = rms_norm(x, norm1_g[l]) * (1.0 + sc_a) + sh_a
        z = h @ w_in[l]
        u_pool, u_ssm, q_c, kv_c, k_pe = jnp.split(z, in_splits, axis=-1)
        y_pool = pool_mixer(u_pool, pool_w[l], pool_scale[l])
        y_ssm = s5_mixer(u_ssm, ssm_lam_re[l], ssm_lam_im[l], ssm_log_dt[l], ssm_b_re[l], ssm_b_im[l],
                         ssm_c_re[l], ssm_c_im[l], ssm_d[l], ssm_w_glu[l])
        y_mla = mla_mixer(q_c, kv_c, k_pe, q_norm_g[l], kv_norm_g[l], w_uq[l], w_ukv[l], cos, sin)
        gn = out_norm_g[l]
        y = jnp.concatenate([rms_norm(y_pool, gn[:POOL_WIDTH]),
                             rms_norm(y_ssm, gn[POOL_WIDTH:POOL_WIDTH + SSM_WIDTH]),
                             rms_norm(y_mla, gn[POOL_WIDTH + SSM_WIDTH:])], axis=-1)
        x = x + g_a * (y @ w_out[l])
        h = rms_norm(x, norm2_g[l]) * (1.0 + sc_f) + sh_f
        x = x + g_f * hier_moe(h, router_w_group[l], router_b_group[l], router_w_expert[l],
                               router_b_expert[l], w_gate[l], w_up[l], w_down[l])
    return rms_norm(x, final_g)
```

```python
import numpy as np
import concourse.bass as bass
import concourse.mybir as mybir
from contextlib import ExitStack

F32 = mybir.dt.float32
BF16 = mybir.dt.bfloat16
I32 = mybir.dt.int32
U32 = mybir.dt.uint32
AF = mybir.ActivationFunctionType
ALU = mybir.AluOpType
AX = mybir.AxisListType

SEM_LIMIT = 30000


class Buf:
    __slots__ = ("name", "last_write", "reads", "dsem", "dcount", "t", "psum")

    def __init__(self, name, t=None):
        self.name = name
        self.last_write = None
        self.reads = []
        self.dsem = None
        self.dcount = 0
        self.t = t
        self.psum = False


class Sched:
    def __init__(self, nc, es: ExitStack):
        self.nc = nc
        self.es = es
        self.es_top = es
        self.eng = {"pe": nc.tensor, "dve": nc.vector, "act": nc.scalar,
                    "pool": nc.gpsimd, "sp": nc.sync}
        self.sem = {}
        self.cnt = {}
        self.epoch = {}
        for k in self.eng:
            self.epoch[k] = 0
            self.sem[k] = es.enter_context(nc.semaphore(f"s_{k}_0"))
            self.cnt[k] = 0
        self.waited = {k: {} for k in self.eng}
        self.semobj = {}
        self.nbuf = 0
        self.ninstr = 0
        self.alldma = {}

    def sb(self, name, shape, dt):
        t = self.es.enter_context(self.nc.sbuf_tensor("sb_" + name, list(shape), dt))
        return t, Buf(name, t)

    def ps(self, name, shape, dt=F32):
        t = self.es.enter_context(self.nc.psum_tensor("ps_" + name, list(shape), dt))
        b = Buf(name, t)
        b.psum = True
        return t, b

    def buf(self, name):
        return Buf(name)

    def _wait(self, e, deps):
        best = {}
        for d in deps:
            if d is None:
                continue
            sem, val, en = d
            k = id(sem)
            if k not in best or best[k][1] < val:
                best[k] = (sem, val, en)
        for k, (sem, val, en) in best.items():
            if self.waited[e].get(k, 0) >= val:
                continue
            self.eng[e].wait_ge(sem, val)
            self.waited[e][k] = val
            self.ninstr += 1

    def _gather(self, e, reads, writes):
        deps = []
        for b in reads:
            lw = b.last_write
            if lw is not None:
                if not (e == "pe" and lw[2] == "pe"):
                    deps.append(lw)
            if b.psum:
                for r in b.reads:
                    if r[2] != e:
                        deps.append(r)
        for b in writes:
            lw = b.last_write
            if lw is not None and (lw[2] != e or e == "dma"):
                deps.append(lw)
            for r in b.reads:
                if r[2] != e or e == "dma":
                    deps.append(r)
        return deps

    def _record(self, dep, reads, writes):
        for b in reads:
            b.reads.append(dep)
            if len(b.reads) > 64:
                best = {}
                for d in b.reads:
                    k = id(d[0])
                    if k not in best or best[k][1] < d[1]:
                        best[k] = d
                b.reads = list(best.values())
        for b in writes:
            b.last_write = dep
            b.reads = []

    def op(self, e, fn, reads=(), writes=()):
        deps = self._gather(e, reads, writes)
        self._wait(e, deps)
        if self.cnt[e] >= SEM_LIMIT:
            self.epoch[e] += 1
            self.sem[e] = self.es_top.enter_context(self.nc.semaphore(f"s_{e}_{self.epoch[e]}"))
            self.cnt[e] = 0
        ins = fn()
        self.cnt[e] += 1
        ins.then_inc(self.sem[e], 1)
        dep = (self.sem[e], self.cnt[e], e)
        self._record(dep, reads, writes)
        self.ninstr += 1
        return ins

    def dma(self, q, fn, reads=(), writes=(), track=None):
        if track is None:
            track = writes[0] if (writes and writes[0].t is not None) else reads[0]
        deps = self._gather("dma", reads, writes)
        self._wait(q, deps)
        if track.dsem is None or track.dcount >= SEM_LIMIT:
            self.nbuf += 1
            track.dsem = self.es_top.enter_context(self.nc.semaphore(f"d_{self.nbuf}"))
            track.dcount = 0
        ins = fn()
        track.dcount += 16
        ins.then_inc(track.dsem, 16)
        dep = (track.dsem, track.dcount, "dma")
        self.alldma[id(track.dsem)] = dep
        self._record(dep, reads, writes)
        self.ninstr += 1
        return ins

    def finish(self, bufs, e="sp"):
        deps = []
        for b in bufs:
            if b.last_write is not None:
                deps.append(b.last_write)
            deps.extend(b.reads)
        deps.extend(self.alldma.values())
        self._wait(e, deps)

    def barrier(self):
        deps = []
        for e in ("pe", "dve", "act", "pool"):
            if self.cnt[e] > 0:
                deps.append((self.sem[e], self.cnt[e], e))
        dd = list(self.alldma.values())
        for e in ("pe", "dve", "act", "pool", "sp"):
            self._wait(e, [d for d in deps if d[2] != e] + dd)
        self.alldma = {}

import math

D = 2048
EPS = 1e-6
TWO_PI = 2.0 * math.pi
_c1 = np.float32(6.28125)
_r = TWO_PI - float(_c1)
_c2 = np.float32(_r)
_c3 = np.float32(_r - float(_c2))
CW1, CW2, CW3 = float(_c1), float(_c2), float(_c3)
MAGIC = 12582912.0
QSCALE = 192.0 ** -0.5


def make_ident(S, nc, idt, idb):
    S.op("pool", lambda: nc.gpsimd.memset(idt[:], 0.0), writes=[idb])
    S.op("pool", lambda: nc.gpsimd.affine_select(out=idt[:], in_=idt[:], pattern=[[-1, idt.shape[1]]], compare_op=ALU.not_equal, fill=1.0, base=0, channel_multiplier=1), reads=[idb], writes=[idb])


def emit_sincos(S, nc, P, N, pos_t, pos_b, invf_t, cst_b, halfpi_t, a, k, r, cos, sinpm):
    (a_t, a_b), (k_t, k_b), (r_t, r_b), (c_t, c_b), (s_t, s_b) = a, k, r, cos, sinpm
    S.op("dve", lambda: nc.vector.tensor_copy(a_t[0:P, 0:N], pos_t[0:P, 0:N]), reads=[pos_b], writes=[a_b])
    S.op("dve", lambda: nc.vector.tensor_scalar(out=a_t[0:P, 0:N], in0=a_t[0:P, 0:N], scalar1=invf_t[0:P, 0:1], scalar2=None, op0=ALU.mult), reads=[a_b, cst_b], writes=[a_b])
    S.op("dve", lambda: nc.vector.tensor_scalar(out=k_t[0:P, 0:N], in0=a_t[0:P, 0:N], scalar1=1.0 / TWO_PI, scalar2=MAGIC, op0=ALU.mult, op1=ALU.add), reads=[a_b], writes=[k_b])
    S.op("dve", lambda: nc.vector.tensor_scalar(out=k_t[0:P, 0:N], in0=k_t[0:P, 0:N], scalar1=-MAGIC, scalar2=None, op0=ALU.add), reads=[k_b], writes=[k_b])
    S.op("dve", lambda: nc.vector.scalar_tensor_tensor(out=r_t[0:P, 0:N], in0=k_t[0:P, 0:N], scalar=-CW1, in1=a_t[0:P, 0:N], op0=ALU.mult, op1=ALU.add), reads=[k_b, a_b], writes=[r_b])
    S.op("dve", lambda: nc.vector.scalar_tensor_tensor(out=r_t[0:P, 0:N], in0=k_t[0:P, 0:N], scalar=-CW2, in1=r_t[0:P, 0:N], op0=ALU.mult, op1=ALU.add), reads=[k_b, r_b], writes=[r_b])
    S.op("dve", lambda: nc.vector.scalar_tensor_tensor(out=r_t[0:P, 0:N], in0=k_t[0:P, 0:N], scalar=-CW3, in1=r_t[0:P, 0:N], op0=ALU.mult, op1=ALU.add), reads=[k_b, r_b], writes=[r_b])
    S.op("dve", lambda: nc.vector.tensor_scalar(out=r_t[0:P, 0:N], in0=r_t[0:P, 0:N], scalar1=-math.pi, scalar2=math.pi, op0=ALU.max, op1=ALU.min), reads=[r_b], writes=[r_b])
    h = P // 2
    S.op("act", lambda: nc.scalar.activation(out=s_t[0:h, 0:N], in_=r_t[0:h, 0:N], func=AF.Sin, scale=-1.0), reads=[r_b], writes=[s_b])
    S.op("act", lambda: nc.scalar.activation(out=s_t[h:P, 0:N], in_=r_t[h:P, 0:N], func=AF.Sin, scale=1.0), reads=[r_b], writes=[s_b])
    S.op("dve", lambda: nc.vector.scalar_tensor_tensor(out=k_t[0:P, 0:N], in0=r_t[0:P, 0:N], scalar=-1.0, in1=r_t[0:P, 0:N], op0=ALU.mult, op1=ALU.max), reads=[r_b], writes=[k_b])
    S.op("act", lambda: nc.scalar.activation(out=c_t[0:P, 0:N], in_=k_t[0:P, 0:N], func=AF.Sin, scale=-1.0, bias=halfpi_t[0:P, 0:1]), reads=[k_b, cst_b], writes=[c_b])


def build_A(NTOK, stage=9):
    nc = bass.Bass("TRN2", target_bir_lowering=False)
    NS = NTOK // 512
    di = lambda n, s, d=F32: nc.dram_tensor(n, list(s), d, kind="ExternalInput").ap()
    do = lambda n, s, d=F32: nc.dram_tensor(n, list(s), d, kind="ExternalOutput").ap()
    x = di("x", [NTOK, D]); cT = di("cT", [128, 16]); wada = di("wadaA", [D, 4096]); bada = di("badaA", [1, 4096])
    g1 = di("g1", [1, D]); w_in = di("w_in", [D, 1856]); w_in_sw = di("w_in_sw", [D, 64])
    gq = di("gq", [128, 4]); gkv = di("gkv", [128, 2])
    wq_n = di("wq_n", [512, 1024]); wq_p = di("wq_p", [512, 512]); wq_ps = di("wq_ps", [512, 512])
    wk = di("wk", [256, 1024]); wv = di("wv", [256, 1024])
    pos = di("pos", [1, NTOK], I32); invf = di("invf", [64, 1])
    upT = do("upT", [512, NTOK]); usT = do("usT", [512, NTOK])
    QT = do("QT", [8, 192, NTOK], BF16); KT = do("KT", [8, 128, NTOK], BF16)
    kpeT = do("kpeT", [64, NTOK], BF16); V = do("V", [NTOK, 1024], BF16)
    outs = [S_ for S_ in ()]
    with ExitStack() as es:
        S = Sched(nc, es)
        ob = {n: S.buf(n) for n in ("upT", "usT", "QT", "KT", "kpeT", "V")}
        idt, idb = S.sb("idt", [128, 128], BF16)
        make_ident(S, nc, idt, idb)
        ones_t, ones_b = S.sb("ones", [128, 128], BF16)
        S.op("pool", lambda: nc.gpsimd.memset(ones_t[:], 1.0), writes=[ones_b])
        cst_t, cst_b = S.sb("cst", [128, 8], F32)
        S.op("pool", lambda: nc.gpsimd.memset(cst_t[:, 0:1], EPS), writes=[cst_b])
        S.op("pool", lambda: nc.gpsimd.memset(cst_t[:, 1:2], math.pi / 2), writes=[cst_b])
        S.dma("sp", lambda: nc.sync.dma_start(out=cst_t[0:64, 2:3], in_=invf[:, :]), writes=[cst_b])
        gq_t, gq_b = S.sb("gq", [128, 4], F32)
        gkv_t, gkv_b = S.sb("gkv", [128, 2], F32)
        S.dma("sp", lambda: nc.sync.dma_start(out=gq_t[:], in_=gq[:, :]), writes=[gq_b])
        S.dma("sp", lambda: nc.sync.dma_start(out=gkv_t[:], in_=gkv[:, :]), writes=[gkv_b])
        win_t, win_b = S.sb("win", [128, 16, 1856 + 64], BF16)
        w_in_v = w_in.rearrange("(c p) n -> p c n", p=128)
        w_in_sw_v = w_in_sw.rearrange("(c p) n -> p c n", p=128)
        for c in range(16):
            S.dma("pool", lambda: nc.gpsimd.dma_start(out=win_t[:, c, 0:1856], in_=w_in_v[:, c, :]), writes=[win_b])
        S.dma("pool", lambda: nc.gpsimd.dma_start(out=win_t[:, :, 1856:1920], in_=w_in_sw_v[:, :, :]), writes=[win_b])
        wq_t, wq_b = S.sb("wq", [128, 4, 2048], BF16)
        S.dma("pool", lambda: nc.gpsimd.dma_start(out=wq_t[:, :, 0:1024], in_=wq_n.rearrange("(c p) n -> p c n", p=128)), writes=[wq_b])
        S.dma("pool", lambda: nc.gpsimd.dma_start(out=wq_t[:, :, 1024:1536], in_=wq_p.rearrange("(c p) n -> p c n", p=128)), writes=[wq_b])
        S.dma("pool", lambda: nc.gpsimd.dma_start(out=wq_t[:, :, 1536:2048], in_=wq_ps.rearrange("(c p) n -> p c n", p=128)), writes=[wq_b])
        wkv_t, wkv_b = S.sb("wkv", [128, 2, 2048], BF16)
        S.dma("pool", lambda: nc.gpsimd.dma_start(out=wkv_t[:, :, 0:1024], in_=wk.rearrange("(c p) n -> p c n", p=128)), writes=[wkv_b])
        S.dma("pool", lambda: nc.gpsimd.dma_start(out=wkv_t[:, :, 1024:2048], in_=wv.rearrange("(c p) n -> p c n", p=128)), writes=[wkv_b])
        gmod_t, gmod_b = S.sb("gmod", [128, D], F32)
        shA_t, shA_b = S.sb("shA", [128, D], F32)
        xt = [S.sb(f"xt{i}", [128, D], F32) for i in range(2)]
        hb = [S.sb(f"hb{i}", [128, D], BF16) for i in range(2)]
        hT_t, hT_b = S.sb("hT", [128, 16, 512], BF16)
        sq_t, sq_b = S.sb("sq", [128, D], BF16)
        st = [S.sb(f"st{i}", [128, 1], F32) for i in range(4)]
        qc_t, qc_b = S.sb("qc", [128, 6, 512], F32)
        qcn_t, qcn_b = S.sb("qcn", [128, 6, 512], BF16)
        rs_t, rs_b = S.sb("rs", [128, 512], F32)
        stg = [S.sb(f"stg{i}", [128, 512], F32) for i in range(4)]
        stgh = [S.sb(f"stgh{i}", [128, 512], BF16) for i in range(4)]
        vst = [S.sb(f"vst{i}", [128, 1024], BF16) for i in range(2)]
        pos_t, pos_b = S.sb("pos", [64, 512], I32)
        tA = S.sb("tA", [64, 512], F32); tK = S.sb("tK", [64, 512], F32); tR = S.sb("tR", [64, 512], F32)
        cos = S.sb("cos", [64, 512], F32); sinpm = S.sb("sinpm", [64, 512], F32)
        kpr = S.sb("kpr", [64, 512], F32); kps = S.sb("kps", [64, 512], F32)
        pT = [S.ps(f"pT{i}", [128, 1024], BF16) for i in range(2)]
        pZ = [S.ps(f"pZ{i}", [128, 512], F32) for i in range(3)]
        pS = S.ps("pS", [128, 512], F32)
        pQ = [S.ps(f"pQ{i}", [128, 512], F32) for i in range(2)]

        if stage < 1:
            S.finish([win_b, wq_b, wkv_b, gq_b, cst_b]); return nc
        cB_t, cB_b = S.sb("cB", [128, 16, 128], F32)
        cT_t, cT_b = S.sb("cTt", [128, 16], F32)
        S.dma("sp", lambda: nc.sync.dma_start(out=cT_t[:], in_=cT[:, :]), writes=[cT_b])
        S.op("dve", lambda: nc.vector.tensor_copy(cB_t[:], cT_t[:].unsqueeze(2).to_broadcast([128, 16, 128])), reads=[cT_b], writes=[cB_b])
        S.dma("sp", lambda: nc.sync.dma_start(out=shA_t[:], in_=bada[0:1, 0:D].partition_broadcast(128)), writes=[shA_b])
        S.dma("sp", lambda: nc.sync.dma_start(out=gmod_t[:], in_=bada[0:1, D:2 * D].partition_broadcast(128)), writes=[gmod_b])
        wada_v = wada.rearrange("(c p) n -> p c n", p=128)
        nsl = 0
        for blk in range(8):
            pz_t, pz_b = pZ[blk % 2]
            for q4 in range(4):
                (w_t, w_b) = xt[nsl % 2]; nsl += 1
                wv_ = w_t[:].rearrange("p (c n) -> p c n", c=4)
                S.dma("sp", lambda: nc.sync.dma_start(out=wv_, in_=wada_v[:, q4 * 4:(q4 + 1) * 4, blk * 512:(blk + 1) * 512]), writes=[w_b])
                for cc in range(4):
                    c = q4 * 4 + cc
                    S.op("pe", lambda: nc.tensor.matmul(pz_t[:], lhsT=cB_t[:, c, :], rhs=wv_[:, cc, :], start=(c == 0), stop=(c == 15)), reads=[cB_b, w_b], writes=[pz_b])
            tgt_t, tgt_b = (shA_t, shA_b) if blk < 4 else (gmod_t, gmod_b)
            cs = slice((blk % 4) * 512, (blk % 4 + 1) * 512)
            S.op("dve", lambda: nc.vector.tensor_tensor(out=tgt_t[:, cs], in0=pz_t[:], in1=tgt_t[:, cs], op=ALU.add), reads=[pz_b, tgt_b], writes=[tgt_b])
        (g_t, g_b) = xt[nsl % 2]; nsl += 1
        S.dma("sp", lambda: nc.sync.dma_start(out=g_t[:], in_=g1[0:1, :].partition_broadcast(128)), writes=[g_b])
        S.op("dve", lambda: nc.vector.scalar_tensor_tensor(out=gmod_t[:], in0=gmod_t[:], scalar=1.0, in1=g_t[:], op0=ALU.add, op1=ALU.mult), reads=[gmod_b, g_b], writes=[gmod_b])

        if stage < 2:
            S.finish([gmod_b, shA_b, win_b, wq_b, wkv_b]); return nc
        x_v = x.rearrange("(n p) d -> n p d", p=128)
        ntile = 0
        nst = 0
        nz = 0
        nq = 0
        nstg = 0
        for s in range(NS):
            t0 = s * 512
            S.dma("sp", lambda: nc.sync.dma_start(out=pos_t[:], in_=pos[0:1, t0:t0 + 512].partition_broadcast(64)), writes=[pos_b])
            emit_sincos(S, nc, 64, 512, pos_t, pos_b, cst_t[:, 2:3], cst_b, cst_t[:, 1:2], tA, tK, tR, cos, sinpm)
            if stage < 3:
                S.finish([cos[1], sinpm[1], gmod_b, shA_b, win_b, wq_b, wkv_b]); return nc
            for j in range(4):
                (x_t, x_b) = xt[nsl % 2]; nsl += 1
                (h_t, h_b) = hb[ntile % 2]
                (ss_t, ss_b) = st[nst % 4]; nst += 1
                (rr_t, rr_b) = st[nst % 4]; nst += 1
                S.dma("sp", lambda: nc.sync.dma_start(out=x_t[:], in_=x_v[s * 4 + j]), writes=[x_b])
                S.op("act", lambda: nc.scalar.activation(out=sq_t[:], in_=x_t[:], func=AF.Square, accum_out=ss_t[:]), reads=[x_b], writes=[sq_b, ss_b])
                S.op("act", lambda: nc.scalar.activation(out=rr_t[:], in_=ss_t[:], func=AF.Sqrt, scale=1.0 / D, bias=cst_t[:, 0:1]), reads=[ss_b, cst_b], writes=[rr_b])
                S.op("dve", lambda: nc.vector.reciprocal(rr_t[:], rr_t[:]), reads=[rr_b], writes=[rr_b])
                S.op("dve", lambda: nc.vector.scalar_tensor_tensor(out=x_t[:], in0=x_t[:], scalar=rr_t[:, 0:1], in1=gmod_t[:], op0=ALU.mult, op1=ALU.mult), reads=[x_b, rr_b, gmod_b], writes=[x_b])
                S.op("pool", lambda: nc.gpsimd.tensor_tensor(out=h_t[:], in0=x_t[:], in1=shA_t[:], op=ALU.add), reads=[x_b, shA_b], writes=[h_b])
                for half in range(2):
                    (p_t, p_b) = pT[half]
                    for cc in range(8):
                        c = half * 8 + cc
                        S.op("pe", lambda: nc.tensor.transpose(p_t[:, cc * 128:(cc + 1) * 128], h_t[:, c * 128:(c + 1) * 128], idt[:]), reads=[h_b, idb], writes=[p_b])
                    eng = "act" if half == 0 else "dve"
                    dst = hT_t[:, half * 8:(half + 1) * 8, j * 128:(j + 1) * 128]
                    src = p_t[:].rearrange("p (c n) -> p c n", c=8)
                    if eng == "act":
                        S.op("act", lambda: nc.scalar.copy(dst, src), reads=[p_b], writes=[hT_b])
                    else:
                        S.op("dve", lambda: nc.vector.tensor_copy(dst, src), reads=[p_b], writes=[hT_b])
                ntile += 1
            if stage < 4:
                S.finish([hT_b, cos[1], sinpm[1], gmod_b, shA_b, win_b, wq_b, wkv_b]); return nc
            def zblock(c0, M):
                nonlocal nz
                (pz_t, pz_b) = pZ[nz % 3]; nz += 1
                for c in range(16):
                    S.op("pe", lambda: nc.tensor.matmul(pz_t[0:M, :], lhsT=win_t[:, c, c0:c0 + M], rhs=hT_t[:, c, :], start=(c == 0), stop=(c == 15)), reads=[win_b, hT_b], writes=[pz_b])
                return pz_t, pz_b
            for blk in range(8):
                pz_t, pz_b = zblock(blk * 128, 128)
                (sg_t, sg_b) = stg[nstg % 4]; nstg += 1
                if blk % 2 == 0:
                    S.op("act", lambda: nc.scalar.copy(sg_t[:], pz_t[:]), reads=[pz_b], writes=[sg_b])
                else:
                    S.op("dve", lambda: nc.vector.tensor_copy(sg_t[:], pz_t[:]), reads=[pz_b], writes=[sg_b])
                dst = (upT if blk < 4 else usT)[(blk % 4) * 128:(blk % 4 + 1) * 128, t0:t0 + 512]
                S.dma("sp", lambda: nc.sync.dma_start(out=dst, in_=sg_t[:]), reads=[sg_b], writes=[ob["upT" if blk < 4 else "usT"]])
            if stage < 5:
                S.finish(list(ob.values()) + [hT_b, wq_b, wkv_b]); return nc
            for grp, (b0, nb, gt, gb_, dim) in enumerate(((0, 4, gq_t, gq_b, 512), (4, 2, gkv_t, gkv_b, 256))):
                (ps_t, ps_b) = pS
                for i in range(nb):
                    blk = b0 + i
                    pz_t, pz_b = zblock(1024 + blk * 128, 128)
                    S.op("act", lambda: nc.scalar.activation(out=sq_t[:, i * 512:(i + 1) * 512], in_=pz_t[:], func=AF.Square), reads=[pz_b], writes=[sq_b])
                    S.op("dve", lambda: nc.vector.tensor_copy(qc_t[:, blk, :], pz_t[:]), reads=[pz_b], writes=[qc_b])
                    S.op("pe", lambda: nc.tensor.matmul(ps_t[:], lhsT=ones_t[:], rhs=sq_t[:, i * 512:(i + 1) * 512], start=(i == 0), stop=(i == nb - 1)), reads=[ones_b, sq_b], writes=[ps_b])
                S.op("act", lambda: nc.scalar.activation(out=rs_t[:], in_=ps_t[:], func=AF.Sqrt, scale=1.0 / dim, bias=cst_t[:, 0:1]), reads=[ps_b, cst_b], writes=[rs_b])
                S.op("dve", lambda: nc.vector.reciprocal(rs_t[:], rs_t[:]), reads=[rs_b], writes=[rs_b])
                for i in range(nb):
                    blk = b0 + i
                    S.op("dve", lambda: nc.vector.scalar_tensor_tensor(out=qcn_t[:, blk, :], in0=qc_t[:, blk, :], scalar=gt[:, i:i + 1], in1=rs_t[:], op0=ALU.mult, op1=ALU.mult), reads=[qc_b, gb_, rs_b], writes=[qcn_b])

            def rope_out(p1_t, p1_b, p2_t, p2_b, scale, dst, obuf):
                nonlocal nstg
                S.op("dve", lambda: nc.vector.tensor_tensor(out=kpr[0][:], in0=p1_t[0:64, :], in1=cos[0][:], op=ALU.mult), reads=[p1_b, cos[1]], writes=[kpr[1]])
                S.op("dve", lambda: nc.vector.tensor_tensor(out=kps[0][:], in0=p2_t[0:64, :], in1=sinpm[0][:], op=ALU.mult), reads=[p2_b, sinpm[1]], writes=[kps[1]])
                (sh_t, sh_b) = stgh[nstg % 4]; nstg += 1
                S.op("pool", lambda: nc.gpsimd.tensor_tensor(out=sh_t[0:64, :], in0=kpr[0][:], in1=kps[0][:], op=ALU.add), reads=[kpr[1], kps[1]], writes=[sh_b])
                S.dma("sp", lambda: nc.sync.dma_start(out=dst, in_=sh_t[0:64, :]), reads=[sh_b], writes=[obuf])
            if stage < 6:
                S.finish(list(ob.values()) + [qcn_b]); return nc
            p1_t, p1_b = zblock(1792, 64)
            p2_t, p2_b = zblock(1856, 64)
            rope_out(p1_t, p1_b, p2_t, p2_b, 1.0, kpeT[:, t0:t0 + 512], ob["kpeT"])
            if stage < 7:
                S.finish(list(ob.values()) + [qcn_b]); return nc
            for h in range(8):
                (pq_t, pq_b) = pQ[nq % 2]; nq += 1
                for c in range(4):
                    S.op("pe", lambda: nc.tensor.matmul(pq_t[:], lhsT=wq_t[:, c, h * 128:(h + 1) * 128], rhs=qcn_t[:, c, :], start=(c == 0), stop=(c == 3)), reads=[wq_b, qcn_b], writes=[pq_b])
                (sh_t, sh_b) = stgh[nstg % 4]; nstg += 1
                S.op("act", lambda: nc.scalar.activation(out=sh_t[:], in_=pq_t[:], func=AF.Copy, scale=QSCALE), reads=[pq_b], writes=[sh_b])
                S.dma("sp", lambda: nc.sync.dma_start(out=QT[h, 0:128, t0:t0 + 512], in_=sh_t[:]), reads=[sh_b], writes=[ob["QT"]])
                (p1_t, p1_b) = pQ[nq % 2]; nq += 1
                for c in range(4):
                    S.op("pe", lambda: nc.tensor.matmul(p1_t[0:64, :], lhsT=wq_t[:, c, 1024 + h * 64:1024 + (h + 1) * 64], rhs=qcn_t[:, c, :], start=(c == 0), stop=(c == 3)), reads=[wq_b, qcn_b], writes=[p1_b])
                (p2_t, p2_b) = pZ[nz % 3]; nz += 1
                for c in range(4):
                    S.op("pe", lambda: nc.tensor.matmul(p2_t[0:64, :], lhsT=wq_t[:, c, 1536 + h * 64:1536 + (h + 1) * 64], rhs=qcn_t[:, c, :], start=(c == 0), stop=(c == 3)), reads=[wq_b, qcn_b], writes=[p2_b])
                S.op("dve", lambda: nc.vector.tensor_tensor(out=kpr[0][:], in0=p1_t[0:64, :], in1=cos[0][:], op=ALU.mult), reads=[p1_b, cos[1]], writes=[kpr[1]])
                S.op("dve", lambda: nc.vector.tensor_tensor(out=kps[0][:], in0=p2_t[0:64, :], in1=sinpm[0][:], op=ALU.mult), reads=[p2_b, sinpm[1]], writes=[kps[1]])
                S.op("pool", lambda: nc.gpsimd.tensor_tensor(out=kpr[0][:], in0=kpr[0][:], in1=kps[0][:], op=ALU.add), reads=[kpr[1], kps[1]], writes=[kpr[1]])
                (sh_t, sh_b) = stgh[nstg % 4]; nstg += 1
                S.op("act", lambda: nc.scalar.activation(out=sh_t[0:64, :], in_=kpr[0][:], func=AF.Copy, scale=QSCALE), reads=[kpr[1]], writes=[sh_b])
                S.dma("sp", lambda: nc.sync.dma_start(out=QT[h, 128:192, t0:t0 + 512], in_=sh_t[0:64, :]), reads=[sh_b], writes=[ob["QT"]])
            if stage < 8:
                S.finish(list(ob.values()) + [qcn_b]); return nc
            for h in range(8):
                (pq_t, pq_b) = pQ[nq % 2]; nq += 1
                for c in range(2):
                    S.op("pe", lambda: nc.tensor.matmul(pq_t[:], lhsT=wkv_t[:, c, h * 128:(h + 1) * 128], rhs=qcn_t[:, 4 + c, :], start=(c == 0), stop=(c == 1)), reads=[wkv_b, qcn_b], writes=[pq_b])
                (sh_t, sh_b) = stgh[nstg % 4]; nstg += 1
                if h % 2 == 0:
                    S.op("act", lambda: nc.scalar.copy(sh_t[:], pq_t[:]), reads=[pq_b], writes=[sh_b])
                else:
                    S.op("dve", lambda: nc.vector.tensor_copy(sh_t[:], pq_t[:]), reads=[pq_b], writes=[sh_b])
                S.dma("sp", lambda: nc.sync.dma_start(out=KT[h, :, t0:t0 + 512], in_=sh_t[:]), reads=[sh_b], writes=[ob["KT"]])
            for j in range(4):
                (v_t, v_b) = vst[j % 2]
                for hh in range(2):
                    (pq_t, pq_b) = pQ[nq % 2]; nq += 1
                    for c in range(2):
                        S.op("pe", lambda: nc.tensor.matmul(pq_t[:], lhsT=qcn_t[:, 4 + c, j * 128:(j + 1) * 128], rhs=wkv_t[:, c, 1024 + hh * 512:1024 + (hh + 1) * 512], start=(c == 0), stop=(c == 1)), reads=[wkv_b, qcn_b], writes=[pq_b])
                    if hh == 0:
                        S.op("act", lambda: nc.scalar.copy(v_t[:, 0:512], pq_t[:]), reads=[pq_b], writes=[v_b])
                    else:
                        S.op("dve", lambda: nc.vector.tensor_copy(v_t[:, 512:1024], pq_t[:]), reads=[pq_b], writes=[v_b])
                S.dma("sp", lambda: nc.sync.dma_start(out=V[t0 + j * 128:t0 + (j + 1) * 128, :], in_=v_t[:]), reads=[v_b], writes=[ob["V"]])
        S.finish(list(ob.values()))
        print("A ninstr", S.ninstr)
    return nc

import math

NEG = -30000.0


def barrier(S):
    deps = []
    for e in ("pe", "dve", "act", "pool"):
        if S.cnt[e] > 0:
            deps.append((S.sem[e], S.cnt[e], e))
    for e in ("pe", "dve", "act", "pool", "sp"):
        S._wait(e, [d for d in deps if d[2] != e])


def emit_attention(S, nc, L, QT, KT, kpeT, V, ymT, ymT_b, idt, idb, ones_t, ones_b):
    NQ = L // 512
    NB = L // 128
    with ExitStack() as es2:
        es_save = S.es
        S.es = es2
        K_t, K_b = S.sb("attK", [128, L], BF16)
        P_t, P_b = S.sb("attKpe", [64, L], BF16)
        V_t, V_b = S.sb("attV", [128, NB, 128], BF16)
        mask_t, mask_b = S.sb("attmask", [128, 4, 512], BF16)
        qn = [S.sb(f"qn{i}", [128, 512], BF16) for i in range(2)]
        qp = [S.sb(f"qp{i}", [64, 512], BF16) for i in range(2)]
        NPT = 4
        pt = [S.sb(f"pt{i}", [128, 512], BF16) for i in range(NPT)]
        rl_t, rl_b = S.sb("attrl", [128, 512], F32)
        ost = [S.sb(f"ost{i}", [128, 512], F32) for i in range(2)]
        acc = [S.sb(f"attacc{i}", [128, 512], F32) for i in range(2)]
        onesf_t, onesf_b = S.sb("attonesf", [128, 128], F32)
        S.op("pool", lambda: nc.gpsimd.memset(onesf_t[:], 1.0), writes=[onesf_b])
        pS = [S.ps(f"aS{i}", [128, 512], F32) for i in range(NPT)]
        pO = [S.ps(f"aO{i}", [128, 512], F32) for i in range(2)]
        pL = [S.ps(f"aL{i}", [128, 512], F32) for i in range(2)]
        S.op("pool", lambda: nc.gpsimd.memset(mask_t[:], 0.0), writes=[mask_b])
        for d in range(4):
            S.op("pool", lambda: nc.gpsimd.affine_select(out=mask_t[:, d, :], in_=mask_t[:, d, :], pattern=[[1, 512]], compare_op=ALU.is_ge, fill=NEG, base=-128 * d, channel_multiplier=-1), reads=[mask_b], writes=[mask_b])
        S.dma("sp", lambda: nc.sync.dma_start(out=P_t[:], in_=kpeT[:, :]), writes=[P_b])
        nqt = 0
        for h in range(2):
            for part in range(4):
                sl = slice(part * L // 4, (part + 1) * L // 4)
                S.dma("sp", lambda: nc.sync.dma_start(out=K_t[:, sl], in_=KT[h, :, sl]), writes=[K_b])
            Vv = V.rearrange("(n p) h d -> p n h d", p=128)
            for part in range(4):
                nsl = slice(part * NB // 4, (part + 1) * NB // 4)
                S.dma("sp", lambda: nc.sync.dma_start(out=V_t[:, nsl, :], in_=Vv[:, nsl, h, :]), writes=[V_b])
            for j in range(NQ):
                (qn_t, qn_b) = qn[nqt % 2]; (qp_t, qp_b) = qp[nqt % 2]
                (o_t, o_b) = pO[nqt % 2]; (l_t, l_b) = pL[nqt % 2]
                (os_t, os_b) = ost[nqt % 2]; (ac_t, ac_b) = acc[nqt % 2]
                nqt += 1
                S.dma("sp", lambda: nc.sync.dma_start(out=qn_t[:], in_=QT[h, 0:128, j * 512:(j + 1) * 512]), writes=[qn_b])
                S.dma("sp", lambda: nc.sync.dma_start(out=qp_t[:], in_=QT[h, 128:192, j * 512:(j + 1) * 512]), writes=[qp_b])
                nblk = 4 * j + 4

                def emit_S(i):
                    (s_t, s_b) = pS[i % NPT]
                    dg = i - 4 * j
                    S.op("pe", lambda: nc.tensor.matmul(s_t[:], lhsT=K_t[:, i * 128:(i + 1) * 128], rhs=qn_t[:], start=True, stop=False), reads=[K_b, qn_b], writes=[s_b])
                    S.op("pe", lambda: nc.tensor.matmul(s_t[:], lhsT=P_t[:, i * 128:(i + 1) * 128], rhs=qp_t[:], start=False, stop=(dg < 0)), reads=[P_b, qp_b], writes=[s_b])
                    if dg >= 0:
                        S.op("pe", lambda: nc.tensor.matmul(s_t[:], lhsT=idt[:], rhs=mask_t[:, dg, :], start=False, stop=True), reads=[idb, mask_b], writes=[s_b])
                LOOK = 2
                for i in range(min(LOOK, nblk)):
                    emit_S(i)
                for i in range(nblk):
                    if i + LOOK < nblk:
                        emit_S(i + LOOK)
                    (s_t, s_b) = pS[i % NPT]
                    (p_t, p_b) = pt[i % NPT]
                    S.op("act", lambda: nc.scalar.activation(out=p_t[:], in_=s_t[:], func=AF.Exp), reads=[s_b], writes=[p_b])
                    S.op("pe", lambda: nc.tensor.matmul(o_t[:], lhsT=V_t[:, i, :], rhs=p_t[:], start=(i == 0), stop=(i == nblk - 1)), reads=[V_b, p_b], writes=[o_b])
                    S.op("pe", lambda: nc.tensor.matmul(l_t[:], lhsT=ones_t[:], rhs=p_t[:], start=(i == 0), stop=(i == nblk - 1)), reads=[ones_b, p_b], writes=[l_b])
                S.op("dve", lambda: nc.vector.reciprocal(rl_t[:], l_t[:]), reads=[l_b], writes=[rl_b])
                S.op("dve", lambda: nc.vector.tensor_tensor(out=os_t[:], in0=o_t[:], in1=rl_t[:], op=ALU.mult), reads=[o_b, rl_b], writes=[os_b])
                S.dma("sp", lambda: nc.sync.dma_start(out=ymT[h * 128:(h + 1) * 128, j * 512:(j + 1) * 512], in_=os_t[:]), reads=[os_b], writes=[ymT_b])
        S.barrier()
        S.es = es_save


def sincos_angle(S, nc, P, N, ang, k, r, halfpi_t, halfpi_b, sin_out, cos_out, sin_scale=1.0):
    (a_t, a_b), (k_t, k_b), (r_t, r_b) = ang, k, r
    S.op("dve", lambda: nc.vector.tensor_scalar(out=k_t, in0=a_t, scalar1=1.0 / TWO_PI, scalar2=MAGIC, op0=ALU.mult, op1=ALU.add), reads=[a_b], writes=[k_b])
    S.op("dve", lambda: nc.vector.tensor_scalar(out=k_t, in0=k_t, scalar1=-MAGIC, scalar2=None, op0=ALU.add), reads=[k_b], writes=[k_b])
    S.op("dve", lambda: nc.vector.scalar_tensor_tensor(out=r_t, in0=k_t, scalar=-CW1, in1=a_t, op0=ALU.mult, op1=ALU.add), reads=[k_b, a_b], writes=[r_b])
    S.op("dve", lambda: nc.vector.scalar_tensor_tensor(out=r_t, in0=k_t, scalar=-CW2, in1=r_t, op0=ALU.mult, op1=ALU.add), reads=[k_b, r_b], writes=[r_b])
    S.op("dve", lambda: nc.vector.scalar_tensor_tensor(out=r_t, in0=k_t, scalar=-CW3, in1=r_t, op0=ALU.mult, op1=ALU.add), reads=[k_b, r_b], writes=[r_b])
    S.op("dve", lambda: nc.vector.tensor_scalar(out=r_t, in0=r_t, scalar1=-math.pi, scalar2=math.pi, op0=ALU.max, op1=ALU.min), reads=[r_b], writes=[r_b])
    S.op("act", lambda: nc.scalar.activation(out=sin_out[0], in_=r_t, func=AF.Sin, scale=1.0), reads=[r_b], writes=[sin_out[1]])
    S.op("dve", lambda: nc.vector.scalar_tensor_tensor(out=k_t, in0=r_t, scalar=-1.0, in1=r_t, op0=ALU.mult, op1=ALU.max), reads=[r_b], writes=[k_b])
    S.op("act", lambda: nc.scalar.activation(out=cos_out[0], in_=k_t, func=AF.Sin, scale=-1.0, bias=halfpi_t), reads=[k_b, halfpi_b], writes=[cos_out[1]])


GELU_C = 2.0 * math.sqrt(2.0 / math.pi)


def emit_ssm(S, nc, L, usT, lamre, lamim, logdt, bre, bim, cA, cB, dsk, ysT, ysT_b, idt, idb, T=512):
    NBLK = L // T
    with ExitStack() as es2:
        es_save = S.es
        S.es = es2
        V = nc.vector; G = nc.gpsimd; A = nc.scalar; PE = nc.tensor
        idf_t, idf_b = S.sb("ssm_idf", [128, 128], F32)
        make_ident(S, nc, idf_t, idf_b)
        hp_t, hp_b = S.sb("ssm_hp", [128, 1], F32)
        S.op("pool", lambda: G.memset(hp_t[:], math.pi / 2), writes=[hp_b])
        par_t, par_b = S.sb("ssm_par", [128, 3, 8], F32)
        S.dma("sp", lambda: nc.sync.dma_start(out=par_t[:, 0, :], in_=lamre[:, :]), writes=[par_b])
        S.dma("sp", lambda: nc.sync.dma_start(out=par_t[:, 1, :], in_=lamim[:, :]), writes=[par_b])
        S.dma("sp", lambda: nc.sync.dma_start(out=par_t[:, 2, :], in_=logdt[:, :]), writes=[par_b])
        d_t, d_b = S.sb("ssm_d", [128, 1], F32)
        S.dma("sp", lambda: nc.sync.dma_start(out=d_t[:], in_=dsk[:, :]), writes=[d_b])
        w = {}
        for n in ("dt", "mag", "th", "sn", "cs", "k", "r", "den", "fre", "fim", "t1", "t2", "thT", "cT", "sT", "spm"):
            w[n] = S.sb("ssm_" + n, [128, 8], F32)
        lr = par_t[:, 0, :]; li = par_t[:, 1, :]
        def tt(out, a, b, op, eng="dve"):
            if eng == "dve":
                S.op("dve", lambda: V.tensor_tensor(out=out[0][:] if not isinstance(out[0], bass.AP) else out[0], in0=a[0], in1=b[0], op=op), reads=[a[1], b[1]], writes=[out[1]])
        P_ = lambda n: (w[n][0][:], w[n][1])
        S.op("act", lambda: A.activation(out=w["dt"][0][:], in_=par_t[:, 2, :], func=AF.Exp), reads=[par_b], writes=[w["dt"][1]])
        S.op("dve", lambda: V.tensor_tensor(out=w["t1"][0][:], in0=lr, in1=w["dt"][0][:], op=ALU.mult), reads=[par_b, w["dt"][1]], writes=[w["t1"][1]])
        S.op("act", lambda: A.activation(out=w["mag"][0][:], in_=w["t1"][0][:], func=AF.Exp), reads=[w["t1"][1]], writes=[w["mag"][1]])
        S.op("dve", lambda: V.tensor_tensor(out=w["th"][0][:], in0=li, in1=w["dt"][0][:], op=ALU.mult), reads=[par_b, w["dt"][1]], writes=[w["th"][1]])
        sincos_angle(S, nc, 128, 8, P_("th"), P_("k"), P_("r"), hp_t[:, 0:1], hp_b, P_("sn"), P_("cs"))
        S.op("dve", lambda: V.tensor_tensor(out=w["cs"][0][:], in0=w["cs"][0][:], in1=w["mag"][0][:], op=ALU.mult), reads=[w["cs"][1], w["mag"][1]], writes=[w["cs"][1]])
        S.op("dve", lambda: V.tensor_tensor(out=w["sn"][0][:], in0=w["sn"][0][:], in1=w["mag"][0][:], op=ALU.mult), reads=[w["sn"][1], w["mag"][1]], writes=[w["sn"][1]])
        S.op("dve", lambda: V.tensor_tensor(out=w["den"][0][:], in0=lr, in1=lr, op=ALU.mult), reads=[par_b], writes=[w["den"][1]])
        S.op("dve", lambda: V.tensor_tensor(out=w["t1"][0][:], in0=li, in1=li, op=ALU.mult), reads=[par_b], writes=[w["t1"][1]])
        S.op("dve", lambda: V.tensor_tensor(out=w["den"][0][:], in0=w["den"][0][:], in1=w["t1"][0][:], op=ALU.add), reads=[w["den"][1], w["t1"][1]], writes=[w["den"][1]])
        S.op("dve", lambda: V.reciprocal(w["den"][0][:], w["den"][0][:]), reads=[w["den"][1]], writes=[w["den"][1]])
        S.op("dve", lambda: V.tensor_scalar(out=w["t2"][0][:], in0=w["cs"][0][:], scalar1=-1.0, scalar2=None, op0=ALU.add), reads=[w["cs"][1]], writes=[w["t2"][1]])
        S.op("dve", lambda: V.tensor_tensor(out=w["fre"][0][:], in0=w["t2"][0][:], in1=lr, op=ALU.mult), reads=[w["t2"][1], par_b], writes=[w["fre"][1]])
        S.op("dve", lambda: V.tensor_tensor(out=w["t1"][0][:], in0=w["sn"][0][:], in1=li, op=ALU.mult), reads=[w["sn"][1], par_b], writes=[w["t1"][1]])
        S.op("dve", lambda: V.tensor_tensor(out=w["fre"][0][:], in0=w["fre"][0][:], in1=w["t1"][0][:], op=ALU.add), reads=[w["fre"][1], w["t1"][1]], writes=[w["fre"][1]])
        S.op("dve", lambda: V.tensor_tensor(out=w["fre"][0][:], in0=w["fre"][0][:], in1=w["den"][0][:], op=ALU.mult), reads=[w["fre"][1], w["den"][1]], writes=[w["fre"][1]])
        S.op("dve", lambda: V.tensor_tensor(out=w["fim"][0][:], in0=w["sn"][0][:], in1=lr, op=ALU.mult), reads=[w["sn"][1], par_b], writes=[w["fim"][1]])
        S.op("dve", lambda: V.tensor_tensor(out=w["t1"][0][:], in0=w["t2"][0][:], in1=li, op=ALU.mult), reads=[w["t2"][1], par_b], writes=[w["t1"][1]])
        S.op("dve", lambda: V.tensor_tensor(out=w["fim"][0][:], in0=w["fim"][0][:], in1=w["t1"][0][:], op=ALU.subtract), reads=[w["fim"][1], w["t1"][1]], writes=[w["fim"][1]])
        S.op("dve", lambda: V.tensor_tensor(out=w["fim"][0][:], in0=w["fim"][0][:], in1=w["den"][0][:], op=ALU.mult), reads=[w["fim"][1], w["den"][1]], writes=[w["fim"][1]])
        bre_t, bre_b = S.sb("ssm_bre", [64, 8, 16], F32); bim_t, bim_b = S.sb("ssm_bim", [64, 8, 16], F32)
        S.dma("sp", lambda: nc.sync.dma_start(out=bre_t[:].rearrange("p g h -> p (g h)"), in_=bre[:, :]), writes=[bre_b])
        S.dma("sp", lambda: nc.sync.dma_start(out=bim_t[:].rearrange("p g h -> p (g h)"), in_=bim[:, :]), writes=[bim_b])
        bb_t, bb_b = S.sb("ssm_bb", [64, 2, 8, 16], F32)
        tmp_t, tmp_b = S.sb("ssm_tmpb", [64, 8, 16], F32)
        fre_b3 = w["fre"][0][0:64, :].unsqueeze(2).to_broadcast([64, 8, 16])
        fim_b3 = w["fim"][0][0:64, :].unsqueeze(2).to_broadcast([64, 8, 16])
        S.op("dve", lambda: V.tensor_tensor(out=bb_t[:, 0], in0=bre_t[:], in1=fre_b3, op=ALU.mult), reads=[bre_b, w["fre"][1]], writes=[bb_b])
        S.op("dve", lambda: V.tensor_tensor(out=tmp_t[:], in0=bim_t[:], in1=fim_b3, op=ALU.mult), reads=[bim_b, w["fim"][1]], writes=[tmp_b])
        S.op("dve", lambda: V.tensor_tensor(out=bb_t[:, 0], in0=bb_t[:, 0], in1=tmp_t[:], op=ALU.subtract), reads=[bb_b, tmp_b], writes=[bb_b])
        S.op("dve", lambda: V.tensor_tensor(out=bb_t[:, 1], in0=bim_t[:], in1=fre_b3, op=ALU.mult), reads=[bim_b, w["fre"][1]], writes=[bb_b])
        S.op("dve", lambda: V.tensor_tensor(out=tmp_t[:], in0=bre_t[:], in1=fim_b3, op=ALU.mult), reads=[bre_b, w["fim"][1]], writes=[tmp_b])
        S.op("dve", lambda: V.tensor_tensor(out=bb_t[:, 1], in0=bb_t[:, 1], in1=tmp_t[:], op=ALU.add), reads=[bb_b, tmp_b], writes=[bb_b])
        pX = S.ps("ssm_pX", [128, 512], F32)
        bbT_t, bbT_b = S.sb("ssm_bbT", [128, 2, 64], F32)
        for i in range(2):
            S.op("pe", lambda: PE.transpose(pX[0][:, i * 64:(i + 1) * 64], bb_t[:, i].rearrange("p g h -> p (g h)"), idf_t[0:64, 0:64]), reads=[bb_b, idf_b], writes=[pX[1]])
        S.op("dve", lambda: V.tensor_copy(bbT_t[:].rearrange("p a b -> p (a b)"), pX[0][:, 0:128]), reads=[pX[1]], writes=[bbT_b])
        gm_t, gm_b = S.sb("ssm_gm", [128, 8], F32)
        S.op("pool", lambda: G.memset(gm_t[:], 1.0), writes=[gm_b])
        S.op("pool", lambda: G.affine_select(out=gm_t[:], in_=gm_t[:], pattern=[[-16, 8]], compare_op=ALU.is_ge, fill=0.0, base=0, channel_multiplier=1), reads=[gm_b], writes=[gm_b])
        S.op("pool", lambda: G.affine_select(out=gm_t[:], in_=gm_t[:], pattern=[[16, 8]], compare_op=ALU.is_ge, fill=0.0, base=15, channel_multiplier=-1), reads=[gm_b], writes=[gm_b])
        LB_t, LB_b = S.sb("ssm_LB", [128, 8, 2, 128], BF16)
        for g in range(8):
            for var in range(2):
                for half in range(2):
                    src = bbT_t[:, half if var == 0 else 1 - half, :]
                    S.op("dve", lambda: V.tensor_scalar(out=LB_t[:, g, var, half * 64:(half + 1) * 64], in0=src, scalar1=gm_t[:, g:g + 1], scalar2=None, op0=ALU.mult), reads=[bbT_b, gm_b], writes=[LB_b])
        cin_t, cin_b = S.sb("ssm_cin", [128, 2, 128], F32)
        S.dma("sp", lambda: nc.sync.dma_start(out=cin_t[:, 0, :], in_=cA[:, :]), writes=[cin_b])
        S.dma("sp", lambda: nc.sync.dma_start(out=cin_t[:, 1, :], in_=cB[:, :]), writes=[cin_b])
        for i in range(2):
            S.op("pe", lambda: PE.transpose(pX[0][:, 128 + i * 128:256 + i * 128], cin_t[:, i, :], idf_t[:]), reads=[cin_b, idf_b], writes=[pX[1]])
        WC_t, WC_b = S.sb("ssm_WC", [128, 8, 2, 128], BF16)
        S.op("pool", lambda: G.memset(WC_t[:], 0.0), writes=[WC_b])
        for g in range(8):
            cs_ = slice(g * 16, (g + 1) * 16)
            S.op("act", lambda: A.activation(out=WC_t[0:64, g, 0, cs_], in_=pX[0][0:64, 128 + g * 16:128 + (g + 1) * 16], func=AF.Copy, scale=1.0), reads=[pX[1]], writes=[WC_b])
            S.op("act", lambda: A.activation(out=WC_t[64:128, g, 0, cs_], in_=pX[0][64:128, 128 + g * 16:128 + (g + 1) * 16], func=AF.Copy, scale=-1.0), reads=[pX[1]], writes=[WC_b])
            S.op("act", lambda: A.activation(out=WC_t[:, g, 1, cs_], in_=pX[0][:, 256 + g * 16:256 + (g + 1) * 16], func=AF.Copy, scale=-1.0), reads=[pX[1]], writes=[WC_b])
        S.op("dve", lambda: V.tensor_scalar(out=w["thT"][0][:], in0=w["th"][0][:], scalar1=float(T), scalar2=None, op0=ALU.mult), reads=[w["th"][1]], writes=[w["thT"][1]])
        sincos_angle(S, nc, 128, 8, P_("thT"), P_("k"), P_("r"), hp_t[:, 0:1], hp_b, P_("sT"), P_("cT"))
        S.op("dve", lambda: V.tensor_copy(w["spm"][0][0:64, :], w["sT"][0][0:64, :]), reads=[w["sT"][1]], writes=[w["spm"][1]])
        S.op("dve", lambda: V.tensor_scalar(out=w["spm"][0][64:128, :], in0=w["sT"][0][64:128, :], scalar1=-1.0, scalar2=None, op0=ALU.mult), reads=[w["sT"][1]], writes=[w["spm"][1]])
        sw_t, sw_b = S.sb("ssm_sw", [128, 128], F32)
        S.op("pool", lambda: G.memset(sw_t[:], 0.0), writes=[sw_b])
        S.op("pool", lambda: G.affine_select(out=sw_t[:], in_=sw_t[:], pattern=[[1, 128]], compare_op=ALU.not_equal, fill=1.0, base=-64, channel_multiplier=-1), reads=[sw_b], writes=[sw_b])
        S.op("pool", lambda: G.affine_select(out=sw_t[:], in_=sw_t[:], pattern=[[1, 128]], compare_op=ALU.not_equal, fill=1.0, base=64, channel_multiplier=-1), reads=[sw_b], writes=[sw_b])
        ROT_t, ROT_b = S.sb("ssm_ROT", [128, 8, 128], F32)
        for g in range(8):
            S.op("dve", lambda: V.tensor_scalar(out=ROT_t[:, g, :], in0=idf_t[:], scalar1=w["cT"][0][:, g:g + 1], scalar2=None, op0=ALU.mult), reads=[idf_b, w["cT"][1]], writes=[ROT_b])
            S.op("dve", lambda: V.scalar_tensor_tensor(out=ROT_t[:, g, :], in0=sw_t[:], scalar=w["spm"][0][:, g:g + 1], in1=ROT_t[:, g, :], op0=ALU.mult, op1=ALU.add), reads=[sw_b, w["spm"][1], ROT_b], writes=[ROT_b])
        COS_t, COS_b = S.sb("ssm_COS", [128, 8, T], F32)
        SIN_t, SIN_b = S.sb("ssm_SIN", [128, 8, T], F32)
        RHO_t, RHO_b = S.sb("ssm_RHO", [128, 8, T], F32)
        SPM_t, SPM_b = S.sb("ssm_SPM", [128, 8, T], F32)
        io_t, io_b = S.sb("ssm_iota", [128, T], F32)
        S.op("pool", lambda: G.iota(io_t[:], pattern=[[1, T]], base=0, channel_multiplier=0, allow_small_or_imprecise_dtypes=True), writes=[io_b])
        an = S.sb("ssm_an", [128, T], F32); kk = S.sb("ssm_kk", [128, T], F32); rr = S.sb("ssm_rr", [128, T], F32)
        for g in range(8):
            S.op("dve", lambda: V.tensor_scalar(out=an[0][:], in0=io_t[:], scalar1=w["th"][0][:, g:g + 1], scalar2=None, op0=ALU.mult), reads=[io_b, w["th"][1]], writes=[an[1]])
            sincos_angle(S, nc, 128, T, (an[0][:], an[1]), (kk[0][:], kk[1]), (rr[0][:], rr[1]), hp_t[:, 0:1], hp_b, (SIN_t[:, g, :], SIN_b), (COS_t[:, g, :], COS_b))
            S.op("dve", lambda: V.tensor_copy(RHO_t[:, g, :], w["mag"][0][:, g:g + 1].to_broadcast([128, T])), reads=[w["mag"][1]], writes=[RHO_b])
            S.op("dve", lambda: V.tensor_copy(SPM_t[0:64, g, :], SIN_t[0:64, g, :]), reads=[SIN_b], writes=[SPM_b])
            S.op("dve", lambda: V.tensor_scalar(out=SPM_t[64:128, g, :], in0=SIN_t[64:128, g, :], scalar1=-1.0, scalar2=None, op0=ALU.mult), reads=[SIN_b], writes=[SPM_b])
        u32 = [S.sb(f"ssm_u32_{i}", [128, T], F32) for i in range(2)]
        u16 = [S.sb(f"ssm_u16_{i}", [128, T], BF16) for i in range(2)]
        t1b = [S.sb(f"ssm_t1_{i}", [128, T], BF16) for i in range(2)]
        t2b = [S.sb(f"ssm_t2_{i}", [128, T], BF16) for i in range(2)]
        rb = [S.sb(f"ssm_r_{i}", [128, T], F32) for i in range(2)]
        m1b = [S.sb(f"ssm_m1_{i}", [128, T], BF16) for i in range(2)]
        m2b = [S.sb(f"ssm_m2_{i}", [128, T], BF16) for i in range(2)]
        rl_t, rl_b_ = S.sb("ssm_rlast", [128, 8], F32)
        rlb = [S.buf(f"rl{g}") for g in range(8)]
        ini = [S.sb(f"ssm_ini{g}", [128, 1], F32) for g in range(8)]
        yb = [S.sb(f"ssm_y_{i}", [128, T], F32) for i in range(2)]
        gt = [S.sb(f"ssm_g_{i}", [128, T], F32) for i in range(2)]
        pA = [S.ps(f"ssm_pA{i}", [128, T], F32) for i in range(2)]
        pB = [S.ps(f"ssm_pB{i}", [128, T], F32) for i in range(2)]
        pY = [S.ps(f"ssm_pY{i}", [128, T], F32) for i in range(1)]
        bpb = [S.ps(f"ssm_pC{i}", [128, T], F32) for i in range(2)]
        pI = pX
        for g in range(8):
            S.op("pool", lambda: G.memset(ini[g][0][:], 0.0), writes=[ini[g][1]])
        seq = [(n, g) for n in range(NBLK) for g in range(8)]

        t1c = [S.sb(f"ssm_t1c_{i}", [128, T], BF16) for i in range(3)]
        t2c = [S.sb(f"ssm_t2c_{i}", [128, T], BF16) for i in range(3)]

        def stageA1(k):
            n, g = seq[k]
            (u_t, u_b) = u32[n % 2]; (uh_t, uh_b) = u16[n % 2]
            if g == 0:
                S.dma("sp", lambda: nc.sync.dma_start(out=u_t[:], in_=usT[:, n * T:(n + 1) * T]), writes=[u_b])
                S.op("act", lambda: A.copy(uh_t[:], u_t[:]), reads=[u_b], writes=[uh_b])
            (pa_t, pa_b) = pA[k % 2]; (pb_t, pb_b) = pB[k % 2]
            (t1_t, t1_b) = t1c[k % 3]; (t2_t, t2_b) = t2c[k % 3]
            S.op("pe", lambda: PE.matmul(pa_t[:], lhsT=LB_t[:, g, 0, :], rhs=uh_t[:], start=True, stop=True), reads=[LB_b, uh_b], writes=[pa_b])
            S.op("pe", lambda: PE.matmul(pb_t[:], lhsT=LB_t[:, g, 1, :], rhs=uh_t[:], start=True, stop=True), reads=[LB_b, uh_b], writes=[pb_b])
            S.op("dve", lambda: V.tensor_tensor(out=t1_t[:], in0=pa_t[:], in1=COS_t[:, g, :], op=ALU.mult), reads=[pa_b, COS_b], writes=[t1_b])
            S.op("dve", lambda: V.tensor_tensor(out=t2_t[:], in0=pb_t[:], in1=SPM_t[:, g, :], op=ALU.mult), reads=[pb_b, SPM_b], writes=[t2_b])

        def stageA2(k):
            (t1_t, t1_b) = t1c[k % 3]; (t2_t, t2_b) = t2c[k % 3]
            (bp_t, bp_b) = bpb[k % 2]
            S.op("pe", lambda: PE.matmul(bp_t[:], lhsT=idt[:], rhs=t1_t[:], start=True, stop=False), reads=[idb, t1_b], writes=[bp_b])
            S.op("pe", lambda: PE.matmul(bp_t[:], lhsT=idt[:], rhs=t2_t[:], start=False, stop=True), reads=[idb, t2_b], writes=[bp_b])

        def stageB(k):
            n, g = seq[k]
            (bp_t, bp_b) = bpb[k % 2]; (r_t, r_b) = rb[k % 2]
            (m1_t, m1_b) = m1b[k % 2]; (m2_t, m2_b) = m2b[k % 2]
            if n > 0:
                S.op("pe", lambda: PE.matmul(pI[0][:, 384 + g:385 + g], lhsT=ROT_t[:, g, :], rhs=rl_t[:, g:g + 1], start=True, stop=True), reads=[ROT_b, rlb[g]], writes=[pI[1]])
                S.op("act", lambda: A.copy(ini[g][0][:], pI[0][:, 384 + g:385 + g]), reads=[pI[1]], writes=[ini[g][1]])
            S.op("dve", lambda: V.tensor_tensor_scan(out=r_t[:], data0=RHO_t[:, g, :], data1=bp_t[:], initial=ini[g][0][:, 0:1], op0=ALU.mult, op1=ALU.add), reads=[RHO_b, bp_b, ini[g][1]], writes=[r_b])
            S.op("act", lambda: A.copy(rl_t[:, g:g + 1], r_t[:, T - 1:T]), reads=[r_b], writes=[rlb[g]])
            S.op("pool", lambda: G.tensor_tensor(out=m1_t[:], in0=r_t[:], in1=COS_t[:, g, :], op=ALU.mult), reads=[r_b, COS_b], writes=[m1_b])
            S.op("pool", lambda: G.tensor_tensor(out=m2_t[:], in0=r_t[:], in1=SIN_t[:, g, :], op=ALU.mult), reads=[r_b, SIN_b], writes=[m2_b])

        def stageC(k):
            n, g = seq[k]
            (u_t, u_b) = u32[n % 2]
            (py_t, py_b) = pY[0]
            (m1_t, m1_b) = m1b[k % 2]; (m2_t, m2_b) = m2b[k % 2]
            S.op("pe", lambda: PE.matmul(py_t[:], lhsT=WC_t[:, g, 0, :], rhs=m1_t[:], start=(g == 0), stop=False), reads=[WC_b, m1_b], writes=[py_b])
            S.op("pe", lambda: PE.matmul(py_t[:], lhsT=WC_t[:, g, 1, :], rhs=m2_t[:], start=False, stop=(g == 7)), reads=[WC_b, m2_b], writes=[py_b])
            if g == 7:
                (y_t, y_b) = yb[n % 2]; (g_t, g_b) = gt[n % 2]
                S.op("dve", lambda: V.scalar_tensor_tensor(out=y_t[:], in0=u_t[:], scalar=d_t[:, 0:1], in1=py_t[:], op0=ALU.mult, op1=ALU.add), reads=[u_b, d_b, py_b], writes=[y_b])
                S.op("dve", lambda: V.tensor_tensor(out=g_t[:], in0=y_t[:], in1=y_t[:], op=ALU.mult), reads=[y_b], writes=[g_b])
                S.op("dve", lambda: V.tensor_scalar(out=g_t[:], in0=g_t[:], scalar1=0.044715, scalar2=1.0, op0=ALU.mult, op1=ALU.add), reads=[g_b], writes=[g_b])
                S.op("dve", lambda: V.tensor_tensor(out=g_t[:], in0=g_t[:], in1=y_t[:], op=ALU.mult), reads=[g_b, y_b], writes=[g_b])
                S.op("act", lambda: A.activation(out=g_t[:], in_=g_t[:], func=AF.Sigmoid, scale=GELU_C), reads=[g_b], writes=[g_b])
                S.op("dve", lambda: V.tensor_tensor(out=y_t[:], in0=y_t[:], in1=g_t[:], op=ALU.mult), reads=[g_b, y_b], writes=[y_b])
                S.dma("sp", lambda: nc.sync.dma_start(out=ysT[:, n * T:(n + 1) * T], in_=y_t[:]), reads=[y_b], writes=[ysT_b])

        NK = len(seq)
        for it in range(NK + 3):
            if it < NK:
                stageA1(it)
            if 0 <= it - 1 < NK:
                stageA2(it - 1)
            if 0 <= it - 2 < NK:
                stageB(it - 2)
            if 0 <= it - 3 < NK:
                stageC(it - 3)
        S.barrier()
        S.es = es_save


def emit_pool(S, nc, L, upT, pw, psc, pe_, pinvw, pfix, ypT, ypT_b, CH=2048):
    NCH = L // CH
    with ExitStack() as es2:
        es_save = S.es
        S.es = es2
        V = nc.vector; G = nc.gpsimd; A = nc.scalar; PE = nc.tensor
        pw32_t, pw32_b = S.sb("pl_w32", [128, 128], F32)
        pw_t, pw_b = S.sb("pl_w", [128, 128], BF16)
        S.dma("sp", lambda: nc.sync.dma_start(out=pw32_t[:], in_=pw[:, :]), writes=[pw32_b])
        S.op("dve", lambda: V.tensor_copy(pw_t[:], pw32_t[:]), reads=[pw32_b], writes=[pw_b])
        c_t, c_b = S.sb("pl_c", [128, 32], F32)
        S.dma("sp", lambda: nc.sync.dma_start(out=c_t[:, 0:1], in_=psc[:, :]), writes=[c_b])
        S.dma("sp", lambda: nc.sync.dma_start(out=c_t[:, 1:2], in_=pinvw[:, :]), writes=[c_b])
        S.dma("sp", lambda: nc.sync.dma_start(out=c_t[:, 2:6], in_=pe_[:, :]), writes=[c_b])
        S.dma("sp", lambda: nc.sync.dma_start(out=c_t[:, 16:32], in_=pfix[:, :]), writes=[c_b])
        e0 = [S.sb(f"pl_e0_{i}", [128, CH + 16], F32) for i in range(2)]
        e1 = S.sb("pl_e1", [128, CH + 16], F32); e2 = S.sb("pl_e2", [128, CH + 16], F32)
        dT = S.sb("pl_dT", [128, CH], BF16)
        tf = S.sb("pl_tf", [128, 16], F32)
        stg = [S.sb(f"pl_stg{i}", [128, 512], F32) for i in range(2)]
        pP = [S.ps(f"pl_p{i}", [128, 512], F32) for i in range(2)]
        nb = 0
        for ch in range(NCH):
            (x_t, x_b) = e0[ch % 2]
            S.dma("sp", lambda: nc.sync.dma_start(out=x_t[:, 16:], in_=upT[:, ch * CH:(ch + 1) * CH]), writes=[x_b])
            if ch == 0:
                S.op("pool", lambda: G.memset(x_t[:, 0:16], 0.0), writes=[x_b])
            else:
                (xp_t, xp_b) = e0[(ch - 1) % 2]
                S.op("pool", lambda: G.tensor_copy(x_t[:, 0:16], xp_t[:, CH:CH + 16]), reads=[xp_b], writes=[x_b])
            src = (x_t, x_b)
            for lvl, sft in enumerate((1, 2, 4, 8)):
                dst = e1 if lvl % 2 == 0 else e2
                S.op("dve", lambda: V.scalar_tensor_tensor(out=dst[0][:, sft:], in0=src[0][:, 0:CH + 16 - sft], scalar=c_t[:, 2 + lvl:3 + lvl], in1=src[0][:, sft:], op0=ALU.mult, op1=ALU.add), reads=[src[1], c_b], writes=[dst[1]])
                S.op("pool", lambda: G.tensor_copy(dst[0][:, 0:sft], src[0][:, 0:sft]), reads=[src[1]], writes=[dst[1]])
                src = dst
            S.op("dve", lambda: V.scalar_tensor_tensor(out=dT[0][:], in0=src[0][:, 16:], scalar=c_t[:, 1:2], in1=x_t[:, 16:], op0=ALU.mult, op1=ALU.subtract), reads=[src[1], c_b, x_b], writes=[dT[1]])
            if ch == 0:
                S.op("dve", lambda: V.tensor_tensor(out=tf[0][:], in0=src[0][:, 16:32], in1=c_t[:, 16:32], op=ALU.mult), reads=[src[1], c_b], writes=[tf[1]])
                S.op("dve", lambda: V.tensor_tensor(out=dT[0][:, 0:16], in0=tf[0][:], in1=x_t[:, 16:32], op=ALU.subtract), reads=[tf[1], x_b, dT[1]], writes=[dT[1]])
            for q in range(CH // 512):
                (p_t, p_b) = pP[nb % 2]; (s_t, s_b) = stg[nb % 2]; nb += 1
                S.op("pe", lambda: PE.matmul(p_t[:], lhsT=pw_t[:], rhs=dT[0][:, q * 512:(q + 1) * 512], start=True, stop=True), reads=[pw_b, dT[1]], writes=[p_b])
                S.op("act", lambda: A.activation(out=s_t[:], in_=p_t[:], func=AF.Copy, scale=c_t[:, 0:1]), reads=[p_b, c_b], writes=[s_b])
                S.dma("sp", lambda: nc.sync.dma_start(out=ypT[:, ch * CH + q * 512: ch * CH + (q + 1) * 512], in_=s_t[:]), reads=[s_b], writes=[ypT_b])
        S.barrier()
        S.es = es_save


def build_B(L, parts=("att", "ssm", "pool")):
    nc = bass.Bass("TRN2", target_bir_lowering=False)
    di = lambda n, s, d=F32: nc.dram_tensor(n, list(s), d, kind="ExternalInput").ap()
    do = lambda n, s, d=F32: nc.dram_tensor(n, list(s), d, kind="ExternalOutput").ap()
    QT = di("QT", [2, 192, L], BF16); KT = di("KT", [2, 128, L], BF16)
    kpeT = di("kpeT", [64, L], BF16); V = di("V", [L, 2, 128], BF16)
    usT = di("usT", [128, L]); lamre = di("lamre", [128, 8]); lamim = di("lamim", [128, 8]); logdt = di("logdt", [128, 8])
    bre = di("bre", [64, 128]); bim = di("bim", [64, 128]); cA = di("cA", [128, 128]); cB = di("cB", [128, 128]); dsk = di("dsk", [128, 1])
    upT = di("upT", [128, L]); pw = di("pw", [128, 128]); psc = di("psc", [128, 1]); pe_ = di("pe", [128, 4]); pinvw = di("pinvw", [128, 1]); pfix = di("pfix", [128, 16])
    ymT = do("ymT", [256, L]); ysT = do("ysT", [128, L]); ypT = do("ypT", [128, L])
    with ExitStack() as es:
        S = Sched(nc, es)
        idt, idb = S.sb("idt", [128, 128], BF16)
        make_ident(S, nc, idt, idb)
        ones_t, ones_b = S.sb("ones", [128, 128], BF16)
        S.op("pool", lambda: nc.gpsimd.memset(ones_t[:], 1.0), writes=[ones_b])
        ymT_b = S.buf("ymT"); ysT_b = S.buf("ysT")
        ypT_b = S.buf("ypT")
        outs = [ymT_b, ysT_b, ypT_b]
        if "pool" in parts:
            emit_pool(S, nc, L, upT, pw, psc, pe_, pinvw, pfix, ypT, ypT_b, CH=min(2048, L))
        if "ssm" in parts:
            emit_ssm(S, nc, L, usT, lamre, lamim, logdt, bre, bim, cA, cB, dsk, ysT, ysT_b, idt, idb)
        if "att" in parts:
            emit_attention(S, nc, L, QT, KT, kpeT, V, ymT, ymT_b, idt, idb, ones_t, ones_b)
        S.finish(outs)
        print("B ninstr", S.ninstr)
    return nc

import math

D = 2048
EPS = 1e-6
NE = 32
TS = 256


def build_C(NTOK, CAP, final, stage=99):
    nc = bass.Bass("TRN2", target_bir_lowering=False)
    NT = NTOK // 128
    NS = NTOK // TS
    NTILE = (2 * NTOK) // 128 + NE
    BIG = 1.0e6
    di = lambda n, s, d=F32: nc.dram_tensor(n, list(s), d, kind="ExternalInput").ap()
    do = lambda n, s, d=F32: nc.dram_tensor(n, list(s), d, kind="ExternalOutput").ap()
    x = di("x", [NTOK, D]); ypT = di("ypT", [512, NTOK]); ysT = di("ysT", [512, NTOK]); ymT = di("ymT", [1024, NTOK])
    cT = di("cT", [128, 16]); wada = di("wadaC", [D, 8192]); bada = di("badaC", [1, 8192])
    wglu = di("wglu", [512, 512]); gn = di("gn", [128, 16]); wout = di("wout", [D, D]); g2 = di("g2", [1, D])
    wr = di("wr", [D, 36]); br = di("br", [1, 36])
    wg = di("wg", [NE, D, 512]); wu = di("wu", [NE, D, 512]); wd = di("wd", [NE, 512, D])
    gfin = di("gfin", [1, D])
    xo = do("xo", [NTOK, D])
    cnt_o = do("cnt", [128, NE])
    Xg = nc.dram_tensor("Xg", [NTILE * 128, D], BF16, kind="Internal").ap()
    Yg = nc.dram_tensor("Yg", [NTILE * 128, D], F32, kind="Internal").ap()
    H2 = nc.dram_tensor("H2", [NTOK, D], BF16, kind="Internal").ap()
    x1 = nc.dram_tensor("x1", [NTOK, D], F32, kind="Internal").ap()
    V = nc.vector; G = nc.gpsimd; A = nc.scalar; PE = nc.tensor
    with ExitStack() as es:
        S = Sched(nc, es)
        xo_b = S.buf("xo"); Xg_b = S.buf("Xg"); Yg_b = S.buf("Yg"); x1_b = S.buf("x1"); H2_b = S.buf("H2")
        idt, idb = S.sb("idt", [128, 128], BF16)
        make_ident(S, nc, idt, idb)
        ones_t, ones_b = S.sb("ones", [128, 128], BF16)
        S.op("pool", lambda: G.memset(ones_t[:], 1.0), writes=[ones_b])
        onesf_t, onesf_b = S.sb("onesf", [128, 128], F32)
        S.op("pool", lambda: G.memset(onesf_t[:], 1.0), writes=[onesf_b])
        tri_t, tri_b = S.sb("tri", [128, 128], F32)
        S.op("pool", lambda: G.memset(tri_t[:], 1.0), writes=[tri_b])
        S.op("pool", lambda: G.affine_select(out=tri_t[:], in_=tri_t[:], pattern=[[1, 128]], compare_op=ALU.is_gt, fill=0.0, base=0, channel_multiplier=-1), reads=[tri_b], writes=[tri_b])
        cst_t, cst_b = S.sb("cst", [128, 4], F32)
        S.op("pool", lambda: G.memset(cst_t[:, 0:1], EPS), writes=[cst_b])
        idxW_t, idxW_b = S.sb("idxW", [128, 4, NTILE], I32)
        mcum_t, mcum_b = S.sb("mcum", [128, NE], F32)
        Mall_t, Mall_b = S.sb("Mall", [128, 2, NT, NE], F32)
        rt_t, rt_b = S.sb("rt", [128, NT, 4], F32)
        sl_t, sl_b = S.sb("sl", [128, NT, 2], I32)
        gF_t, gF_b = S.sb("gF", [128, D], F32)
        with ExitStack() as es2:
            S.es = es2
            gA_t, gA_b = S.sb("gA", [128, D], F32)
            gm2_t, gm2_b = S.sb("gm2", [128, D], F32)
            shF_t, shF_b = S.sb("shF", [128, D], F32)
            xt = [S.sb(f"xt{i}", [128, D], F32) for i in range(2)]
            xn = [S.sb(f"xn{i}", [128, D], F32) for i in range(2)]
            pZ = [S.ps(f"pZ{i}", [128, 512], F32) for i in range(2)]
            pG = S.ps("pG", [128, 512], F32)
            pSS = S.ps("pSS", [128, 512], F32)
            pT = [S.ps(f"pT{i}", [128, 1024], BF16) for i in range(2)]
            pR = S.ps("pR", [128, 512], F32)
            cB_t, cB_b = S.sb("cB", [128, 16, 128], F32)
            cT_t, cT_b = S.sb("cTt", [128, 16], F32)
            S.dma("sp", lambda: nc.sync.dma_start(out=cT_t[:], in_=cT[:, :]), writes=[cT_b])
            S.op("dve", lambda: V.tensor_copy(cB_t[:], cT_t[:].unsqueeze(2).to_broadcast([128, 16, 128])), reads=[cT_b], writes=[cB_b])
            tg = [(gA_t, gA_b), (shF_t, shF_b), (gm2_t, gm2_b), (gF_t, gF_b)]
            for i, (t_, b_) in enumerate(tg):
                S.dma("sp", lambda: nc.sync.dma_start(out=t_[:], in_=bada[0:1, i * D:(i + 1) * D].partition_broadcast(128)), writes=[b_])
            wada_v = wada.rearrange("(c p) n -> p c n", p=128)
            nsl = 0
            for blk in range(16):
                pz_t, pz_b = pZ[blk % 2]
                for q4 in range(4):
                    (w_t, w_b) = xt[nsl % 2]; nsl += 1
                    wv_ = w_t[:].rearrange("p (c n) -> p c n", c=4)
                    S.dma("sp", lambda: nc.sync.dma_start(out=wv_, in_=wada_v[:, q4 * 4:(q4 + 1) * 4, blk * 512:(blk + 1) * 512]), writes=[w_b])
                    for cc in range(4):
                        c = q4 * 4 + cc
                        S.op("pe", lambda: PE.matmul(pz_t[:], lhsT=cB_t[:, c, :], rhs=wv_[:, cc, :], start=(c == 0), stop=(c == 15)), reads=[cB_b, w_b], writes=[pz_b])
                tgt_t, tgt_b = tg[blk // 4]
                cs = slice((blk % 4) * 512, (blk % 4 + 1) * 512)
                S.op("dve", lambda: V.tensor_tensor(out=tgt_t[:, cs], in0=pz_t[:], in1=tgt_t[:, cs], op=ALU.add), reads=[pz_b, tgt_b], writes=[tgt_b])
            (g_t, g_b) = xt[nsl % 2]; nsl += 1
            S.dma("sp", lambda: nc.sync.dma_start(out=g_t[:], in_=g2[0:1, :].partition_broadcast(128)), writes=[g_b])
            S.op("dve", lambda: V.scalar_tensor_tensor(out=gm2_t[:], in0=gm2_t[:], scalar=1.0, in1=g_t[:], op0=ALU.add, op1=ALU.mult), reads=[gm2_b, g_b], writes=[gm2_b])
            if stage < 1:
                S.finish([gA_b, gm2_b, shF_b, gF_b]); return nc
            wout_t, wout_b = S.sb("wout", [128, 16, D], BF16)
            wout_v = wout.rearrange("(c p) n -> p c n", p=128)
            for c in range(16):
                S.dma("pool", lambda: G.dma_start(out=wout_t[:, c, :], in_=wout_v[:, c, :]), writes=[wout_b])
            wglu_t, wglu_b = S.sb("wglu", [128, 4, 512], BF16)
            S.dma("pool", lambda: G.dma_start(out=wglu_t[:], in_=wglu.rearrange("(c p) n -> p c n", p=128)), writes=[wglu_b])
            wr_t, wr_b = S.sb("wrt", [128, 16, 36], BF16)
            S.dma("pool", lambda: G.dma_start(out=wr_t[:], in_=wr.rearrange("(c p) n -> p c n", p=128)), writes=[wr_b])
            br_t, br_b = S.sb("brt", [128, 36], F32)
            S.dma("sp", lambda: nc.sync.dma_start(out=br_t[:], in_=br[0:1, :].partition_broadcast(128)), writes=[br_b])
            gn_t, gn_b = S.sb("gnt", [128, 16], F32)
            S.dma("sp", lambda: nc.sync.dma_start(out=gn_t[:], in_=gn[:, :]), writes=[gn_b])
            yr_t, yr_b = S.sb("yr", [128, 16, TS], F32)
            yrb = [S.buf(f"yr{i}") for i in range(16)]
            ysb_t, ysb_b = S.sb("ysb", [128, 4, TS], BF16)
            sq_t, sq_b = S.sb("sq", [128, D], BF16)
            sg_t, sg_b = S.sb("sg", [128, TS], F32)
            rstd = [S.sb(f"rstd{i}", [128, TS], F32) for i in range(3)]
            yn_t, yn_b = S.sb("yn", [128, 16, TS], BF16)
            tmpo = [S.sb(f"tmpo{i}", [128, 512], F32) for i in range(2)]
            h2 = [S.sb(f"h2_{i}", [128, D], BF16) for i in range(2)]
            h2T_t, h2T_b = S.sb("h2T", [128, 16, 128], BF16)
            st = [S.sb(f"st{i}", [128, 1], F32) for i in range(4)]
            H2_v = H2.rearrange("(n p) d -> n p d", p=128)
            S.op("pool", lambda: G.memset(mcum_t[:], 0.0), writes=[mcum_b])
            R = {}
            for n_, w_ in (("lg", 36), ("gmax", 1), ("ngmax", 1), ("goh", 4), ("gex", 4), ("gsum", 1), ("gw", 1), ("el", 8), ("m1", 1), ("oh1", 8),
                           ("el2", 8), ("m2", 1), ("oh2", 8), ("dd", 1), ("s1", 1), ("s2", 1), ("M1", NE), ("M2", NE), ("M", NE), ("po", NE), ("jk", NE)):
                R[n_] = S.sb("r_" + n_, [128, w_], F32)
            srcs = [(ypT, 0), (ypT, 1), (ypT, 2), (ypT, 3), (ysT, 0), (ysT, 1), (ysT, 2), (ysT, 3)] + [(ymT, i) for i in range(8)]
            grp_of = [0] * 4 + [1] * 4 + [2] * 8
            gdim = [512.0, 512.0, 1024.0]
            x_v = x.rearrange("(n p) d -> n p d", p=128)
            x1_v = x1.rearrange("(n p) d -> n p d", p=128)
            nst = 0
            ntile = 0
            npz = 0
            for s in range(NS):
                t0 = s * TS
                for i, (src, bi) in enumerate(srcs):
                    S.dma("sp", lambda: nc.sync.dma_start(out=yr_t[:, i, :], in_=src[bi * 128:(bi + 1) * 128, t0:t0 + TS]), writes=[yrb[i]], track=yrb[i])
                for i in range(4):
                    S.op("act", lambda: A.copy(ysb_t[:, i, :], yr_t[:, 4 + i, :]), reads=[yrb[4 + i]], writes=[ysb_b])
                for j in range(4):
                    for i in range(4):
                        S.op("pe", lambda: PE.matmul(pG[0][:, 0:TS], lhsT=wglu_t[:, i, j * 128:(j + 1) * 128], rhs=ysb_t[:, i, :], start=(i == 0), stop=(i == 3)), reads=[wglu_b, ysb_b], writes=[pG[1]])
                    S.op("act", lambda: A.activation(out=sg_t[:], in_=pG[0][:, 0:TS], func=AF.Sigmoid), reads=[pG[1]], writes=[sg_b])
                    S.op("dve", lambda: V.tensor_tensor(out=yr_t[:, 4 + j, :], in0=yr_t[:, 4 + j, :], in1=sg_t[:], op=ALU.mult), reads=[yrb[4 + j], sg_b], writes=[yrb[4 + j]])
                for gi, (b0, nb) in enumerate(((0, 4), (4, 4), (8, 8))):
                    for i in range(nb):
                        S.op("act", lambda: A.activation(out=sq_t[:, i * TS:(i + 1) * TS], in_=yr_t[:, b0 + i, :], func=AF.Square), reads=[yrb[b0 + i]], writes=[sq_b])
                    for i in range(nb):
                        S.op("pe", lambda: PE.matmul(pSS[0][:, 0:TS], lhsT=ones_t[:], rhs=sq_t[:, i * TS:(i + 1) * TS], start=(i == 0), stop=(i == nb - 1)), reads=[ones_b, sq_b], writes=[pSS[1]])
                    (rs_t, rs_b) = rstd[gi]
                    S.op("act", lambda: A.activation(out=rs_t[:], in_=pSS[0][:, 0:TS], func=AF.Sqrt, scale=1.0 / gdim[gi], bias=cst_t[:, 0:1]), reads=[pSS[1], cst_b], writes=[rs_b])
                    S.op("dve", lambda: V.reciprocal(rs_t[:], rs_t[:]), reads=[rs_b], writes=[rs_b])
                    for i in range(nb):
                        blk = b0 + i
                        eng = "dve" if i % 2 == 0 else "dve"
                        S.op("dve", lambda: V.scalar_tensor_tensor(out=yn_t[:, blk, :], in0=yr_t[:, blk, :], scalar=gn_t[:, blk:blk + 1], in1=rs_t[:], op0=ALU.mult, op1=ALU.mult), reads=[yrb[blk], gn_b, rs_b], writes=[yn_b])
                for j in range(TS // 128):
                    ti = s * (TS // 128) + j
                    (x_t, x_b) = xt[nsl % 2]; nsl += 1
                    (n_t, n_b) = xn[ti % 2]
                    (h_t, h_b) = h2[ti % 2]
                    S.dma("sp", lambda: nc.sync.dma_start(out=x_t[:], in_=x_v[ti]), writes=[x_b])
                    for q in range(4):
                        (pz_t, pz_b) = pZ[npz % 2]; (to_t, to_b) = tmpo[npz % 2]; npz += 1
                        cs = slice(q * 512, (q + 1) * 512)
                        for c in range(16):
                            S.op("pe", lambda: PE.matmul(pz_t[:], lhsT=yn_t[:, c, j * 128:(j + 1) * 128], rhs=wout_t[:, c, cs], start=(c == 0), stop=(c == 15)), reads=[yn_b, wout_b], writes=[pz_b])
                        S.op("dve", lambda: V.tensor_tensor(out=to_t[:], in0=pz_t[:], in1=gA_t[:, cs], op=ALU.mult), reads=[pz_b, gA_b], writes=[to_b])
                        S.op("pool", lambda: G.tensor_tensor(out=n_t[:, cs], in0=to_t[:], in1=x_t[:, cs], op=ALU.add), reads=[to_b, x_b], writes=[n_b])
                    S.dma("sp", lambda: nc.sync.dma_start(out=x1_v[ti], in_=n_t[:]), reads=[n_b], writes=[x1_b])
                    (ss_t, ss_b) = st[nst % 4]; nst += 1
                    (rr_t, rr_b) = st[nst % 4]; nst += 1
                    S.op("act", lambda: A.activation(out=sq_t[:], in_=n_t[:], func=AF.Square, accum_out=ss_t[:]), reads=[n_b], writes=[sq_b, ss_b])
                    S.op("act", lambda: A.activation(out=rr_t[:], in_=ss_t[:], func=AF.Sqrt, scale=1.0 / D, bias=cst_t[:, 0:1]), reads=[ss_b, cst_b], writes=[rr_b])
                    S.op("dve", lambda: V.reciprocal(rr_t[:], rr_t[:]), reads=[rr_b], writes=[rr_b])
                    S.op("dve", lambda: V.scalar_tensor_tensor(out=x_t[:], in0=n_t[:], scalar=rr_t[:, 0:1], in1=gm2_t[:], op0=ALU.mult, op1=ALU.mult), reads=[n_b, rr_b, gm2_b], writes=[x_b])
                    S.op("pool", lambda: G.tensor_tensor(out=h_t[:], in0=x_t[:], in1=shF_t[:], op=ALU.add), reads=[x_b, shF_b], writes=[h_b])
                    for half in range(2):
                        (p_t, p_b) = pT[half]
                        for cc in range(8):
                            c = half * 8 + cc
                            S.op("pe", lambda: PE.transpose(p_t[:, cc * 128:(cc + 1) * 128], h_t[:, c * 128:(c + 1) * 128], idt[:]), reads=[h_b, idb], writes=[p_b])
                        dst = h2T_t[:, half * 8:(half + 1) * 8, :]
                        src_ = p_t[:].rearrange("p (c n) -> p c n", c=8)
                        if half == 0:
                            S.op("act", lambda: A.copy(dst, src_), reads=[p_b], writes=[h2T_b])
                        else:
                            S.op("dve", lambda: V.tensor_copy(dst, src_), reads=[p_b], writes=[h2T_b])
                    for c in range(16):
                        S.op("pe", lambda: PE.matmul(pR[0][:, 0:36], lhsT=h2T_t[:, c, :], rhs=wr_t[:, c, :], start=(c == 0), stop=(c == 15)), reads=[h2T_b, wr_b], writes=[pR[1]])
                    r = lambda n_: R[n_][0]
                    rb_ = lambda n_: R[n_][1]
                    def dv(fn, rd, wrn):
                        S.op("dve", fn, reads=[rb_(n_) if isinstance(n_, str) else n_ for n_ in rd], writes=[rb_(n_) if isinstance(n_, str) else n_ for n_ in wrn])
                    dv(lambda: V.tensor_tensor(out=r("lg")[:], in0=pR[0][:, 0:36], in1=br_t[:], op=ALU.add), [pR[1], br_b], ["lg"])
                    dv(lambda: V.reduce_max(out=r("gmax")[:], in_=r("lg")[:, 0:4], axis=AX.X), ["lg"], ["gmax"])
                    dv(lambda: V.tensor_scalar(out=r("goh")[:], in0=r("lg")[:, 0:4], scalar1=r("gmax")[:, 0:1], scalar2=None, op0=ALU.is_equal), ["lg", "gmax"], ["goh"])
                    dv(lambda: V.tensor_scalar(out=r("ngmax")[:], in0=r("gmax")[:], scalar1=-1.0, scalar2=None, op0=ALU.mult), ["gmax"], ["ngmax"])
                    S.op("act", lambda: A.activation(out=r("gex")[:], in_=r("lg")[:, 0:4], func=AF.Exp, bias=r("ngmax")[:, 0:1], accum_out=r("gsum")[:]), reads=[rb_("lg"), rb_("ngmax")], writes=[rb_("gex"), rb_("gsum")])
                    dv(lambda: V.reciprocal(r("gw")[:], r("gsum")[:]), ["gsum"], ["gw"])
                    dv(lambda: V.tensor_scalar(out=r("el")[:], in0=r("lg")[:, 4:12], scalar1=r("goh")[:, 0:1], scalar2=None, op0=ALU.mult), ["lg", "goh"], ["el"])
                    for g in range(1, 4):
                        dv(lambda: V.scalar_tensor_tensor(out=r("el")[:], in0=r("lg")[:, 4 + 8 * g:12 + 8 * g], scalar=r("goh")[:, g:g + 1], in1=r("el")[:], op0=ALU.mult, op1=ALU.add), ["lg", "goh", "el"], ["el"])
                    dv(lambda: V.reduce_max(out=r("m1")[:], in_=r("el")[:], axis=AX.X), ["el"], ["m1"])
                    dv(lambda: V.tensor_scalar(out=r("oh1")[:], in0=r("el")[:], scalar1=r("m1")[:, 0:1], scalar2=None, op0=ALU.is_equal), ["el", "m1"], ["oh1"])
                    dv(lambda: V.scalar_tensor_tensor(out=r("el2")[:], in0=r("oh1")[:], scalar=-1e30, in1=r("el")[:], op0=ALU.mult, op1=ALU.add), ["oh1", "el"], ["el2"])
                    dv(lambda: V.reduce_max(out=r("m2")[:], in_=r("el2")[:], axis=AX.X), ["el2"], ["m2"])
                    dv(lambda: V.tensor_scalar(out=r("oh2")[:], in0=r("el2")[:], scalar1=r("m2")[:, 0:1], scalar2=None, op0=ALU.is_equal), ["el2", "m2"], ["oh2"])
                    dv(lambda: V.tensor_tensor(out=r("dd")[:], in0=r("m2")[:], in1=r("m1")[:], op=ALU.subtract), ["m1", "m2"], ["dd"])
                    S.op("act", lambda: A.activation(out=r("s1")[:], in_=r("dd")[:], func=AF.Sigmoid, scale=-1.0), reads=[rb_("dd")], writes=[rb_("s1")])
                    S.op("act", lambda: A.activation(out=r("s2")[:], in_=r("dd")[:], func=AF.Sigmoid, scale=1.0), reads=[rb_("dd")], writes=[rb_("s2")])
                    dv(lambda: V.tensor_tensor(out=rt_t[:, ti, 2:3], in0=r("s1")[:], in1=r("gw")[:], op=ALU.mult), ["s1", "gw"], [rt_b])
                    dv(lambda: V.tensor_tensor(out=rt_t[:, ti, 3:4], in0=r("s2")[:], in1=r("gw")[:], op=ALU.mult), ["s2", "gw"], [rt_b])
                    gohb = r("goh")[:].unsqueeze(2).to_broadcast([128, 4, 8])
                    for (mn, ohn) in (("M1", "oh1"), ("M2", "oh2")):
                        ohb = r(ohn)[:].unsqueeze(1).to_broadcast([128, 4, 8])
                        dv(lambda: V.tensor_tensor(out=r(mn)[:].rearrange("p (g e) -> p g e", g=4), in0=gohb, in1=ohb, op=ALU.mult), ["goh", ohn], [mn])
                    dv(lambda: V.tensor_tensor(out=r("M")[:], in0=r("M1")[:], in1=r("M2")[:], op=ALU.add), ["M1", "M2"], ["M"])
                    S.op("pe", lambda: PE.matmul(pR[0][:, 64:64 + NE], lhsT=tri_t[:], rhs=r("M")[:], start=True, stop=False), reads=[tri_b, rb_("M")], writes=[pR[1]])
                    S.op("pe", lambda: PE.matmul(pR[0][:, 64:64 + NE], lhsT=onesf_t[:], rhs=mcum_t[:], start=False, stop=True), reads=[onesf_b, mcum_b], writes=[pR[1]])
                    dv(lambda: V.tensor_copy(r("po")[:], pR[0][:, 64:64 + NE]), [pR[1]], ["po"])
                    dv(lambda: V.tensor_tensor(out=mcum_t[:], in0=mcum_t[:], in1=r("M")[:], op=ALU.add), [mcum_b, "M"], [mcum_b])
                    for k_, mn in enumerate(("M1", "M2")):
                        dv(lambda: V.tensor_tensor(out=r("jk")[:], in0=r(mn)[:], in1=r("po")[:], op=ALU.mult), [mn, "po"], ["jk"])
                        dv(lambda: V.reduce_sum(out=rt_t[:, ti, k_:k_ + 1], in_=r("jk")[:], axis=AX.X), ["jk"], [rt_b])
                        dv(lambda: V.tensor_copy(Mall_t[:, k_, ti, :], r(mn)[:]), [mn], [Mall_b])
                    S.dma("sp", lambda: nc.sync.dma_start(out=H2_v[ti], in_=h_t[:]), reads=[h_b], writes=[H2_b])
                    ntile += 1
            cnt_b = S.buf("cnt")
            S.dma("sp", lambda: nc.sync.dma_start(out=cnt_o[:, :], in_=mcum_t[:]), reads=[mcum_b], writes=[cnt_b])
            S.barrier()
            S.es = es
        with ExitStack() as es2b:
            S.es = es2b
            H2_v = H2.rearrange("(n p) d -> n p d", p=128)
            pR = S.ps("pR2", [128, 512], F32)
            h2 = [S.sb(f"h2b_{i}", [128, D], BF16) for i in range(2)]
            T = {}
            for n_, shp in (("cnt", [128, NE]), ("ntl", [128, NE]), ("pend", [128, NE]), ("pst", [128, NE]), ("one", [128, NE]), ("thr", [128, 64]),
                            ("cmp", [128, NE, 64]), ("tau", [128, NTILE]), ("texp", [128, NTILE]), ("chg", [128, NTILE]), ("idf", [128, NTILE]),
                            ("pid", [128, 1]), ("sadd", [128, 2, NT])):
                T[n_] = S.sb("t_" + n_, shp, F32)
            tq = lambda n_: T[n_][0]
            tb = lambda n_: T[n_][1]
            cmp2_t, cmp2_b = S.sb("t_cmp2", [128, NTILE, NE], F32)
            mp_t, mp_b = S.sb("t_mp", [128, 2, NT, NE], F32)
            S.op("pe", lambda: PE.matmul(pR[0][:, 128:128 + NE], lhsT=onesf_t[:], rhs=mcum_t[:], start=True, stop=True), reads=[onesf_b, mcum_b], writes=[pR[1]])
            S.op("dve", lambda: V.tensor_copy(tq("cnt")[:], pR[0][:, 128:128 + NE]), reads=[pR[1]], writes=[tb("cnt")])
            S.op("pool", lambda: G.iota(tq("thr")[:], pattern=[[128, 64]], base=0, channel_multiplier=0, allow_small_or_imprecise_dtypes=True), writes=[tb("thr")])
            S.op("pool", lambda: G.iota(tq("tau")[:], pattern=[[128, NTILE]], base=0, channel_multiplier=0, allow_small_or_imprecise_dtypes=True), writes=[tb("tau")])
            S.op("pool", lambda: G.iota(tq("pid")[:], pattern=[[0, 1]], base=0, channel_multiplier=1, allow_small_or_imprecise_dtypes=True), writes=[tb("pid")])
            S.op("pool", lambda: G.memset(tq("one")[:], 1.0), writes=[tb("one")])
            S.op("dve", lambda: V.tensor_tensor(out=tq("cmp")[:], in0=tq("cnt")[:].unsqueeze(2).to_broadcast([128, NE, 64]), in1=tq("thr")[:].unsqueeze(1).to_broadcast([128, NE, 64]), op=ALU.is_gt), reads=[tb("cnt"), tb("thr")], writes=[tb("cmp")])
            S.op("dve", lambda: V.tensor_reduce(out=tq("ntl")[:], in_=tq("cmp")[:], op=ALU.add, axis=AX.X), reads=[tb("cmp")], writes=[tb("ntl")])
            S.op("dve", lambda: V.tensor_scalar(out=tq("ntl")[:], in0=tq("ntl")[:], scalar1=128.0, scalar2=None, op0=ALU.mult), reads=[tb("ntl")], writes=[tb("ntl")])
            S.op("dve", lambda: V.tensor_tensor_scan(out=tq("pend")[:], data0=tq("one")[:], data1=tq("ntl")[:], initial=0.0, op0=ALU.mult, op1=ALU.add), reads=[tb("one"), tb("ntl")], writes=[tb("pend")])
            S.op("dve", lambda: V.tensor_tensor(out=tq("pst")[:], in0=tq("pend")[:], in1=tq("ntl")[:], op=ALU.subtract), reads=[tb("pend"), tb("ntl")], writes=[tb("pst")])
            S.op("dve", lambda: V.tensor_tensor(out=mp_t[:].rearrange("p k n e -> p (k n) e"), in0=Mall_t[:].rearrange("p k n e -> p (k n) e"), in1=tq("pst")[:].unsqueeze(1).to_broadcast([128, 2 * NT, NE]), op=ALU.mult), reads=[Mall_b, tb("pst")], writes=[mp_b])
            S.op("dve", lambda: V.tensor_reduce(out=tq("sadd")[:].rearrange("p k n -> p (k n)"), in_=mp_t[:].rearrange("p k n e -> p (k n) e"), op=ALU.add, axis=AX.X), reads=[mp_b], writes=[tb("sadd")])
            for k_ in range(2):
                S.op("dve", lambda: V.tensor_tensor(out=rt_t[:, :, k_], in0=rt_t[:, :, k_], in1=tq("sadd")[:, k_, :], op=ALU.add), reads=[rt_b, tb("sadd")], writes=[rt_b])
            S.op("dve", lambda: V.tensor_copy(sl_t[:], rt_t[:, :, 0:2]), reads=[rt_b], writes=[sl_b])
            S.op("dve", lambda: V.tensor_tensor(out=cmp2_t[:], in0=tq("tau")[:].unsqueeze(2).to_broadcast([128, NTILE, NE]), in1=tq("pend")[:].unsqueeze(1).to_broadcast([128, NTILE, NE]), op=ALU.is_ge), reads=[tb("tau"), tb("pend")], writes=[cmp2_b])
            S.op("dve", lambda: V.tensor_reduce(out=tq("texp")[:], in_=cmp2_t[:], op=ALU.add, axis=AX.X), reads=[cmp2_b], writes=[tb("texp")])
            S.op("dve", lambda: V.tensor_scalar(out=tq("texp")[:], in0=tq("texp")[:], scalar1=float(NE - 1), scalar2=None, op0=ALU.min), reads=[tb("texp")], writes=[tb("texp")])
            S.op("pool", lambda: G.memset(tq("chg")[:, 0:1], 1.0), writes=[tb("chg")])
            S.op("dve", lambda: V.tensor_tensor(out=tq("chg")[:, 1:NTILE], in0=tq("texp")[:, 1:NTILE], in1=tq("texp")[:, 0:NTILE - 1], op=ALU.not_equal), reads=[tb("texp"), tb("chg")], writes=[tb("chg")])
            S.op("dve", lambda: V.tensor_scalar(out=tq("idf")[:], in0=tq("texp")[:], scalar1=128.0, scalar2=tq("pid")[:, 0:1], op0=ALU.mult, op1=ALU.add), reads=[tb("texp"), tb("pid")], writes=[tb("idf")])
            S.op("dve", lambda: V.scalar_tensor_tensor(out=tq("idf")[:], in0=tq("idf")[:], scalar=-BIG, in1=tq("chg")[:], op0=ALU.add, op1=ALU.mult), reads=[tb("idf"), tb("chg")], writes=[tb("idf")])
            S.op("dve", lambda: V.tensor_scalar(out=tq("idf")[:], in0=tq("idf")[:], scalar1=BIG, scalar2=None, op0=ALU.add), reads=[tb("idf")], writes=[tb("idf")])
            for a_ in range(4):
                S.op("dve", lambda: V.tensor_scalar(out=tq("tau")[:], in0=tq("idf")[:], scalar1=4.0, scalar2=float(a_), op0=ALU.mult, op1=ALU.add), reads=[tb("idf"), idxW_b], writes=[tb("tau")])
                S.op("dve", lambda: V.tensor_copy(idxW_t[:, a_, :], tq("tau")[:]), reads=[tb("tau")], writes=[idxW_b])
            for ti in range(NT):
                (h_t, h_b) = h2[ti % 2]
                S.dma("sp", lambda: nc.sync.dma_start(out=h_t[:], in_=H2_v[ti]), reads=[H2_b], writes=[h_b])
                for k_ in range(2):
                    S.dma("pool", lambda: G.indirect_dma_start(out=Xg[:, :], out_offset=bass.IndirectOffsetOnAxis(ap=sl_t[:, ti, k_:k_ + 1], axis=0), in_=h_t[:, :], in_offset=None), reads=[h_b, sl_b], writes=[Xg_b], track=h_b)
            S.barrier()
            S.es = es
        if stage < 2:
            S.finish([Xg_b, x1_b]); return nc
        with ExitStack() as es3:
            S.es = es3
            w1_t, w1_b = S.sb("w1", [128, 16 * 512], BF16)
            w3_t, w3_b = S.sb("w3", [128, 16 * 512], BF16)
            w2_t, w2_b = S.sb("w2", [128, 4 * D], BF16)
            xr = [S.sb(f"xr{i}", [128, D], BF16) for i in range(2)]
            XT = [S.sb(f"XT{i}", [128, 16, 128], BF16) for i in range(2)]
            sl_ = [S.sb(f"silu{i}", [128, 512], F32) for i in range(2)]
            ac = [S.sb(f"act{i}", [128, 512], BF16) for i in range(2)]
            aT = [S.sb(f"aT{i}", [128, 4, 128], BF16) for i in range(2)]
            yo = [S.sb(f"yo{i}", [128, D], F32) for i in range(2)]
            pT = [S.ps(f"eT{i}", [128, 1024], BF16) for i in range(2)]
            pH1 = [S.ps(f"eH1_{i}", [128, 512], F32) for i in range(2)]
            pH3 = [S.ps(f"eH3_{i}", [128, 512], F32) for i in range(2)]
            pY = [S.ps(f"eY{i}", [128, 512], F32) for i in range(2)]
            Xg_v = Xg.rearrange("(n p) d -> n p d", p=128)
            Yg_v = Yg.rearrange("(n p) d -> n p d", p=128)
            wg_v = wg.rearrange("e (p a c) n -> (e p a) (c n)", a=4, c=4)
            wu_v = wu.rearrange("e (p a c) n -> (e p a) (c n)", a=4, c=4)
            wd_v = wd.rearrange("e (p j) n -> (e p j) n", j=4)
            w1v = w1_t[:].rearrange("p (c n) -> p c n", c=16)
            w3v = w3_t[:].rearrange("p (c n) -> p c n", c=16)
            w2v = w2_t[:].rearrange("p (j n) -> p j n", j=4)
            ny = 0
            breg4 = G.to_reg(NE * 128 * 4 - 1)
            for tau in range(NTILE):
                (r_t, r_b) = xr[tau % 2]; (x_t, x_b) = XT[tau % 2]
                (h1_t, h1_b) = pH1[tau % 2]; (h3_t, h3_b) = pH3[tau % 2]
                (s_t, s_b) = sl_[tau % 2]; (a_t, a_b) = ac[tau % 2]; (at_t, at_b) = aT[tau % 2]
                (y_t, y_b) = yo[tau % 2]
                S.dma("sp", lambda: nc.sync.dma_start(out=r_t[:], in_=Xg_v[tau]), reads=[Xg_b], writes=[r_b])
                for (wt_, wb_, src_) in ((w1_t, w1_b, wg_v), (w3_t, w3_b, wu_v), (w2_t, w2_b, wd_v)):
                    for a_ in range(4):
                        S.dma("pool", lambda: G.indirect_dma_start(out=wt_[:, a_ * 2048:(a_ + 1) * 2048], out_offset=None, in_=src_[:, :], in_offset=bass.IndirectOffsetOnAxis(ap=idxW_t[:, a_, tau:tau + 1], axis=0), bounds_check=breg4, oob_is_err=False), reads=[idxW_b], writes=[wb_], track=wb_)
                rv = r_t[:].rearrange("s (p c) -> s c p", c=16)
                for half in range(2):
                    (p_t, p_b) = pT[half]
                    for cc in range(8):
                        c = half * 8 + cc
                        S.op("pe", lambda: PE.transpose(p_t[:, cc * 128:(cc + 1) * 128], rv[:, c, :], idt[:]), reads=[r_b, idb], writes=[p_b])
                    dst = x_t[:, half * 8:(half + 1) * 8, :]
                    src2 = p_t[:].rearrange("p (c n) -> p c n", c=8)
                    if half == 0:
                        S.op("act", lambda: A.copy(dst, src2), reads=[p_b], writes=[x_b])
                    else:
                        S.op("dve", lambda: V.tensor_copy(dst, src2), reads=[p_b], writes=[x_b])
                for c in range(16):
                    S.op("pe", lambda: PE.matmul(h1_t[:], lhsT=x_t[:, c, :], rhs=w1v[:, c, :], start=(c == 0), stop=(c == 15)), reads=[x_b, w1_b], writes=[h1_b])
                for c in range(16):
                    S.op("pe", lambda: PE.matmul(h3_t[:], lhsT=x_t[:, c, :], rhs=w3v[:, c, :], start=(c == 0), stop=(c == 15)), reads=[x_b, w3_b], writes=[h3_b])
                S.op("act", lambda: A.activation(out=s_t[:], in_=h1_t[:], func=AF.Silu), reads=[h1_b], writes=[s_b])
                S.op("dve", lambda: V.tensor_tensor(out=a_t[:], in0=h3_t[:], in1=s_t[:], op=ALU.mult), reads=[h3_b, s_b], writes=[a_b])
                av = a_t[:].rearrange("s (p j) -> s j p", j=4)
                (p_t, p_b) = pT[0]
                for j in range(4):
                    S.op("pe", lambda: PE.transpose(p_t[:, j * 128:(j + 1) * 128], av[:, j, :], idt[:]), reads=[a_b, idb], writes=[p_b])
                S.op("act", lambda: A.copy(at_t[:], p_t[:, 0:512].rearrange("p (j n) -> p j n", j=4)), reads=[p_b], writes=[at_b])
                for q in range(4):
                    (py_t, py_b) = pY[ny % 2]; ny += 1
                    for j in range(4):
                        S.op("pe", lambda: PE.matmul(py_t[:], lhsT=at_t[:, j, :], rhs=w2v[:, j, q * 512:(q + 1) * 512], start=(j == 0), stop=(j == 3)), reads=[at_b, w2_b], writes=[py_b])
                    if q % 2 == 0:
                        S.op("act", lambda: A.copy(y_t[:, q * 512:(q + 1) * 512], py_t[:]), reads=[py_b], writes=[y_b])
                    else:
                        S.op("dve", lambda: V.tensor_copy(y_t[:, q * 512:(q + 1) * 512], py_t[:]), reads=[py_b], writes=[y_b])
                S.dma("sp", lambda: nc.sync.dma_start(out=Yg_v[tau], in_=y_t[:]), reads=[y_b], writes=[Yg_b])
            S.barrier()
            S.es = es
        if stage < 3:
            S.finish([Yg_b, x1_b]); return nc
        with ExitStack() as es4:
            S.es = es4
            y1 = [S.sb(f"y1_{i}", [128, D], F32) for i in range(2)]
            y2 = [S.sb(f"y2_{i}", [128, D], F32) for i in range(2)]
            xb = [S.sb(f"xb{i}", [128, D], F32) for i in range(2)]
            jq_t, jq_b = S.sb("jq", [128, D], BF16)
            st = [S.sb(f"fst{i}", [128, 1], F32) for i in range(4)]
            gfin_t, gfin_b = S.sb("gfin", [128, D], F32)
            if final:
                S.dma("sp", lambda: nc.sync.dma_start(out=gfin_t[:], in_=gfin[0:1, :].partition_broadcast(128)), writes=[gfin_b])
            x1_v = x1.rearrange("(n p) d -> n p d", p=128)
            xo_v = xo.rearrange("(n p) d -> n p d", p=128)
            nst = 0
            for ti in range(NT):
                (a_t, a_b) = y1[ti % 2]; (b_t, b_b) = y2[ti % 2]; (x_t, x_b) = xb[ti % 2]
                S.dma("pool", lambda: G.indirect_dma_start(out=a_t[:, :], out_offset=None, in_=Yg[:, :], in_offset=bass.IndirectOffsetOnAxis(ap=sl_t[:, ti, 0:1], axis=0)), reads=[Yg_b, sl_b], writes=[a_b])
                S.dma("pool", lambda: G.indirect_dma_start(out=b_t[:, :], out_offset=None, in_=Yg[:, :], in_offset=bass.IndirectOffsetOnAxis(ap=sl_t[:, ti, 1:2], axis=0)), reads=[Yg_b, sl_b], writes=[b_b])
                S.dma("sp", lambda: nc.sync.dma_start(out=x_t[:], in_=x1_v[ti]), reads=[x1_b], writes=[x_b])
                S.op("dve", lambda: V.tensor_scalar(out=a_t[:], in0=a_t[:], scalar1=rt_t[:, ti, 2:3], scalar2=None, op0=ALU.mult), reads=[a_b, rt_b], writes=[a_b])
                S.op("dve", lambda: V.scalar_tensor_tensor(out=a_t[:], in0=b_t[:], scalar=rt_t[:, ti, 3:4], in1=a_t[:], op0=ALU.mult, op1=ALU.add), reads=[a_b, b_b, rt_b], writes=[a_b])
                S.op("pool", lambda: G.tensor_tensor(out=a_t[:], in0=a_t[:], in1=gF_t[:], op=ALU.mult), reads=[a_b, gF_b], writes=[a_b])
                S.op("pool", lambda: G.tensor_tensor(out=x_t[:], in0=x_t[:], in1=a_t[:], op=ALU.add), reads=[a_b, x_b], writes=[x_b])
                if final:
                    (ss_t, ss_b) = st[nst % 4]; nst += 1
                    (rr_t, rr_b) = st[nst % 4]; nst += 1
                    S.op("act", lambda: A.activation(out=jq_t[:], in_=x_t[:], func=AF.Square, accum_out=ss_t[:]), reads=[x_b], writes=[jq_b, ss_b])
                    S.op("act", lambda: A.activation(out=rr_t[:], in_=ss_t[:], func=AF.Sqrt, scale=1.0 / D, bias=cst_t[:, 0:1]), reads=[ss_b, cst_b], writes=[rr_b])
                    S.op("dve", lambda: V.reciprocal(rr_t[:], rr_t[:]), reads=[rr_b], writes=[rr_b])
                    S.op("dve", lambda: V.scalar_tensor_tensor(out=x_t[:], in0=x_t[:], scalar=rr_t[:, 0:1], in1=gfin_t[:], op0=ALU.mult, op1=ALU.mult), reads=[x_b, rr_b, gfin_b], writes=[x_b])
                S.dma("sp", lambda: nc.sync.dma_start(out=xo_v[ti], in_=x_t[:]), reads=[x_b], writes=[xo_b])
            S.finish([xo_b])
            S.barrier()
            S.es = es
        print("C ninstr", S.ninstr)
    return nc

from concourse.bass_utils import run_bass_kernel_spmd
import os

B_, L_, NCORE = 2, 16384, 8
NTOK_ = 4096
CAP_ = 768
POOL_WINDOWS_ = (2, 4, 8, 16)
_prog_cache = {}


def _prog(key, fn):
    if key not in _prog_cache:
        _prog_cache[key] = fn()
    return _prog_cache[key]


def _c(a):
    return np.ascontiguousarray(a)


def _inv_freq():
    return np.power(np.float32(10000.0), -np.arange(0, 64, 2, dtype=np.float32) / np.float32(64)).astype(np.float32)


def _run(nc, in_maps):
    res = run_bass_kernel_spmd(nc, in_maps, core_ids=list(range(NCORE)))
    return res.results


def kernel(x, c, positions, w_ada, b_ada, norm1_g, w_in, pool_w, pool_scale,
           ssm_lam_re, ssm_lam_im, ssm_log_dt, ssm_b_re, ssm_b_im, ssm_c_re, ssm_c_im,
           ssm_d, ssm_w_glu, q_norm_g, kv_norm_g, w_uq, w_ukv, out_norm_g, w_out,
           norm2_g, router_w_group, router_b_group, router_w_expert, router_b_expert,
           w_gate, w_up, w_down, final_g):
    f32 = np.float32
    x = np.asarray(x, f32); c = np.asarray(c, f32); positions = np.asarray(positions, np.int32)
    depth = w_ada.shape[0]
    invf = _inv_freq()
    invf2 = np.concatenate([invf, invf]).reshape(64, 1).astype(f32)
    xs = [_c(x[k // 4, (k % 4) * NTOK_:(k % 4 + 1) * NTOK_, :]) for k in range(NCORE)]
    cTs = [_c(c[b].reshape(16, 128).T) for b in range(B_)]
    ncA = _prog("A", lambda: build_A(NTOK_))
    ncB = _prog("B", lambda: build_B(L_))
    for l in range(depth):
        wl = lambda a: np.asarray(a[l], f32)
        wada_l = wl(w_ada); bada_l = wl(b_ada)
        win_l = wl(w_in); wuq_l = wl(w_uq); wukv_l = wl(w_ukv)
        shared = {
            "wadaA": _c(wada_l[:, 0:4096]), "badaA": _c(bada_l[0:4096].reshape(1, -1)),
            "g1": wl(norm1_g).reshape(1, -1), "w_in": win_l,
            "w_in_sw": _c(np.concatenate([win_l[:, 1824:1856], win_l[:, 1792:1824]], axis=1)),
            "gq": _c(wl(q_norm_g).reshape(4, 128).T), "gkv": _c(wl(kv_norm_g).reshape(2, 128).T),
            "wq_n": _c(wuq_l[:, :, :128].reshape(512, 1024)),
            "wq_p": _c(wuq_l[:, :, 128:].reshape(512, 512)),
            "wq_ps": _c(np.concatenate([wuq_l[:, :, 160:192], wuq_l[:, :, 128:160]], axis=2).reshape(512, 512)),
            "wk": _c(wukv_l[:, :, :128].reshape(256, 1024)), "wv": _c(wukv_l[:, :, 128:].reshape(256, 1024)),
            "invf": invf2,
        }
        in_maps = []
        for k in range(NCORE):
            b, q = k // 4, k % 4
            m = dict(shared)
            m["x"] = xs[k]; m["cT"] = cTs[b]
            m["pos"] = _c(positions[b, q * NTOK_:(q + 1) * NTOK_].reshape(1, -1))
            in_maps.append(m)
        ra = _run(ncA, in_maps)
        cat = lambda name, b, ax: np.concatenate([ra[b * 4 + q][name] for q in range(4)], axis=ax)
        lam_re = wl(ssm_lam_re); lam_im = wl(ssm_lam_im); log_dt = wl(ssm_log_dt)
        b_re = wl(ssm_b_re); b_im = wl(ssm_b_im); c_re = wl(ssm_c_re); c_im = wl(ssm_c_im)
        dsk = wl(ssm_d); pw = wl(pool_w); psc = wl(pool_scale)
        in_maps = [None] * NCORE
        for b in range(B_):
            upT = cat("upT", b, 1); usT = cat("usT", b, 1); QT = cat("QT", b, 2); KT = cat("KT", b, 2)
            kpeT = cat("kpeT", b, 1); Vv = cat("V", b, 0).reshape(L_, 8, 128)
            for r in range(4):
                gs = slice(8 * r, 8 * r + 8)
                w = POOL_WINDOWS_[r]
                e = np.array([1.0 if (1 << kk) < w else 0.0 for kk in range(4)], f32)
                fix = np.array([1.0 / min(t + 1, w) for t in range(16)], f32)
                in_maps[b * 4 + r] = {
                    "QT": _c(QT[2 * r:2 * r + 2]), "KT": _c(KT[2 * r:2 * r + 2]), "kpeT": kpeT, "V": _c(Vv[:, 2 * r:2 * r + 2, :]),
                    "usT": _c(usT[128 * r:128 * (r + 1)]),
                    "lamre": _c(np.concatenate([lam_re[gs].T, lam_re[gs].T], 0)), "lamim": _c(np.concatenate([lam_im[gs].T, lam_im[gs].T], 0)),
                    "logdt": _c(np.broadcast_to(log_dt[gs][None, :], (128, 8))),
                    "bre": _c(b_re[gs].transpose(1, 0, 2).reshape(64, 128)), "bim": _c(b_im[gs].transpose(1, 0, 2).reshape(64, 128)),
                    "cA": _c(np.concatenate([c_re[gs], c_im[gs]], -1).reshape(128, 128)),
                    "cB": _c(np.concatenate([c_im[gs], c_re[gs]], -1).reshape(128, 128)),
                    "dsk": _c(dsk[128 * r:128 * (r + 1)].reshape(128, 1)),
                    "upT": _c(upT[128 * r:128 * (r + 1)]), "pw": _c(pw[r]), "psc": _c(psc[128 * r:128 * (r + 1)].reshape(128, 1)),
                    "pe": _c(np.broadcast_to(e, (128, 4))), "pinvw": np.full((128, 1), 1.0 / w, f32),
                    "pfix": _c(np.broadcast_to(fix, (128, 16))),
                }
        del ra
        rb = _run(ncB, in_maps)
        final = (l == depth - 1)
        ncC = _prog(("C", final), lambda: build_C(NTOK_, CAP_, final))
        sharedC = {
            "wadaC": _c(wada_l[:, 4096:]), "badaC": _c(bada_l[4096:].reshape(1, -1)),
            "wglu": wl(ssm_w_glu), "gn": _c(wl(out_norm_g).reshape(16, 128).T), "wout": wl(w_out),
            "g2": wl(norm2_g).reshape(1, -1),
            "wr": _c(np.concatenate([wl(router_w_group), wl(router_w_expert)], 1)),
            "br": _c(np.concatenate([wl(router_b_group), wl(router_b_expert)]).reshape(1, 36)),
            "wg": wl(w_gate), "wu": wl(w_up), "wd": wl(w_down), "gfin": np.asarray(final_g, f32).reshape(1, -1),
        }
        in_maps = []
        for k in range(NCORE):
            b, q = k // 4, k % 4
            ts = slice(q * NTOK_, (q + 1) * NTOK_)
            m = dict(sharedC)
            m["x"] = xs[k]; m["cT"] = cTs[b]
            m["ypT"] = _c(np.concatenate([rb[b * 4 + r]["ypT"][:, ts] for r in range(4)], 0))
            m["ysT"] = _c(np.concatenate([rb[b * 4 + r]["ysT"][:, ts] for r in range(4)], 0))
            m["ymT"] = _c(np.concatenate([rb[b * 4 + r]["ymT"][:, ts] for r in range(4)], 0))
            in_maps.append(m)
        del rb
        rc = _run(ncC, in_maps)
        xs = [np.asarray(rc[k]["xo"], f32) for k in range(NCORE)]
        if os.environ.get("KDEBUG"):
            cc = np.stack([np.asarray(rc[k]["cnt"]).sum(0) for k in range(NCORE)])
            print("KDEBUG layer", l, "expert counts per core: max", cc.max(), "min", cc.min(), "mean", cc.mean(), flush=True)
        del rc
    out = np.stack([np.concatenate(xs[b * 4:(b + 1) * 4], axis=0) for b in range(B_)], axis=0)
    return out.astype(f32)
```

```python
import numpy as np
import concourse.bass as bass
import concourse.mybir as mybir
from contextlib import ExitStack

F32 = mybir.dt.float32
BF16 = mybir.dt.bfloat16
I32 = mybir.dt.int32
U32 = mybir.dt.uint32
AF = mybir.ActivationFunctionType
ALU = mybir.AluOpType
AX = mybir.AxisListType

SEM_LIMIT = 30000


class Buf:
    __slots__ = ("name", "last_write", "reads", "dsem", "dcount", "t", "psum")

    def __init__(self, name, t=None):
        self.name = name
        self.last_write = None
        self.reads = []
        self.dsem = None
        self.dcount = 0
        self.t = t
        self.psum = False


class Sched:
    def __init__(self, nc, es: ExitStack):
        self.nc = nc
        self.es = es
        self.es_top = es
        self.eng = {"pe": nc.tensor, "dve": nc.vector, "act": nc.scalar,
                    "pool": nc.gpsimd, "sp": nc.sync}
        self.sem = {}
        self.cnt = {}
        self.epoch = {}
        for k in self.eng:
            self.epoch[k] = 0
            self.sem[k] = es.enter_context(nc.semaphore(f"s_{k}_0"))
            self.cnt[k] = 0
        self.waited = {k: {} for k in self.eng}
        self.semobj = {}
        self.nbuf = 0
        self.ninstr = 0
        self.alldma = {}

    def sb(self, name, shape, dt):
        t = self.es.enter_context(self.nc.sbuf_tensor("sb_" + name, list(shape), dt))
        return t, Buf(name, t)

    def ps(self, name, shape, dt=F32):
        t = self.es.enter_context(self.nc.psum_tensor("ps_" + name, list(shape), dt))
        b = Buf(name, t)
        b.psum = True
        return t, b

    def buf(self, name):
        return Buf(name)

    def _wait(self, e, deps):
        best = {}
        for d in deps:
            if d is None:
                continue
            sem, val, en = d
            k = id(sem)
            if k not in best or best[k][1] < val:
                best[k] = (sem, val, en)
        for k, (sem, val, en) in best.items():
            if self.waited[e].get(k, 0) >= val:
                continue
            self.eng[e].wait_ge(sem, val)
            self.waited[e][k] = val
            self.ninstr += 1

    def _gather(self, e, reads, writes):
        deps = []
        for b in reads:
            lw = b.last_write
            if lw is not None:
                if not (e == "pe" and lw[2] == "pe"):
                    deps.append(lw)
            if b.psum:
                for r in b.reads:
                    if r[2] != e:
                        deps.append(r)
        for b in writes:
            lw = b.last_write
            if lw is not None and (lw[2] != e or e == "dma"):
                deps.append(lw)
            for r in b.reads:
                if r[2] != e or e == "dma":
                    deps.append(r)
        return deps

    def _record(self, dep, reads, writes):
        for b in reads:
            b.reads.append(dep)
            if len(b.reads) > 64:
                best = {}
                for d in b.reads:
                    k = id(d[0])
                    if k not in best or best[k][1] < d[1]:
                        best[k] = d
                b.reads = list(best.values())
        for b in writes:
            b.last_write = dep
            b.reads = []

    def op(self, e, fn, reads=(), writes=()):
        deps = self._gather(e, reads, writes)
        self._wait(e, deps)
        if self.cnt[e] >= SEM_LIMIT:
            self.epoch[e] += 1
            self.sem[e] = self.es_top.enter_context(self.nc.semaphore(f"s_{e}_{self.epoch[e]}"))
            self.cnt[e] = 0
        ins = fn()
        self.cnt[e] += 1
        ins.then_inc(self.sem[e], 1)
        dep = (self.sem[e], self.cnt[e], e)
        self._record(dep, reads, writes)
        self.ninstr += 1
        return ins

    def dma(self, q, fn, reads=(), writes=(), track=None):
        if track is None:
            track = writes[0] if (writes and writes[0].t is not None) else reads[0]
        deps = self._gather("dma", reads, writes)
        self._wait(q, deps)
        if track.dsem is None or track.dcount >= SEM_LIMIT:
            self.nbuf += 1
            track.dsem = self.es_top.enter_context(self.nc.semaphore(f"d_{self.nbuf}"))
            track.dcount = 0
        ins = fn()
        track.dcount += 16
        ins.then_inc(track.dsem, 16)
        dep = (track.dsem, track.dcount, "dma")
        self.alldma[id(track.dsem)] = dep
        self._record(dep, reads, writes)
        self.ninstr += 1
        return ins

    def finish(self, bufs, e="sp"):
        deps = []
        for b in bufs:
            if b.last_write is not None:
                deps.append(b.last_write)
            deps.extend(b.reads)
        deps.extend(self.alldma.values())
        self._wait(e, deps)

    def barrier(self):
        deps = []
        for e in ("pe", "dve", "act", "pool"):
            if self.cnt[e] > 0:
                deps.append((self.sem[e], self.cnt[e], e))
        dd = list(self.alldma.values())
        for e in ("pe", "dve", "act", "pool", "sp"):
            self._wait(e, [d for d in deps if d[2] != e] + dd)
        self.alldma = {}

import math

D = 2048
EPS = 1e-6
TWO_PI = 2.0 * math.pi
_c1 = np.float32(6.28125)
_r = TWO_PI - float(_c1)
_c2 = np.float32(_r)
_c3 = np.float32(_r - float(_c2))
CW1, CW2, CW3 = float(_c1), float(_c2), float(_c3)
MAGIC = 12582912.0
QSCALE = 192.0 ** -0.5


def make_ident(S, nc, idt, idb):
    S.op("pool", lambda: nc.gpsimd.memset(idt[:], 0.0), writes=[idb])
    S.op("pool", lambda: nc.gpsimd.affine_select(out=idt[:], in_=idt[:], pattern=[[-1, idt.shape[1]]], compare_op=ALU.not_equal, fill=1.0, base=0, channel_multiplier=1), reads=[idb], writes=[idb])


def emit_sincos(S, nc, P, N, pos_t, pos_b, invf_t, cst_b, halfpi_t, a, k, r, cos, sinpm):
    (a_t, a_b), (k_t, k_b), (r_t, r_b), (c_t, c_b), (s_t, s_b) = a, k, r, cos, sinpm
    S.op("dve", lambda: nc.vector.tensor_copy(a_t[0:P, 0:N], pos_t[0:P, 0:N]), reads=[pos_b], writes=[a_b])
    S.op("dve", lambda: nc.vector.tensor_scalar(out=a_t[0:P, 0:N], in0=a_t[0:P, 0:N], scalar1=invf_t[0:P, 0:1], scalar2=None, op0=ALU.mult), reads=[a_b, cst_b], writes=[a_b])
    S.op("dve", lambda: nc.vector.tensor_scalar(out=k_t[0:P, 0:N], in0=a_t[0:P, 0:N], scalar1=1.0 / TWO_PI, scalar2=MAGIC, op0=ALU.mult, op1=ALU.add), reads=[a_b], writes=[k_b])
    S.op("dve", lambda: nc.vector.tensor_scalar(out=k_t[0:P, 0:N], in0=k_t[0:P, 0:N], scalar1=-MAGIC, scalar2=None, op0=ALU.add), reads=[k_b], writes=[k_b])
    S.op("dve", lambda: nc.vector.scalar_tensor_tensor(out=r_t[0:P, 0:N], in0=k_t[0:P, 0:N], scalar=-CW1, in1=a_t[0:P, 0:N], op0=ALU.mult, op1=ALU.add), reads=[k_b, a_b], writes=[r_b])
    S.op("dve", lambda: nc.vector.scalar_tensor_tensor(out=r_t[0:P, 0:N], in0=k_t[0:P, 0:N], scalar=-CW2, in1=r_t[0:P, 0:N], op0=ALU.mult, op1=ALU.add), reads=[k_b, r_b], writes=[r_b])
    S.op("dve", lambda: nc.vector.scalar_tensor_tensor(out=r_t[0:P, 0:N], in0=k_t[0:P, 0:N], scalar=-CW3, in1=r_t[0:P, 0:N], op0=ALU.mult, op1=ALU.add), reads=[k_b, r_b], writes=[r_b])
    S.op("dve", lambda: nc.vector.tensor_scalar(out=r_t[0:P, 0:N], in0=r_t[0:P, 0:N], scalar1=-math.pi, scalar2=math.pi, op0=ALU.max, op1=ALU.min), reads=[r_b], writes=[r_b])
    h = P // 2
    S.op("act", lambda: nc.scalar.activation(out=s_t[0:h, 0:N], in_=r_t[0:h, 0:N], func=AF.Sin, scale=-1.0), reads=[r_b], writes=[s_b])
    S.op("act", lambda: nc.scalar.activation(out=s_t[h:P, 0:N], in_=r_t[h:P, 0:N], func=AF.Sin, scale=1.0), reads=[r_b], writes=[s_b])
    S.op("dve", lambda: nc.vector.scalar_tensor_tensor(out=k_t[0:P, 0:N], in0=r_t[0:P, 0:N], scalar=-1.0, in1=r_t[0:P, 0:N], op0=ALU.mult, op1=ALU.max), reads=[r_b], writes=[k_b])
    S.op("act", lambda: nc.scalar.activation(out=c_t[0:P, 0:N], in_=k_t[0:P, 0:N], func=AF.Sin, scale=-1.0, bias=halfpi_t[0:P, 0:1]), reads=[k_b, cst_b], writes=[c_b])


def build_A(NTOK, stage=9):
    nc = bass.Bass("TRN2", target_bir_lowering=False)
    NS = NTOK // 512
    di = lambda n, s, d=F32: nc.dram_tensor(n, list(s), d, kind="ExternalInput").ap()
    do = lambda n, s, d=F32: nc.dram_tensor(n, list(s), d, kind="ExternalOutput").ap()
    x = di("x", [NTOK, D]); cT = di("cT", [128, 16]); wada = di("wadaA", [D, 4096]); bada = di("badaA", [1, 4096])
    g1 = di("g1", [1, D]); w_in = di("w_in", [D, 1856]); w_in_sw = di("w_in_sw", [D, 64])
    gq = di("gq", [128, 4]); gkv = di("gkv", [128, 2])
    wq_n = di("wq_n", [512, 1024]); wq_p = di("wq_p", [512, 512]); wq_ps = di("wq_ps", [512, 512])
    wk = di("wk", [256, 1024]); wv = di("wv", [256, 1024])
    pos = di("pos", [1, NTOK], I32); invf = di("invf", [64, 1])
    upT = do("upT", [512, NTOK]); usT = do("usT", [512, NTOK])
    QT = do("QT", [8, 192, NTOK], BF16); KT = do("KT", [8, 128, NTOK], BF16)
    kpeT = do("kpeT", [64, NTOK], BF16); V = do("V", [NTOK, 1024], BF16)
    outs = [S_ for S_ in ()]
    with ExitStack() as es:
        S = Sched(nc, es)
        ob = {n: S.buf(n) for n in ("upT", "usT", "QT", "KT", "kpeT", "V")}
        idt, idb = S.sb("idt", [128, 128], BF16)
        make_ident(S, nc, idt, idb)
        ones_t, ones_b = S.sb("ones", [128, 128], BF16)
        S.op("pool", lambda: nc.gpsimd.memset(ones_t[:], 1.0), writes=[ones_b])
        cst_t, cst_b = S.sb("cst", [128, 8], F32)
        S.op("pool", lambda: nc.gpsimd.memset(cst_t[:, 0:1], EPS), writes=[cst_b])
        S.op("pool", lambda: nc.gpsimd.memset(cst_t[:, 1:2], math.pi / 2), writes=[cst_b])
        S.dma("sp", lambda: nc.sync.dma_start(out=cst_t[0:64, 2:3], in_=invf[:, :]), writes=[cst_b])
        gq_t, gq_b = S.sb("gq", [128, 4], F32)
        gkv_t, gkv_b = S.sb("gkv", [128, 2], F32)
        S.dma("sp", lambda: nc.sync.dma_start(out=gq_t[:], in_=gq[:, :]), writes=[gq_b])
        S.dma("sp", lambda: nc.sync.dma_start(out=gkv_t[:], in_=gkv[:, :]), writes=[gkv_b])
        win_t, win_b = S.sb("win", [128, 16, 1856 + 64], BF16)
        w_in_v = w_in.rearrange("(c p) n -> p c n", p=128)
        w_in_sw_v = w_in_sw.rearrange("(c p) n -> p c n", p=128)
        for c in range(16):
            S.dma("pool", lambda: nc.gpsimd.dma_start(out=win_t[:, c, 0:1856], in_=w_in_v[:, c, :]), writes=[win_b])
        S.dma("pool", lambda: nc.gpsimd.dma_start(out=win_t[:, :, 1856:1920], in_=w_in_sw_v[:, :, :]), writes=[win_b])
        wq_t, wq_b = S.sb("wq", [128, 4, 2048], BF16)
        S.dma("pool", lambda: nc.gpsimd.dma_start(out=wq_t[:, :, 0:1024], in_=wq_n.rearrange("(c p) n -> p c n", p=128)), writes=[wq_b])
        S.dma("pool", lambda: nc.gpsimd.dma_start(out=wq_t[:, :, 1024:1536], in_=wq_p.rearrange("(c p) n -> p c n", p=128)), writes=[wq_b])
        S.dma("pool", lambda: nc.gpsimd.dma_start(out=wq_t[:, :, 1536:2048], in_=wq_ps.rearrange("(c p) n -> p c n", p=128)), writes=[wq_b])
        wkv_t, wkv_b = S.sb("wkv", [128, 2, 2048], BF16)
        S.dma("pool", lambda: nc.gpsimd.dma_start(out=wkv_t[:, :, 0:1024], in_=wk.rearrange("(c p) n -> p c n", p=128)), writes=[wkv_b])
        S.dma("pool", lambda: nc.gpsimd.dma_start(out=wkv_t[:, :, 1024:2048], in_=wv.rearrange("(c p) n -> p c n", p=128)), writes=[wkv_b])
        gmod_t, gmod_b = S.sb("gmod", [128, D], F32)
        shA_t, shA_b = S.sb("shA", [128, D], F32)
        xt = [S.sb(f"xt{i}", [128, D], F32) for i in range(2)]
        hb = [S.sb(f"hb{i}", [128, D], BF16) for i in range(2)]
        hT_t, hT_b = S.sb("hT", [128, 16, 512], BF16)
        sq_t, sq_b = S.sb("sq", [128, D], BF16)
        st = [S.sb(f"st{i}", [128, 1], F32) for i in range(4)]
        qc_t, qc_b = S.sb("qc", [128, 6, 512], F32)
        qcn_t, qcn_b = S.sb("qcn", [128, 6, 512], BF16)
        rs_t, rs_b = S.sb("rs", [128, 512], F32)
        stg = [S.sb(f"stg{i}", [128, 512], F32) for i in range(4)]
        stgh = [S.sb(f"stgh{i}", [128, 512], BF16) for i in range(4)]
        vst = [S.sb(f"vst{i}", [128, 1024], BF16) for i in range(2)]
        pos_t, pos_b = S.sb("pos", [64, 512], I32)
        tA = S.sb("tA", [64, 512], F32); tK = S.sb("tK", [64, 512], F32); tR = S.sb("tR", [64, 512], F32)
        cos = S.sb("cos", [64, 512], F32); sinpm = S.sb("sinpm", [64, 512], F32)
        kpr = S.sb("kpr", [64, 512], F32); kps = S.sb("kps", [64, 512], F32)
        pT = [S.ps(f"pT{i}", [128, 1024], BF16) for i in range(2)]
        pZ = [S.ps(f"pZ{i}", [128, 512], F32) for i in range(3)]
        pS = S.ps("pS", [128, 512], F32)
        pQ = [S.ps(f"pQ{i}", [128, 512], F32) for i in range(2)]

        if stage < 1:
            S.finish([win_b, wq_b, wkv_b, gq_b, cst_b]); return nc
        cB_t, cB_b = S.sb("cB", [128, 16, 128], F32)
        cT_t, cT_b = S.sb("cTt", [128, 16], F32)
        S.dma("sp", lambda: nc.sync.dma_start(out=cT_t[:], in_=cT[:, :]), writes=[cT_b])
        S.op("dve", lambda: nc.vector.tensor_copy(cB_t[:], cT_t[:].unsqueeze(2).to_broadcast([128, 16, 128])), reads=[cT_b], writes=[cB_b])
        S.dma("sp", lambda: nc.sync.dma_start(out=shA_t[:], in_=bada[0:1, 0:D].partition_broadcast(128)), writes=[shA_b])
        S.dma("sp", lambda: nc.sync.dma_start(out=gmod_t[:], in_=bada[0:1, D:2 * D].partition_broadcast(128)), writes=[gmod_b])
        wada_v = wada.rearrange("(c p) n -> p c n", p=128)
        nsl = 0
        for blk in range(8):
            pz_t, pz_b = pZ[blk % 2]
            for q4 in range(4):
                (w_t, w_b) = xt[nsl % 2]; nsl += 1
                wv_ = w_t[:].rearrange("p (c n) -> p c n", c=4)
                S.dma("sp", lambda: nc.sync.dma_start(out=wv_, in_=wada_v[:, q4 * 4:(q4 + 1) * 4, blk * 512:(blk + 1) * 512]), writes=[w_b])
                for cc in range(4):
                    c = q4 * 4 + cc
                    S.op("pe", lambda: nc.tensor.matmul(pz_t[:], lhsT=cB_t[:, c, :], rhs=wv_[:, cc, :], start=(c == 0), stop=(c == 15)), reads=[cB_b, w_b], writes=[pz_b])
            tgt_t, tgt_b = (shA_t, shA_b) if blk < 4 else (gmod_t, gmod_b)
            cs = slice((blk % 4) * 512, (blk % 4 + 1) * 512)
            S.op("dve", lambda: nc.vector.tensor_tensor(out=tgt_t[:, cs], in0=pz_t[:], in1=tgt_t[:, cs], op=ALU.add), reads=[pz_b, tgt_b], writes=[tgt_b])
        (g_t, g_b) = xt[nsl % 2]; nsl += 1
        S.dma("sp", lambda: nc.sync.dma_start(out=g_t[:], in_=g1[0:1, :].partition_broadcast(128)), writes=[g_b])
        S.op("dve", lambda: nc.vector.scalar_tensor_tensor(out=gmod_t[:], in0=gmod_t[:], scalar=1.0, in1=g_t[:], op0=ALU.add, op1=ALU.mult), reads=[gmod_b, g_b], writes=[gmod_b])

        if stage < 2:
            S.finish([gmod_b, shA_b, win_b, wq_b, wkv_b]); return nc
        x_v = x.rearrange("(n p) d -> n p d", p=128)
        ntile = 0
        nst = 0
        nz = 0
        nq = 0
        nstg = 0
        for s in range(NS):
            t0 = s * 512
            S.dma("sp", lambda: nc.sync.dma_start(out=pos_t[:], in_=pos[0:1, t0:t0 + 512].partition_broadcast(64)), writes=[pos_b])
            emit_sincos(S, nc, 64, 512, pos_t, pos_b, cst_t[:, 2:3], cst_b, cst_t[:, 1:2], tA, tK, tR, cos, sinpm)
            if stage < 3:
                S.finish([cos[1], sinpm[1], gmod_b, shA_b, win_b, wq_b, wkv_b]); return nc
            for j in range(4):
                (x_t, x_b) = xt[nsl % 2]; nsl += 1
                (h_t, h_b) = hb[ntile % 2]
                (ss_t, ss_b) = st[nst % 4]; nst += 1
                (rr_t, rr_b) = st[nst % 4]; nst += 1
                S.dma("sp", lambda: nc.sync.dma_start(out=x_t[:], in_=x_v[s * 4 + j]), writes=[x_b])
                S.op("act", lambda: nc.scalar.activation(out=sq_t[:], in_=x_t[:], func=AF.Square, accum_out=ss_t[:]), reads=[x_b], writes=[sq_b, ss_b])
                S.op("act", lambda: nc.scalar.activation(out=rr_t[:], in_=ss_t[:], func=AF.Sqrt, scale=1.0 / D, bias=cst_t[:, 0:1]), reads=[ss_b, cst_b], writes=[rr_b])
                S.op("dve", lambda: nc.vector.reciprocal(rr_t[:], rr_t[:]), reads=[rr_b], writes=[rr_b])
                S.op("dve", lambda: nc.vector.scalar_tensor_tensor(out=x_t[:], in0=x_t[:], scalar=rr_t[:, 0:1], in1=gmod_t[:], op0=ALU.mult, op1=ALU.mult), reads=[x_b, rr_b, gmod_b], writes=[x_b])
                S.op("pool", lambda: nc.gpsimd.tensor_tensor(out=h_t[:], in0=x_t[:], in1=shA_t[:], op=ALU.add), reads=[x_b, shA_b], writes=[h_b])
                for half in range(2):
                    (p_t, p_b) = pT[half]
                    for cc in range(8):
                        c = half * 8 + cc
                        S.op("pe", lambda: nc.tensor.transpose(p_t[:, cc * 128:(cc + 1) * 128], h_t[:, c * 128:(c + 1) * 128], idt[:]), reads=[h_b, idb], writes=[p_b])
                    eng = "act" if half == 0 else "dve"
                    dst = hT_t[:, half * 8:(half + 1) * 8, j * 128:(j + 1) * 128]
                    src = p_t[:].rearrange("p (c n) -> p c n", c=8)
                    if eng == "act":
                        S.op("act", lambda: nc.scalar.copy(dst, src), reads=[p_b], writes=[hT_b])
                    else:
                        S.op("dve", lambda: nc.vector.tensor_copy(dst, src), reads=[p_b], writes=[hT_b])
                ntile += 1
            if stage < 4:
                S.finish([hT_b, cos[1], sinpm[1], gmod_b, shA_b, win_b, wq_b, wkv_b]); return nc
            def zblock(c0, M):
                nonlocal nz
                (pz_t, pz_b) = pZ[nz % 3]; nz += 1
                for c in range(16):
                    S.op("pe", lambda: nc.tensor.matmul(pz_t[0:M, :], lhsT=win_t[:, c, c0:c0 + M], rhs=hT_t[:, c, :], start=(c == 0), stop=(c == 15)), reads=[win_b, hT_b], writes=[pz_b])
                return pz_t, pz_b
            for blk in range(8):
                pz_t, pz_b = zblock(blk * 128, 128)
                (sg_t, sg_b) = stg[nstg % 4]; nstg += 1
                if blk % 2 == 0:
                    S.op("act", lambda: nc.scalar.copy(sg_t[:], pz_t[:]), reads=[pz_b], writes=[sg_b])
                else:
                    S.op("dve", lambda: nc.vector.tensor_copy(sg_t[:], pz_t[:]), reads=[pz_b], writes=[sg_b])
                dst = (upT if blk < 4 else usT)[(blk % 4) * 128:(blk % 4 + 1) * 128, t0:t0 + 512]
                S.dma("sp", lambda: nc.sync.dma_start(out=dst, in_=sg_t[:]), reads=[sg_b], writes=[ob["upT" if blk < 4 else "usT"]])
            if stage < 5:
                S.finish(list(ob.values()) + [hT_b, wq_b, wkv_b]); return nc
            for grp, (b0, nb, gt, gb_, dim) in enumerate(((0, 4, gq_t, gq_b, 512), (4, 2, gkv_t, gkv_b, 256))):
                (ps_t, ps_b) = pS
                for i in range(nb):
                    blk = b0 + i
                    pz_t, pz_b = zblock(1024 + blk * 128, 128)
                    S.op("act", lambda: nc.scalar.activation(out=sq_t[:, i * 512:(i + 1) * 512], in_=pz_t[:], func=AF.Square), reads=[pz_b], writes=[sq_b])
                    S.op("dve", lambda: nc.vector.tensor_copy(qc_t[:, blk, :], pz_t[:]), reads=[pz_b], writes=[qc_b])
                    S.op("pe", lambda: nc.tensor.matmul(ps_t[:], lhsT=ones_t[:], rhs=sq_t[:, i * 512:(i + 1) * 512], start=(i == 0), stop=(i == nb - 1)), reads=[ones_b, sq_b], writes=[ps_b])
                S.op("act", lambda: nc.scalar.activation(out=rs_t[:], in_=ps_t[:], func=AF.Sqrt, scale=1.0 / dim, bias=cst_t[:, 0:1]), reads=[ps_b, cst_b], writes=[rs_b])
                S.op("dve", lambda: nc.vector.reciprocal(rs_t[:], rs_t[:]), reads=[rs_b], writes=[rs_b])
                for i in range(nb):
                    blk = b0 + i
                    S.op("dve", lambda: nc.vector.scalar_tensor_tensor(out=qcn_t[:, blk, :], in0=qc_t[:, blk, :], scalar=gt[:, i:i + 1], in1=rs_t[:], op0=ALU.mult, op1=ALU.mult), reads=[qc_b, gb_, rs_b], writes=[qcn_b])

            def rope_out(p1_t, p1_b, p2_t, p2_b, scale, dst, obuf):
                nonlocal nstg
                S.op("dve", lambda: nc.vector.tensor_tensor(out=kpr[0][:], in0=p1_t[0:64, :], in1=cos[0][:], op=ALU.mult), reads=[p1_b, cos[1]], writes=[kpr[1]])
                S.op("dve", lambda: nc.vector.tensor_tensor(out=kps[0][:], in0=p2_t[0:64, :], in1=sinpm[0][:], op=ALU.mult), reads=[p2_b, sinpm[1]], writes=[kps[1]])
                (sh_t, sh_b) = stgh[nstg % 4]; nstg += 1
                S.op("pool", lambda: nc.gpsimd.tensor_tensor(out=sh_t[0:64, :], in0=kpr[0][:], in1=kps[0][:], op=ALU.add), reads=[kpr[1], kps[1]], writes=[sh_b])
                S.dma("sp", lambda: nc.sync.dma_start(out=dst, in_=sh_t[0:64, :]), reads=[sh_b], writes=[obuf])
            if stage < 6:
                S.finish(list(ob.values()) + [qcn_b]); return nc
            p1_t, p1_b = zblock(1792, 64)
            p2_t, p2_b = zblock(1856, 64)
            rope_out(p1_t, p1_b, p2_t, p2_b, 1.0, kpeT[:, t0:t0 + 512], ob["kpeT"])
            if stage < 7:
                S.finish(list(ob.values()) + [qcn_b]); return nc
            for h in range(8):
                (pq_t, pq_b) = pQ[nq % 2]; nq += 1
                for c in range(4):
                    S.op("pe", lambda: nc.tensor.matmul(pq_t[:], lhsT=wq_t[:, c, h * 128:(h + 1) * 128], rhs=qcn_t[:, c, :], start=(c == 0), stop=(c == 3)), reads=[wq_b, qcn_b], writes=[pq_b])
                (sh_t, sh_b) = stgh[nstg % 4]; nstg += 1
                S.op("act", lambda: nc.scalar.activation(out=sh_t[:], in_=pq_t[:], func=AF.Copy, scale=QSCALE), reads=[pq_b], writes=[sh_b])
                S.dma("sp", lambda: nc.sync.dma_start(out=QT[h, 0:128, t0:t0 + 512], in_=sh_t[:]), reads=[sh_b], writes=[ob["QT"]])
                (p1_t, p1_b) = pQ[nq % 2]; nq += 1
                for c in range(4):
                    S.op("pe", lambda: nc.tensor.matmul(p1_t[0:64, :], lhsT=wq_t[:, c, 1024 + h * 64:1024 + (h + 1) * 64], rhs=qcn_t[:, c, :], start=(c == 0), stop=(c == 3)), reads=[wq_b, qcn_b], writes=[p1_b])
                (p2_t, p2_b) = pZ[nz % 3]; nz += 1
                for c in range(4):
                    S.op("pe", lambda: nc.tensor.matmul(p2_t[0:64, :], lhsT=wq_t[:, c, 1536 + h * 64:1536 + (h + 1) * 64], rhs=qcn_t[:, c, :], start=(c == 0), stop=(c == 3)), reads=[wq_b, qcn_b], writes=[p2_b])
                S.op("dve", lambda: nc.vector.tensor_tensor(out=kpr[0][:], in0=p1_t[0:64, :], in1=cos[0][:], op=ALU.mult), reads=[p1_b, cos[1]], writes=[kpr[1]])
                S.op("dve", lambda: nc.vector.tensor_tensor(out=kps[0][:], in0=p2_t[0:64, :], in1=sinpm[0][:], op=ALU.mult), reads=[p2_b, sinpm[1]], writes=[kps[1]])
                S.op("pool", lambda: nc.gpsimd.tensor_tensor(out=kpr[0][:], in0=kpr[0][:], in1=kps[0][:], op=ALU.add), reads=[kpr[1], kps[1]], writes=[kpr[1]])
                (sh_t, sh_b) = stgh[nstg % 4]; nstg += 1
                S.op("act", lambda: nc.scalar.activation(out=sh_t[0:64, :], in_=kpr[0][:], func=AF.Copy, scale=QSCALE), reads=[kpr[1]], writes=[sh_b])
                S.dma("sp", lambda: nc.sync.dma_start(out=QT[h, 128:192, t0:t0 + 512], in_=sh_t[0:64, :]), reads=[sh_b], writes=[ob["QT"]])
            if stage < 8:
                S.finish(list(ob.values()) + [qcn_b]); return nc
            for h in range(8):
                (pq_t, pq_b) = pQ[nq % 2]; nq += 1
                for c in range(2):
                    S.op("pe", lambda: nc.tensor.matmul(pq_t[:], lhsT=wkv_t[:, c, h * 128:(h + 1) * 128], rhs=qcn_t[:, 4 + c, :], start=(c == 0), stop=(c == 1)), reads=[wkv_b, qcn_b], writes=[pq_b])
                (sh_t, sh_b) = stgh[nstg % 4]; nstg += 1
                if h % 2 == 0:
                    S.op("act", lambda: nc.scalar.copy(sh_t[:], pq_t[:]), reads=[pq_b], writes=[sh_b])
                else:
                    S.op("dve", lambda: nc.vector.tensor_copy(sh_t[:], pq_t[:]), reads=[pq_b], writes=[sh_b])
                S.dma("sp", lambda: nc.sync.dma_start(out=KT[h, :, t0:t0 + 512], in_=sh_t[:]), reads=[sh_b], writes=[ob["KT"]])
            for j in range(4):
                (v_t, v_b) = vst[j % 2]
                for hh in range(2):
                    (pq_t, pq_b) = pQ[nq % 2]; nq += 1
                    for c in range(2):
                        S.op("pe", lambda: nc.tensor.matmul(pq_t[:], lhsT=qcn_t[:, 4 + c, j * 128:(j + 1) * 128], rhs=wkv_t[:, c, 1024 + hh * 512:1024 + (hh + 1) * 512], start=(c == 0), stop=(c == 1)), reads=[wkv_b, qcn_b], writes=[pq_b])
                    if hh == 0:
                        S.op("act", lambda: nc.scalar.copy(v_t[:, 0:512], pq_t[:]), reads=[pq_b], writes=[v_b])
                    else:
                        S.op("dve", lambda: nc.vector.tensor_copy(v_t[:, 512:1024], pq_t[:]), reads=[pq_b], writes=[v_b])
                S.dma("sp", lambda: nc.sync.dma_start(out=V[t0 + j * 128:t0 + (j + 1) * 128, :], in_=v_t[:]), reads=[v_b], writes=[ob["V"]])
        S.finish(list(ob.values()))
        print("A ninstr", S.ninstr)
    return nc

import math

NEG = -30000.0


def barrier(S):
    deps = []
    for e in ("pe", "dve", "act", "pool"):
        if S.cnt[e] > 0:
            deps.append((S.sem[e], S.cnt[e], e))
    for e in ("pe", "dve", "act", "pool", "sp"):
        S._wait(e, [d for d in deps if d[2] != e])


def emit_attention(S, nc, L, QT, KT, kpeT, V, ymT, ymT_b, idt, idb, ones_t, ones_b):
    NQ = L // 512
    NB = L // 128
    with ExitStack() as es2:
        es_save = S.es
        S.es = es2
        K_t, K_b = S.sb("attK", [128, L], BF16)
        P_t, P_b = S.sb("attKpe", [64, L], BF16)
        V_t, V_b = S.sb("attV", [128, NB, 128], BF16)
        mask_t, mask_b = S.sb("attmask", [128, 4, 512], BF16)
        qn = [S.sb(f"qn{i}", [128, 512], BF16) for i in range(2)]
        qp = [S.sb(f"qp{i}", [64, 512], BF16) for i in range(2)]
        NPT = 4
        pt = [S.sb(f"pt{i}", [128, 512], BF16) for i in range(NPT)]
        rl_t, rl_b = S.sb("attrl", [128, 512], F32)
        ost = [S.sb(f"ost{i}", [128, 512], F32) for i in range(2)]
        acc = [S.sb(f"attacc{i}", [128, 512], F32) for i in range(2)]
        onesf_t, onesf_b = S.sb("attonesf", [128, 128], F32)
        S.op("pool", lambda: nc.gpsimd.memset(onesf_t[:], 1.0), writes=[onesf_b])
        pS = [S.ps(f"aS{i}", [128, 512], F32) for i in range(NPT)]
        pO = [S.ps(f"aO{i}", [128, 512], F32) for i in range(2)]
        pL = [S.ps(f"aL{i}", [128, 512], F32) for i in range(2)]
        S.op("pool", lambda: nc.gpsimd.memset(mask_t[:], 0.0), writes=[mask_b])
        for d in range(4):
            S.op("pool", lambda: nc.gpsimd.affine_select(out=mask_t[:, d, :], in_=mask_t[:, d, :], pattern=[[1, 512]], compare_op=ALU.is_ge, fill=NEG, base=-128 * d, channel_multiplier=-1), reads=[mask_b], writes=[mask_b])
        S.dma("sp", lambda: nc.sync.dma_start(out=P_t[:], in_=kpeT[:, :]), writes=[P_b])
        nqt = 0
        for h in range(2):
            for part in range(4):
                sl = slice(part * L // 4, (part + 1) * L // 4)
                S.dma("sp", lambda: nc.sync.dma_start(out=K_t[:, sl], in_=KT[h, :, sl]), writes=[K_b])
            Vv = V.rearrange("(n p) h d -> p n h d", p=128)
            for part in range(4):
                nsl = slice(part * NB // 4, (part + 1) * NB // 4)
                S.dma("sp", lambda: nc.sync.dma_start(out=V_t[:, nsl, :], in_=Vv[:, nsl, h, :]), writes=[V_b])
            for j in range(NQ):
                (qn_t, qn_b) = qn[nqt % 2]; (qp_t, qp_b) = qp[nqt % 2]
                (o_t, o_b) = pO[nqt % 2]; (l_t, l_b) = pL[nqt % 2]
                (os_t, os_b) = ost[nqt % 2]; (ac_t, ac_b) = acc[nqt % 2]
                nqt += 1
                S.dma("sp", lambda: nc.sync.dma_start(out=qn_t[:], in_=QT[h, 0:128, j * 512:(j + 1) * 512]), writes=[qn_b])
                S.dma("sp", lambda: nc.sync.dma_start(out=qp_t[:], in_=QT[h, 128:192, j * 512:(j + 1) * 512]), writes=[qp_b])
                nblk = 4 * j + 4

                def emit_S(i):
                    (s_t, s_b) = pS[i % NPT]
                    dg = i - 4 * j
                    S.op("pe", lambda: nc.tensor.matmul(s_t[:], lhsT=K_t[:, i * 128:(i + 1) * 128], rhs=qn_t[:], start=True, stop=False), reads=[K_b, qn_b], writes=[s_b])
                    S.op("pe", lambda: nc.tensor.matmul(s_t[:], lhsT=P_t[:, i * 128:(i + 1) * 128], rhs=qp_t[:], start=False, stop=(dg < 0)), reads=[P_b, qp_b], writes=[s_b])
                    if dg >= 0:
                        S.op("pe", lambda: nc.tensor.matmul(s_t[:], lhsT=idt[:], rhs=mask_t[:, dg, :], start=False, stop=True), reads=[idb, mask_b], writes=[s_b])
                LOOK = 2
                for i in range(min(LOOK, nblk)):
                    emit_S(i)
                for i in range(nblk):
                    if i + LOOK < nblk:
                        emit_S(i + LOOK)
                    (s_t, s_b) = pS[i % NPT]
                    (p_t, p_b) = pt[i % NPT]
                    S.op("act", lambda: nc.scalar.activation(out=p_t[:], in_=s_t[:], func=AF.Exp), reads=[s_b], writes=[p_b])
                    S.op("pe", lambda: nc.tensor.matmul(o_t[:], lhsT=V_t[:, i, :], rhs=p_t[:], start=(i == 0), stop=(i == nblk - 1)), reads=[V_b, p_b], writes=[o_b])
                    S.op("pe", lambda: nc.tensor.matmul(l_t[:], lhsT=ones_t[:], rhs=p_t[:], start=(i == 0), stop=(i == nblk - 1)), reads=[ones_b, p_b], writes=[l_b])
                S.op("dve", lambda: nc.vector.reciprocal(rl_t[:], l_t[:]), reads=[l_b], writes=[rl_b])
                S.op("dve", lambda: nc.vector.tensor_tensor(out=os_t[:], in0=o_t[:], in1=rl_t[:], op=ALU.mult), reads=[o_b, rl_b], writes=[os_b])
                S.dma("sp", lambda: nc.sync.dma_start(out=ymT[h * 128:(h + 1) * 128, j * 512:(j + 1) * 512], in_=os_t[:]), reads=[os_b], writes=[ymT_b])
        S.barrier()
        S.es = es_save


def sincos_angle(S, nc, P, N, ang, k, r, halfpi_t, halfpi_b, sin_out, cos_out, sin_scale=1.0):
    (a_t, a_b), (k_t, k_b), (r_t, r_b) = ang, k, r
    S.op("dve", lambda: nc.vector.tensor_scalar(out=k_t, in0=a_t, scalar1=1.0 / TWO_PI, scalar2=MAGIC, op0=ALU.mult, op1=ALU.add), reads=[a_b], writes=[k_b])
    S.op("dve", lambda: nc.vector.tensor_scalar(out=k_t, in0=k_t, scalar1=-MAGIC, scalar2=None, op0=ALU.add), reads=[k_b], writes=[k_b])
    S.op("dve", lambda: nc.vector.scalar_tensor_tensor(out=r_t, in0=k_t, scalar=-CW1, in1=a_t, op0=ALU.mult, op1=ALU.add), reads=[k_b, a_b], writes=[r_b])
    S.op("dve", lambda: nc.vector.scalar_tensor_tensor(out=r_t, in0=k_t, scalar=-CW2, in1=r_t, op0=ALU.mult, op1=ALU.add), reads=[k_b, r_b], writes=[r_b])
    S.op("dve", lambda: nc.vector.scalar_tensor_tensor(out=r_t, in0=k_t, scalar=-CW3, in1=r_t, op0=ALU.mult, op1=ALU.add), reads=[k_b, r_b], writes=[r_b])
    S.op("dve", lambda: nc.vector.tensor_scalar(out=r_t, in0=r_t, scalar1=-math.pi, scalar2=math.pi, op0=ALU.max, op1=ALU.min), reads=[r_b], writes=[r_b])
    S.op("act", lambda: nc.scalar.activation(out=sin_out[0], in_=r_t, func=AF.Sin, scale=1.0), reads=[r_b], writes=[sin_out[1]])
    S.op("dve", lambda: nc.vector.scalar_tensor_tensor(out=k_t, in0=r_t, scalar=-1.0, in1=r_t, op0=ALU.mult, op1=ALU.max), reads=[r_b], writes=[k_b])
    S.op("act", lambda: nc.scalar.activation(out=cos_out[0], in_=k_t, func=AF.Sin, scale=-1.0, bias=halfpi_t), reads=[k_b, halfpi_b], writes=[cos_out[1]])


GELU_C = 2.0 * math.sqrt(2.0 / math.pi)


def emit_ssm(S, nc, L, usT, lamre, lamim, logdt, bre, bim, cA, cB, dsk, ysT, ysT_b, idt, idb, T=512):
    NBLK = L // T
    with ExitStack() as es2:
        es_save = S.es
        S.es = es2
        V = nc.vector; G = nc.gpsimd; A = nc.scalar; PE = nc.tensor
        idf_t, idf_b = S.sb("ssm_idf", [128, 128], F32)
        make_ident(S, nc, idf_t, idf_b)
        hp_t, hp_b = S.sb("ssm_hp", [128, 1], F32)
        S.op("pool", lambda: G.memset(hp_t[:], math.pi / 2), writes=[hp_b])
        par_t, par_b = S.sb("ssm_par", [128, 3, 8], F32)
        S.dma("sp", lambda: nc.sync.dma_start(out=par_t[:, 0, :], in_=lamre[:, :]), writes=[par_b])
        S.dma("sp", lambda: nc.sync.dma_start(out=par_t[:, 1, :], in_=lamim[:, :]), writes=[par_b])
        S.dma("sp", lambda: nc.sync.dma_start(out=par_t[:, 2, :], in_=logdt[:, :]), writes=[par_b])
        d_t, d_b = S.sb("ssm_d", [128, 1], F32)
        S.dma("sp", lambda: nc.sync.dma_start(out=d_t[:], in_=dsk[:, :]), writes=[d_b])
        w = {}
        for n in ("dt", "mag", "th", "sn", "cs", "k", "r", "den", "fre", "fim", "t1", "t2", "thT", "cT", "sT", "spm"):
            w[n] = S.sb("ssm_" + n, [128, 8], F32)
        lr = par_t[:, 0, :]; li = par_t[:, 1, :]
        def tt(out, a, b, op, eng="dve"):
            if eng == "dve":
                S.op("dve", lambda: V.tensor_tensor(out=out[0][:] if not isinstance(out[0], bass.AP) else out[0], in0=a[0], in1=b[0], op=op), reads=[a[1], b[1]], writes=[out[1]])
        P_ = lambda n: (w[n][0][:], w[n][1])
        S.op("act", lambda: A.activation(out=w["dt"][0][:], in_=par_t[:, 2, :], func=AF.Exp), reads=[par_b], writes=[w["dt"][1]])
        S.op("dve", lambda: V.tensor_tensor(out=w["t1"][0][:], in0=lr, in1=w["dt"][0][:], op=ALU.mult), reads=[par_b, w["dt"][1]], writes=[w["t1"][1]])
        S.op("act", lambda: A.activation(out=w["mag"][0][:], in_=w["t1"][0][:], func=AF.Exp), reads=[w["t1"][1]], writes=[w["mag"][1]])
        S.op("dve", lambda: V.tensor_tensor(out=w["th"][0][:], in0=li, in1=w["dt"][0][:], op=ALU.mult), reads=[par_b, w["dt"][1]], writes=[w["th"][1]])
        sincos_angle(S, nc, 128, 8, P_("th"), P_("k"), P_("r"), hp_t[:, 0:1], hp_b, P_("sn"), P_("cs"))
        S.op("dve", lambda: V.tensor_tensor(out=w["cs"][0][:], in0=w["cs"][0][:], in1=w["mag"][0][:], op=ALU.mult), reads=[w["cs"][1], w["mag"][1]], writes=[w["cs"][1]])
        S.op("dve", lambda: V.tensor_tensor(out=w["sn"][0][:], in0=w["sn"][0][:], in1=w["mag"][0][:], op=ALU.mult), reads=[w["sn"][1], w["mag"][1]], writes=[w["sn"][1]])
        S.op("dve", lambda: V.tensor_tensor(out=w["den"][0][:], in0=lr, in1=lr, op=ALU.mult), reads=[par_b], writes=[w["den"][1]])
        S.op("dve", lambda: V.tensor_tensor(out=w["t1"][0][:], in0=li, in1=li, op=ALU.mult), reads=[par_b], writes=[w["t1"][1]])
        S.op("dve", lambda: V.tensor_tensor(out=w["den"][0][:], in0=w["den"][0][:], in1=w["t1"][0][:], op=ALU.add), reads=[w["den"][1], w["t1"][1]], writes=[w["den"][1]])
        S.op("dve", lambda: V.reciprocal(w["den"][0][:], w["den"][0][:]), reads=[w["den"][1]], writes=[w["den"][1]])
        S.op("dve", lambda: V.tensor_scalar(out=w["t2"][0][:], in0=w["cs"][0][:], scalar1=-1.0, scalar2=None, op0=ALU.add), reads=[w["cs"][1]], writes=[w["t2"][1]])
        S.op("dve", lambda: V.tensor_tensor(out=w["fre"][0][:], in0=w["t2"][0][:], in1=lr, op=ALU.mult), reads=[w["t2"][1], par_b], writes=[w["fre"][1]])
        S.op("dve", lambda: V.tensor_tensor(out=w["t1"][0][:], in0=w["sn"][0][:], in1=li, op=ALU.mult), reads=[w["sn"][1], par_b], writes=[w["t1"][1]])
        S.op("dve", lambda: V.tensor_tensor(out=w["fre"][0][:], in0=w["fre"][0][:], in1=w["t1"][0][:], op=ALU.add), reads=[w["fre"][1], w["t1"][1]], writes=[w["fre"][1]])
        S.op("dve", lambda: V.tensor_tensor(out=w["fre"][0][:], in0=w["fre"][0][:], in1=w["den"][0][:], op=ALU.mult), reads=[w["fre"][1], w["den"][1]], writes=[w["fre"][1]])
        S.op("dve", lambda: V.tensor_tensor(out=w["fim"][0][:], in0=w["sn"][0][:], in1=lr, op=ALU.mult), reads=[w["sn"][1], par_b], writes=[w["fim"][1]])
        S.op("dve", lambda: V.tensor_tensor(out=w["t1"][0][:], in0=w["t2"][0][:], in1=li, op=ALU.mult), reads=[w["t2"][1], par_b], writes=[w["t1"][1]])
        S.op("dve", lambda: V.tensor_tensor(out=w["fim"][0][:], in0=w["fim"][0][:], in1=w["t1"][0][:], op=ALU.subtract), reads=[w["fim"][1], w["t1"][1]], writes=[w["fim"][1]])
        S.op("dve", lambda: V.tensor_tensor(out=w["fim"][0][:], in0=w["fim"][0][:], in1=w["den"][0][:], op=ALU.mult), reads=[w["fim"][1], w["den"][1]], writes=[w["fim"][1]])
        bre_t, bre_b = S.sb("ssm_bre", [64, 8, 16], F32); bim_t, bim_b = S.sb("ssm_bim", [64, 8, 16], F32)
        S.dma("sp", lambda: nc.sync.dma_start(out=bre_t[:].rearrange("p g h -> p (g h)"), in_=bre[:, :]), writes=[bre_b])
        S.dma("sp", lambda: nc.sync.dma_start(out=bim_t[:].rearrange("p g h -> p (g h)"), in_=bim[:, :]), writes=[bim_b])
        bb_t, bb_b = S.sb("ssm_bb", [64, 2, 8, 16], F32)
        tmp_t, tmp_b = S.sb("ssm_tmpb", [64, 8, 16], F32)
        fre_b3 = w["fre"][0][0:64, :].unsqueeze(2).to_broadcast([64, 8, 16])
        fim_b3 = w["fim"][0][0:64, :].unsqueeze(2).to_broadcast([64, 8, 16])
        S.op("dve", lambda: V.tensor_tensor(out=bb_t[:, 0], in0=bre_t[:], in1=fre_b3, op=ALU.mult), reads=[bre_b, w["fre"][1]], writes=[bb_b])
        S.op("dve", lambda: V.tensor_tensor(out=tmp_t[:], in0=bim_t[:], in1=fim_b3, op=ALU.mult), reads=[bim_b, w["fim"][1]], writes=[tmp_b])
        S.op("dve", lambda: V.tensor_tensor(out=bb_t[:, 0], in0=bb_t[:, 0], in1=tmp_t[:], op=ALU.subtract), reads=[bb_b, tmp_b], writes=[bb_b])
        S.op("dve", lambda: V.tensor_tensor(out=bb_t[:, 1], in0=bim_t[:], in1=fre_b3, op=ALU.mult), reads=[bim_b, w["fre"][1]], writes=[bb_b])
        S.op("dve", lambda: V.tensor_tensor(out=tmp_t[:], in0=bre_t[:], in1=fim_b3, op=ALU.mult), reads=[bre_b, w["fim"][1]], writes=[tmp_b])
        S.op("dve", lambda: V.tensor_tensor(out=bb_t[:, 1], in0=bb_t[:, 1], in1=tmp_t[:], op=ALU.add), reads=[bb_b, tmp_b], writes=[bb_b])
        pX = S.ps("ssm_pX", [128, 512], F32)
        bbT_t, bbT_b = S.sb("ssm_bbT", [128, 2, 64], F32)
        for i in range(2):
            S.op("pe", lambda: PE.transpose(pX[0][:, i * 64:(i + 1) * 64], bb_t[:, i].rearrange("p g h -> p (g h)"), idf_t[0:64, 0:64]), reads=[bb_b, idf_b], writes=[pX[1]])
        S.op("dve", lambda: V.tensor_copy(bbT_t[:].rearrange("p a b -> p (a b)"), pX[0][:, 0:128]), reads=[pX[1]], writes=[bbT_b])
        gm_t, gm_b = S.sb("ssm_gm", [128, 8], F32)
        S.op("pool", lambda: G.memset(gm_t[:], 1.0), writes=[gm_b])
        S.op("pool", lambda: G.affine_select(out=gm_t[:], in_=gm_t[:], pattern=[[-16, 8]], compare_op=ALU.is_ge, fill=0.0, base=0, channel_multiplier=1), reads=[gm_b], writes=[gm_b])
        S.op("pool", lambda: G.affine_select(out=gm_t[:], in_=gm_t[:], pattern=[[16, 8]], compare_op=ALU.is_ge, fill=0.0, base=15, channel_multiplier=-1), reads=[gm_b], writes=[gm_b])
        LB_t, LB_b = S.sb("ssm_LB", [128, 8, 2, 128], BF16)
        for g in range(8):
            for var in range(2):
                for half in range(2):
                    src = bbT_t[:, half if var == 0 else 1 - half, :]
                    S.op("dve", lambda: V.tensor_scalar(out=LB_t[:, g, var, half * 64:(half + 1) * 64], in0=src, scalar1=gm_t[:, g:g + 1], scalar2=None, op0=ALU.mult), reads=[bbT_b, gm_b], writes=[LB_b])
        cin_t, cin_b = S.sb("ssm_cin", [128, 2, 128], F32)
        S.dma("sp", lambda: nc.sync.dma_start(out=cin_t[:, 0, :], in_=cA[:, :]), writes=[cin_b])
        S.dma("sp", lambda: nc.sync.dma_start(out=cin_t[:, 1, :], in_=cB[:, :]), writes=[cin_b])
        for i in range(2):
            S.op("pe", lambda: PE.transpose(pX[0][:, 128 + i * 128:256 + i * 128], cin_t[:, i, :], idf_t[:]), reads=[cin_b, idf_b], writes=[pX[1]])
        WC_t, WC_b = S.sb("ssm_WC", [128, 8, 2, 128], BF16)
        S.op("pool", lambda: G.memset(WC_t[:], 0.0), writes=[WC_b])
        for g in range(8):
            cs_ = slice(g * 16, (g + 1) * 16)
            S.op("act", lambda: A.activation(out=WC_t[0:64, g, 0, cs_], in_=pX[0][0:64, 128 + g * 16:128 + (g + 1) * 16], func=AF.Copy, scale=1.0), reads=[pX[1]], writes=[WC_b])
            S.op("act", lambda: A.activation(out=WC_t[64:128, g, 0, cs_], in_=pX[0][64:128, 128 + g * 16:128 + (g + 1) * 16], func=AF.Copy, scale=-1.0), reads=[pX[1]], writes=[WC_b])
            S.op("act", lambda: A.activation(out=WC_t[:, g, 1, cs_], in_=pX[0][:, 256 + g * 16:256 + (g + 1) * 16], func=AF.Copy, scale=-1.0), reads=[pX[1]], writes=[WC_b])
        S.op("dve", lambda: V.tensor_scalar(out=w["thT"][0][:], in0=w["th"][0][:], scalar1=float(T), scalar2=None, op0=ALU.mult), reads=[w["th"][1]], writes=[w["thT"][1]])
        sincos_angle(S, nc, 128, 8, P_("thT"), P_("k"), P_("r"), hp_t[:, 0:1], hp_b, P_("sT"), P_("cT"))
        S.op("dve", lambda: V.tensor_copy(w["spm"][0][0:64, :], w["sT"][0][0:64, :]), reads=[w["sT"][1]], writes=[w["spm"][1]])
        S.op("dve", lambda: V.tensor_scalar(out=w["spm"][0][64:128, :], in0=w["sT"][0][64:128, :], scalar1=-1.0, scalar2=None, op0=ALU.mult), reads=[w["sT"][1]], writes=[w["spm"][1]])
        sw_t, sw_b = S.sb("ssm_sw", [128, 128], F32)
        S.op("pool", lambda: G.memset(sw_t[:], 0.0), writes=[sw_b])
        S.op("pool", lambda: G.affine_select(out=sw_t[:], in_=sw_t[:], pattern=[[1, 128]], compare_op=ALU.not_equal, fill=1.0, base=-64, channel_multiplier=-1), reads=[sw_b], writes=[sw_b])
        S.op("pool", lambda: G.affine_select(out=sw_t[:], in_=sw_t[:], pattern=[[1, 128]], compare_op=ALU.not_equal, fill=1.0, base=64, channel_multiplier=-1), reads=[sw_b], writes=[sw_b])
        ROT_t, ROT_b = S.sb("ssm_ROT", [128, 8, 128], F32)
        for g in range(8):
            S.op("dve", lambda: V.tensor_scalar(out=ROT_t[:, g, :], in0=idf_t[:], scalar1=w["cT"][0][:, g:g + 1], scalar2=None, op0=ALU.mult), reads=[idf_b, w["cT"][1]], writes=[ROT_b])
            S.op("dve", lambda: V.scalar_tensor_tensor(out=ROT_t[:, g, :], in0=sw_t[:], scalar=w["spm"][0][:, g:g + 1], in1=ROT_t[:, g, :], op0=ALU.mult, op1=ALU.add), reads=[sw_b, w["spm"][1], ROT_b], writes=[ROT_b])
        COS_t, COS_b = S.sb("ssm_COS", [128, 8, T], F32)
        SIN_t, SIN_b = S.sb("ssm_SIN", [128, 8, T], F32)
        RHO_t, RHO_b = S.sb("ssm_RHO", [128, 8, T], F32)
        SPM_t, SPM_b = S.sb("ssm_SPM", [128, 8, T], F32)
        io_t, io_b = S.sb("ssm_iota", [128, T], F32)
        S.op("pool", lambda: G.iota(io_t[:], pattern=[[1, T]], base=0, channel_multiplier=0, allow_small_or_imprecise_dtypes=True), writes=[io_b])
        an = S.sb("ssm_an", [128, T], F32); kk = S.sb("ssm_kk", [128, T], F32); rr = S.sb("ssm_rr", [128, T], F32)
        for g in range(8):
            S.op("dve", lambda: V.tensor_scalar(out=an[0][:], in0=io_t[:], scalar1=w["th"][0][:, g:g + 1], scalar2=None, op0=ALU.mult), reads=[io_b, w["th"][1]], writes=[an[1]])
            sincos_angle(S, nc, 128, T, (an[0][:], an[1]), (kk[0][:], kk[1]), (rr[0][:], rr[1]), hp_t[:, 0:1], hp_b, (SIN_t[:, g, :], SIN_b), (COS_t[:, g, :], COS_b))
            S.op("dve", lambda: V.tensor_copy(RHO_t[:, g, :], w["mag"][0][:, g:g + 1].to_broadcast([128, T])), reads=[w["mag"][1]], writes=[RHO_b])
            S.op("dve", lambda: V.tensor_copy(SPM_t[0:64, g, :], SIN_t[0:64, g, :]), reads=[SIN_b], writes=[SPM_b])
            S.op("dve", lambda: V.tensor_scalar(out=SPM_t[64:128, g, :], in0=SIN_t[64:128, g, :], scalar1=-1.0, scalar2=None, op0=ALU.mult), reads=[SIN_b], writes=[SPM_b])
        u32 = [S.sb(f"ssm_u32_{i}", [128, T], F32) for i in range(2)]
        u16 = [S.sb(f"ssm_u16_{i}", [128, T], BF16) for i in range(2)]
        t1b = [S.sb(f"ssm_t1_{i}", [128, T], BF16) for i in range(2)]
        t2b = [S.sb(f"ssm_t2_{i}", [128, T], BF16) for i in range(2)]
        rb = [S.sb(f"ssm_r_{i}", [128, T], F32) for i in range(2)]
        m1b = [S.sb(f"ssm_m1_{i}", [128, T], BF16) for i in range(2)]
        m2b = [S.sb(f"ssm_m2_{i}", [128, T], BF16) for i in range(2)]
        rl_t, rl_b_ = S.sb("ssm_rlast", [128, 8], F32)
        rlb = [S.buf(f"rl{g}") for g in range(8)]
        ini = [S.sb(f"ssm_ini{g}", [128, 1], F32) for g in range(8)]
        yb = [S.sb(f"ssm_y_{i}", [128, T], F32) for i in range(2)]
        gt = [S.sb(f"ssm_g_{i}", [128, T], F32) for i in range(2)]
        pA = [S.ps(f"ssm_pA{i}", [128, T], F32) for i in range(2)]
        pB = [S.ps(f"ssm_pB{i}", [128, T], F32) for i in range(2)]
        pY = [S.ps(f"ssm_pY{i}", [128, T], F32) for i in range(1)]
        bpb = [S.ps(f"ssm_pC{i}", [128, T], F32) for i in range(2)]
        pI = pX
        for g in range(8):
            S.op("pool", lambda: G.memset(ini[g][0][:], 0.0), writes=[ini[g][1]])
        seq = [(n, g) for n in range(NBLK) for g in range(8)]

        t1c = [S.sb(f"ssm_t1c_{i}", [128, T], BF16) for i in range(3)]
        t2c = [S.sb(f"ssm_t2c_{i}", [128, T], BF16) for i in range(3)]

        def stageA1(k):
            n, g = seq[k]
            (u_t, u_b) = u32[n % 2]; (uh_t, uh_b) = u16[n % 2]
            if g == 0:
                S.dma("sp", lambda: nc.sync.dma_start(out=u_t[:], in_=usT[:, n * T:(n + 1) * T]), writes=[u_b])
                S.op("act", lambda: A.copy(uh_t[:], u_t[:]), reads=[u_b], writes=[uh_b])
            (pa_t, pa_b) = pA[k % 2]; (pb_t, pb_b) = pB[k % 2]
            (t1_t, t1_b) = t1c[k % 3]; (t2_t, t2_b) = t2c[k % 3]
            S.op("pe", lambda: PE.matmul(pa_t[:], lhsT=LB_t[:, g, 0, :], rhs=uh_t[:], start=True, stop=True), reads=[LB_b, uh_b], writes=[pa_b])
            S.op("pe", lambda: PE.matmul(pb_t[:], lhsT=LB_t[:, g, 1, :], rhs=uh_t[:], start=True, stop=True), reads=[LB_b, uh_b], writes=[pb_b])
            S.op("dve", lambda: V.tensor_tensor(out=t1_t[:], in0=pa_t[:], in1=COS_t[:, g, :], op=ALU.mult), reads=[pa_b, COS_b], writes=[t1_b])
            S.op("dve", lambda: V.tensor_tensor(out=t2_t[:], in0=pb_t[:], in1=SPM_t[:, g, :], op=ALU.mult), reads=[pb_b, SPM_b], writes=[t2_b])

        def stageA2(k):
            (t1_t, t1_b) = t1c[k % 3]; (t2_t, t2_b) = t2c[k % 3]
            (bp_t, bp_b) = bpb[k % 2]
            S.op("pe", lambda: PE.matmul(bp_t[:], lhsT=idt[:], rhs=t1_t[:], start=True, stop=False), reads=[idb, t1_b], writes=[bp_b])
            S.op("pe", lambda: PE.matmul(bp_t[:], lhsT=idt[:], rhs=t2_t[:], start=False, stop=True), reads=[idb, t2_b], writes=[bp_b])

        def stageB(k):
            n, g = seq[k]
            (bp_t, bp_b) = bpb[k % 2]; (r_t, r_b) = rb[k % 2]
            (m1_t, m1_b) = m1b[k % 2]; (m2_t, m2_b) = m2b[k % 2]
            if n > 0:
                S.op("pe", lambda: PE.matmul(pI[0][:, 384 + g:385 + g], lhsT=ROT_t[:, g, :], rhs=rl_t[:, g:g + 1], start=True, stop=True), reads=[ROT_b, rlb[g]], writes=[pI[1]])
                S.op("act", lambda: A.copy(ini[g][0][:], pI[0][:, 384 + g:385 + g]), reads=[pI[1]], writes=[ini[g][1]])
            S.op("dve", lambda: V.tensor_tensor_scan(out=r_t[:], data0=RHO_t[:, g, :], data1=bp_t[:], initial=ini[g][0][:, 0:1], op0=ALU.mult, op1=ALU.add), reads=[RHO_b, bp_b, ini[g][1]], writes=[r_b])
            S.op("act", lambda: A.copy(rl_t[:, g:g + 1], r_t[:, T - 1:T]), reads=[r_b], writes=[rlb[g]])
            S.op("pool", lambda: G.tensor_tensor(out=m1_t[:], in0=r_t[:], in1=COS_t[:, g, :], op=ALU.mult), reads=[r_b, COS_b], writes=[m1_b])
            S.op("pool", lambda: G.tensor_tensor(out=m2_t[:], in0=r_t[:], in1=SIN_t[:, g, :], op=ALU.mult), reads=[r_b, SIN_b], writes=[m2_b])

        def stageC(k):
            n, g = seq[k]
            (u_t, u_b) = u32[n % 2]
            (py_t, py_b) = pY[0]
            (m1_t, m1_b) = m1b[k % 2]; (m2_t, m2_b) = m2b[k % 2]
            S.op("pe", lambda: PE.matmul(py_t[:], lhsT=WC_t[:, g, 0, :], rhs=m1_t[:], start=(g == 0), stop=False), reads=[WC_b, m1_b], writes=[py_b])
            S.op("pe", lambda: PE.matmul(py_t[:], lhsT=WC_t[:, g, 1, :], rhs=m2_t[:], start=False, stop=(g == 7)), reads=[WC_b, m2_b], writes=[py_b])
            if g == 7:
                (y_t, y_b) = yb[n % 2]; (g_t, g_b) = gt[n % 2]
                S.op("dve", lambda: V.scalar_tensor_tensor(out=y_t[:], in0=u_t[:], scalar=d_t[:, 0:1], in1=py_t[:], op0=ALU.mult, op1=ALU.add), reads=[u_b, d_b, py_b], writes=[y_b])
                S.op("dve", lambda: V.tensor_tensor(out=g_t[:], in0=y_t[:], in1=y_t[:], op=ALU.mult), reads=[y_b], writes=[g_b])
                S.op("dve", lambda: V.tensor_scalar(out=g_t[:], in0=g_t[:], scalar1=0.044715, scalar2=1.0, op0=ALU.mult, op1=ALU.add), reads=[g_b], writes=[g_b])
                S.op("dve", lambda: V.tensor_tensor(out=g_t[:], in0=g_t[:], in1=y_t[:], op=ALU.mult), reads=[g_b, y_b], writes=[g_b])
                S.op("act", lambda: A.activation(out=g_t[:], in_=g_t[:], func=AF.Sigmoid, scale=GELU_C), reads=[g_b], writes=[g_b])
                S.op("dve", lambda: V.tensor_tensor(out=y_t[:], in0=y_t[:], in1=g_t[:], op=ALU.mult), reads=[g_b, y_b], writes=[y_b])
                S.dma("sp", lambda: nc.sync.dma_start(out=ysT[:, n * T:(n + 1) * T], in_=y_t[:]), reads=[y_b], writes=[ysT_b])

        NK = len(seq)
        for it in range(NK + 3):
            if it < NK:
                stageA1(it)
            if 0 <= it - 1 < NK:
                stageA2(it - 1)
            if 0 <= it - 2 < NK:
                stageB(it - 2)
            if 0 <= it - 3 < NK:
                stageC(it - 3)
        S.barrier()
        S.es = es_save


def emit_pool(S, nc, L, upT, pw, psc, pe_, pinvw, pfix, ypT, ypT_b, CH=2048):
    NCH = L // CH
    with ExitStack() as es2:
        es_save = S.es
        S.es = es2
        V = nc.vector; G = nc.gpsimd; A = nc.scalar; PE = nc.tensor
        pw32_t, pw32_b = S.sb("pl_w32", [128, 128], F32)
        pw_t, pw_b = S.sb("pl_w", [128, 128], BF16)
        S.dma("sp", lambda: nc.sync.dma_start(out=pw32_t[:], in_=pw[:, :]), writes=[pw32_b])
        S.op("dve", lambda: V.tensor_copy(pw_t[:], pw32_t[:]), reads=[pw32_b], writes=[pw_b])
        c_t, c_b = S.sb("pl_c", [128, 32], F32)
        S.dma("sp", lambda: nc.sync.dma_start(out=c_t[:, 0:1], in_=psc[:, :]), writes=[c_b])
        S.dma("sp", lambda: nc.sync.dma_start(out=c_t[:, 1:2], in_=pinvw[:, :]), writes=[c_b])
        S.dma("sp", lambda: nc.sync.dma_start(out=c_t[:, 2:6], in_=pe_[:, :]), writes=[c_b])
        S.dma("sp", lambda: nc.sync.dma_start(out=c_t[:, 16:32], in_=pfix[:, :]), writes=[c_b])
        e0 = [S.sb(f"pl_e0_{i}", [128, CH + 16], F32) for i in range(2)]
        e1 = S.sb("pl_e1", [128, CH + 16], F32); e2 = S.sb("pl_e2", [128, CH + 16], F32)
        dT = S.sb("pl_dT", [128, CH], BF16)
        tf = S.sb("pl_tf", [128, 16], F32)
        stg = [S.sb(f"pl_stg{i}", [128, 512], F32) for i in range(2)]
        pP = [S.ps(f"pl_p{i}", [128, 512], F32) for i in range(2)]
        nb = 0
        for ch in range(NCH):
            (x_t, x_b) = e0[ch % 2]
            S.dma("sp", lambda: nc.sync.dma_start(out=x_t[:, 16:], in_=upT[:, ch * CH:(ch + 1) * CH]), writes=[x_b])
            if ch == 0:
                S.op("pool", lambda: G.memset(x_t[:, 0:16], 0.0), writes=[x_b])
            else:
                (xp_t, xp_b) = e0[(ch - 1) % 2]
                S.op("pool", lambda: G.tensor_copy(x_t[:, 0:16], xp_t[:, CH:CH + 16]), reads=[xp_b], writes=[x_b])
            src = (x_t, x_b)
            for lvl, sft in enumerate((1, 2, 4, 8)):
                dst = e1 if lvl % 2 == 0 else e2
                S.op("dve", lambda: V.scalar_tensor_tensor(out=dst[0][:, sft:], in0=src[0][:, 0:CH + 16 - sft], scalar=c_t[:, 2 + lvl:3 + lvl], in1=src[0][:, sft:], op0=ALU.mult, op1=ALU.add), reads=[src[1], c_b], writes=[dst[1]])
                S.op("pool", lambda: G.tensor_copy(dst[0][:, 0:sft], src[0][:, 0:sft]), reads=[src[1]], writes=[dst[1]])
                src = dst
            S.op("dve", lambda: V.scalar_tensor_tensor(out=dT[0][:], in0=src[0][:, 16:], scalar=c_t[:, 1:2], in1=x_t[:, 16:], op0=ALU.mult, op1=ALU.subtract), reads=[src[1], c_b, x_b], writes=[dT[1]])
            if ch == 0:
                S.op("dve", lambda: V.tensor_tensor(out=tf[0][:], in0=src[0][:, 16:32], in1=c_t[:, 16:32], op=ALU.mult), reads=[src[1], c_b], writes=[tf[1]])
                S.op("dve", lambda: V.tensor_tensor(out=dT[0][:, 0:16], in0=tf[0][:], in1=x_t[:, 16:32], op=ALU.subtract), reads=[tf[1], x_b, dT[1]], writes=[dT[1]])
            for q in range(CH // 512):
                (p_t, p_b) = pP[nb % 2]; (s_t, s_b) = stg[nb % 2]; nb += 1
                S.op("pe", lambda: PE.matmul(p_t[:], lhsT=pw_t[:], rhs=dT[0][:, q * 512:(q + 1) * 512], start=True, stop=True), reads=[pw_b, dT[1]], writes=[p_b])
                S.op("act", lambda: A.activation(out=s_t[:], in_=p_t[:], func=AF.Copy, scale=c_t[:, 0:1]), reads=[p_b, c_b], writes=[s_b])
                S.dma("sp", lambda: nc.sync.dma_start(out=ypT[:, ch * CH + q * 512: ch * CH + (q + 1) * 512], in_=s_t[:]), reads=[s_b], writes=[ypT_b])
        S.barrier()
        S.es = es_save


def build_B(L, parts=("att", "ssm", "pool")):
    nc = bass.Bass("TRN2", target_bir_lowering=False)
    di = lambda n, s, d=F32: nc.dram_tensor(n, list(s), d, kind="ExternalInput").ap()
    do = lambda n, s, d=F32: nc.dram_tensor(n, list(s), d, kind="ExternalOutput").ap()
    QT = di("QT", [2, 192, L], BF16); KT = di("KT", [2, 128, L], BF16)
    kpeT = di("kpeT", [64, L], BF16); V = di("V", [L, 2, 128], BF16)
    usT = di("usT", [128, L]); lamre = di("lamre", [128, 8]); lamim = di("lamim", [128, 8]); logdt = di("logdt", [128, 8])
    bre = di("bre", [64, 128]); bim = di("bim", [64, 128]); cA = di("cA", [128, 128]); cB = di("cB", [128, 128]); dsk = di("dsk", [128, 1])
    upT = di("upT", [128, L]); pw = di("pw", [128, 128]); psc = di("psc", [128, 1]); pe_ = di("pe", [128, 4]); pinvw = di("pinvw", [128, 1]); pfix = di("pfix", [128, 16])
    ymT = do("ymT", [256, L]); ysT = do("ysT", [128, L]); ypT = do("ypT", [128, L])
    with ExitStack() as es:
        S = Sched(nc, es)
        idt, idb = S.sb("idt", [128, 128], BF16)
        make_ident(S, nc, idt, idb)
        ones_t, ones_b = S.sb("ones", [128, 128], BF16)
        S.op("pool", lambda: nc.gpsimd.memset(ones_t[:], 1.0), writes=[ones_b])
        ymT_b = S.buf("ymT"); ysT_b = S.buf("ysT")
        ypT_b = S.buf("ypT")
        outs = [ymT_b, ysT_b, ypT_b]
        if "pool" in parts:
            emit_pool(S, nc, L, upT, pw, psc, pe_, pinvw, pfix, ypT, ypT_b, CH=min(2048, L))
        if "ssm" in parts:
            emit_ssm(S, nc, L, usT, lamre, lamim, logdt, bre, bim, cA, cB, dsk, ysT, ysT_b, idt, idb)
        if "att" in parts:
            emit_attention(S, nc, L, QT, KT, kpeT, V, ymT, ymT_b, idt, idb, ones_t, ones_b)
        S.finish(outs)
        print("B ninstr", S.ninstr)
    return nc

import math

D = 2048
EPS = 1e-6
NE = 32
TS = 256


def build_C(NTOK, CAP, final, stage=99):
    nc = bass.Bass("TRN2", target_bir_lowering=False)
    NT = NTOK // 128
    NS = NTOK // TS
    NTILE = (2 * NTOK) // 128 + NE
    BIG = 1.0e6
    di = lambda n, s, d=F32: nc.dram_tensor(n, list(s), d, kind="ExternalInput").ap()
    do = lambda n, s, d=F32: nc.dram_tensor(n, list(s), d, kind="ExternalOutput").ap()
    x = di("x", [NTOK, D]); ypT = di("ypT", [512, NTOK]); ysT = di("ysT", [512, NTOK]); ymT = di("ymT", [1024, NTOK])
    cT = di("cT", [128, 16]); wada = di("wadaC", [D, 8192]); bada = di("badaC", [1, 8192])
    wglu = di("wglu", [512, 512]); gn = di("gn", [128, 16]); wout = di("wout", [D, D]); g2 = di("g2", [1, D])
    wr = di("wr", [D, 36]); br = di("br", [1, 36])
    wg = di("wg", [NE, D, 512]); wu = di("wu", [NE, D, 512]); wd = di("wd", [NE, 512, D])
    gfin = di("gfin", [1, D])
    xo = do("xo", [NTOK, D])
    cnt_o = do("cnt", [128, NE])
    Xg = nc.dram_tensor("Xg", [NTILE * 128, D], BF16, kind="Internal").ap()
    Yg = nc.dram_tensor("Yg", [NTILE * 128, D], F32, kind="Internal").ap()
    H2 = nc.dram_tensor("H2", [NTOK, D], BF16, kind="Internal").ap()
    x1 = nc.dram_tensor("x1", [NTOK, D], F32, kind="Internal").ap()
    V = nc.vector; G = nc.gpsimd; A = nc.scalar; PE = nc.tensor
    with ExitStack() as es:
        S = Sched(nc, es)
        xo_b = S.buf("xo"); Xg_b = S.buf("Xg"); Yg_b = S.buf("Yg"); x1_b = S.buf("x1"); H2_b = S.buf("H2")
        idt, idb = S.sb("idt", [128, 128], BF16)
        make_ident(S, nc, idt, idb)
        ones_t, ones_b = S.sb("ones", [128, 128], BF16)
        S.op("pool", lambda: G.memset(ones_t[:], 1.0), writes=[ones_b])
        onesf_t, onesf_b = S.sb("onesf", [128, 128], F32)
        S.op("pool", lambda: G.memset(onesf_t[:], 1.0), writes=[onesf_b])
        tri_t, tri_b = S.sb("tri", [128, 128], F32)
        S.op("pool", lambda: G.memset(tri_t[:], 1.0), writes=[tri_b])
        S.op("pool", lambda: G.affine_select(out=tri_t[:], in_=tri_t[:], pattern=[[1, 128]], compare_op=ALU.is_gt, fill=0.0, base=0, channel_multiplier=-1), reads=[tri_b], writes=[tri_b])
        cst_t, cst_b = S.sb("cst", [128, 4], F32)
        S.op("pool", lambda: G.memset(cst_t[:, 0:1], EPS), writes=[cst_b])
        idxW_t, idxW_b = S.sb("idxW", [128, 4, NTILE], I32)
        mcum_t, mcum_b = S.sb("mcum", [128, NE], F32)
        Mall_t, Mall_b = S.sb("Mall", [128, 2, NT, NE], F32)
        rt_t, rt_b = S.sb("rt", [128, NT, 4], F32)
        sl_t, sl_b = S.sb("sl", [128, NT, 2], I32)
        gF_t, gF_b = S.sb("gF", [128, D], F32)
        with ExitStack() as es2:
            S.es = es2
            gA_t, gA_b = S.sb("gA", [128, D], F32)
            gm2_t, gm2_b = S.sb("gm2", [128, D], F32)
            shF_t, shF_b = S.sb("shF", [128, D], F32)
            xt = [S.sb(f"xt{i}", [128, D], F32) for i in range(2)]
            xn = [S.sb(f"xn{i}", [128, D], F32) for i in range(2)]
            pZ = [S.ps(f"pZ{i}", [128, 512], F32) for i in range(2)]
            pG = S.ps("pG", [128, 512], F32)
            pSS = S.ps("pSS", [128, 512], F32)
            pT = [S.ps(f"pT{i}", [128, 1024], BF16) for i in range(2)]
            pR = S.ps("pR", [128, 512], F32)
            cB_t, cB_b = S.sb("cB", [128, 16, 128], F32)
            cT_t, cT_b = S.sb("cTt", [128, 16], F32)
            S.dma("sp", lambda: nc.sync.dma_start(out=cT_t[:], in_=cT[:, :]), writes=[cT_b])
            S.op("dve", lambda: V.tensor_copy(cB_t[:], cT_t[:].unsqueeze(2).to_broadcast([128, 16, 128])), reads=[cT_b], writes=[cB_b])
            tg = [(gA_t, gA_b), (shF_t, shF_b), (gm2_t, gm2_b), (gF_t, gF_b)]
            for i, (t_, b_) in enumerate(tg):
                S.dma("sp", lambda: nc.sync.dma_start(out=t_[:], in_=bada[0:1, i * D:(i + 1) * D].partition_broadcast(128)), writes=[b_])
            wada_v = wada.rearrange("(c p) n -> p c n", p=128)
            nsl = 0
            for blk in range(16):
                pz_t, pz_b = pZ[blk % 2]
                for q4 in range(4):
                    (w_t, w_b) = xt[nsl % 2]; nsl += 1
                    wv_ = w_t[:].rearrange("p (c n) -> p c n", c=4)
                    S.dma("sp", lambda: nc.sync.dma_start(out=wv_, in_=wada_v[:, q4 * 4:(q4 + 1) * 4, blk * 512:(blk + 1) * 512]), writes=[w_b])
                    for cc in range(4):
                        c = q4 * 4 + cc
                        S.op("pe", lambda: PE.matmul(pz_t[:], lhsT=cB_t[:, c, :], rhs=wv_[:, cc, :], start=(c == 0), stop=(c == 15)), reads=[cB_b, w_b], writes=[pz_b])
                tgt_t, tgt_b = tg[blk // 4]
                cs = slice((blk % 4) * 512, (blk % 4 + 1) * 512)
                S.op("dve", lambda: V.tensor_tensor(out=tgt_t[:, cs], in0=pz_t[:], in1=tgt_t[:, cs], op=ALU.add), reads=[pz_b, tgt_b], writes=[tgt_b])
            (g_t, g_b) = xt[nsl % 2]; nsl += 1
            S.dma("sp", lambda: nc.sync.dma_start(out=g_t[:], in_=g2[0:1, :].partition_broadcast(128)), writes=[g_b])
            S.op("dve", lambda: V.scalar_tensor_tensor(out=gm2_t[:], in0=gm2_t[:], scalar=1.0, in1=g_t[:], op0=ALU.add, op1=ALU.mult), reads=[gm2_b, g_b], writes=[gm2_b])
            if stage < 1:
                S.finish([gA_b, gm2_b, shF_b, gF_b]); return nc
            wout_t, wout_b = S.sb("wout", [128, 16, D], BF16)
            wout_v = wout.rearrange("(c p) n -> p c n", p=128)
            for c in range(16):
                S.dma("pool", lambda: G.dma_start(out=wout_t[:, c, :], in_=wout_v[:, c, :]), writes=[wout_b])
            wglu_t, wglu_b = S.sb("wglu", [128, 4, 512], BF16)
            S.dma("pool", lambda: G.dma_start(out=wglu_t[:], in_=wglu.rearrange("(c p) n -> p c n", p=128)), writes=[wglu_b])
            wr_t, wr_b = S.sb("wrt", [128, 16, 36], BF16)
            S.dma("pool", lambda: G.dma_start(out=wr_t[:], in_=wr.rearrange("(c p) n -> p c n", p=128)), writes=[wr_b])
            br_t, br_b = S.sb("brt", [128, 36], F32)
            S.dma("sp", lambda: nc.sync.dma_start(out=br_t[:], in_=br[0:1, :].partition_broadcast(128)), writes=[br_b])
            gn_t, gn_b = S.sb("gnt", [128, 16], F32)
            S.dma("sp", lambda: nc.sync.dma_start(out=gn_t[:], in_=gn[:, :]), writes=[gn_b])
            yr_t, yr_b = S.sb("yr", [128, 16, TS], F32)
            yrb = [S.buf(f"yr{i}") for i in range(16)]
            ysb_t, ysb_b = S.sb("ysb", [128, 4, TS], BF16)
            sq_t, sq_b = S.sb("sq", [128, D], BF16)
            sg_t, sg_b = S.sb("sg", [128, TS], F32)
            rstd = [S.sb(f"rstd{i}", [128, TS], F32) for i in range(3)]
            yn_t, yn_b = S.sb("yn", [128, 16, TS], BF16)
            tmpo = [S.sb(f"tmpo{i}", [128, 512], F32) for i in range(2)]
            h2 = [S.sb(f"h2_{i}", [128, D], BF16) for i in range(2)]
            h2T_t, h2T_b = S.sb("h2T", [128, 16, 128], BF16)
            st = [S.sb(f"st{i}", [128, 1], F32) for i in range(4)]
            H2_v = H2.rearrange("(n p) d -> n p d", p=128)
            S.op("pool", lambda: G.memset(mcum_t[:], 0.0), writes=[mcum_b])
            R = {}
            for n_, w_ in (("lg", 36), ("gmax", 1), ("ngmax", 1), ("goh", 4), ("gex", 4), ("gsum", 1), ("gw", 1), ("el", 8), ("m1", 1), ("oh1", 8),
                           ("el2", 8), ("m2", 1), ("oh2", 8), ("dd", 1), ("s1", 1), ("s2", 1), ("M1", NE), ("M2", NE), ("M", NE), ("po", NE), ("jk", NE)):
                R[n_] = S.sb("r_" + n_, [128, w_], F32)
            srcs = [(ypT, 0), (ypT, 1), (ypT, 2), (ypT, 3), (ysT, 0), (ysT, 1), (ysT, 2), (ysT, 3)] + [(ymT, i) for i in range(8)]
            grp_of = [0] * 4 + [1] * 4 + [2] * 8
            gdim = [512.0, 512.0, 1024.0]
            x_v = x.rearrange("(n p) d -> n p d", p=128)
            x1_v = x1.rearrange("(n p) d -> n p d", p=128)
            nst = 0
            ntile = 0
            npz = 0
            for s in range(NS):
                t0 = s * TS
                for i, (src, bi) in enumerate(srcs):
                    S.dma("sp", lambda: nc.sync.dma_start(out=yr_t[:, i, :], in_=src[bi * 128:(bi + 1) * 128, t0:t0 + TS]), writes=[yrb[i]], track=yrb[i])
                for i in range(4):
                    S.op("act", lambda: A.copy(ysb_t[:, i, :], yr_t[:, 4 + i, :]), reads=[yrb[4 + i]], writes=[ysb_b])
                for j in range(4):
                    for i in range(4):
                        S.op("pe", lambda: PE.matmul(pG[0][:, 0:TS], lhsT=wglu_t[:, i, j * 128:(j + 1) * 128], rhs=ysb_t[:, i, :], start=(i == 0), stop=(i == 3)), reads=[wglu_b, ysb_b], writes=[pG[1]])
                    S.op("act", lambda: A.activation(out=sg_t[:], in_=pG[0][:, 0:TS], func=AF.Sigmoid), reads=[pG[1]], writes=[sg_b])
                    S.op("dve", lambda: V.tensor_tensor(out=yr_t[:, 4 + j, :], in0=yr_t[:, 4 + j, :], in1=sg_t[:], op=ALU.mult), reads=[yrb[4 + j], sg_b], writes=[yrb[4 + j]])
                for gi, (b0, nb) in enumerate(((0, 4), (4, 4), (8, 8))):
                    for i in range(nb):
                        S.op("act", lambda: A.activation(out=sq_t[:, i * TS:(i + 1) * TS], in_=yr_t[:, b0 + i, :], func=AF.Square), reads=[yrb[b0 + i]], writes=[sq_b])
                    for i in range(nb):
                        S.op("pe", lambda: PE.matmul(pSS[0][:, 0:TS], lhsT=ones_t[:], rhs=sq_t[:, i * TS:(i + 1) * TS], start=(i == 0), stop=(i == nb - 1)), reads=[ones_b, sq_b], writes=[pSS[1]])
                    (rs_t, rs_b) = rstd[gi]
                    S.op("act", lambda: A.activation(out=rs_t[:], in_=pSS[0][:, 0:TS], func=AF.Sqrt, scale=1.0 / gdim[gi], bias=cst_t[:, 0:1]), reads=[pSS[1], cst_b], writes=[rs_b])
                    S.op("dve", lambda: V.reciprocal(rs_t[:], rs_t[:]), reads=[rs_b], writes=[rs_b])
                    for i in range(nb):
                        blk = b0 + i
                        eng = "dve" if i % 2 == 0 else "dve"
                        S.op("dve", lambda: V.scalar_tensor_tensor(out=yn_t[:, blk, :], in0=yr_t[:, blk, :], scalar=gn_t[:, blk:blk + 1], in1=rs_t[:], op0=ALU.mult, op1=ALU.mult), reads=[yrb[blk], gn_b, rs_b], writes=[yn_b])
                for j in range(TS // 128):
                    ti = s * (TS // 128) + j
                    (x_t, x_b) = xt[nsl % 2]; nsl += 1
                    (n_t, n_b) = xn[ti % 2]
                    (h_t, h_b) = h2[ti % 2]
                    S.dma("sp", lambda: nc.sync.dma_start(out=x_t[:], in_=x_v[ti]), writes=[x_b])
                    for q in range(4):
                        (pz_t, pz_b) = pZ[npz % 2]; (to_t, to_b) = tmpo[npz % 2]; npz += 1
                        cs = slice(q * 512, (q + 1) * 512)
                        for c in range(16):
                            S.op("pe", lambda: PE.matmul(pz_t[:], lhsT=yn_t[:, c, j * 128:(j + 1) * 128], rhs=wout_t[:, c, cs], start=(c == 0), stop=(c == 15)), reads=[yn_b, wout_b], writes=[pz_b])
                        S.op("dve", lambda: V.tensor_tensor(out=to_t[:], in0=pz_t[:], in1=gA_t[:, cs], op=ALU.mult), reads=[pz_b, gA_b], writes=[to_b])
                        S.op("pool", lambda: G.tensor_tensor(out=n_t[:, cs], in0=to_t[:], in1=x_t[:, cs], op=ALU.add), reads=[to_b, x_b], writes=[n_b])
                    S.dma("sp", lambda: nc.sync.dma_start(out=x1_v[ti], in_=n_t[:]), reads=[n_b], writes=[x1_b])
                    (ss_t, ss_b) = st[nst % 4]; nst += 1
                    (rr_t, rr_b) = st[nst % 4]; nst += 1
                    S.op("act", lambda: A.activation(out=sq_t[:], in_=n_t[:], func=AF.Square, accum_out=ss_t[:]), reads=[n_b], writes=[sq_b, ss_b])
                    S.op("act", lambda: A.activation(out=rr_t[:], in_=ss_t[:], func=AF.Sqrt, scale=1.0 / D, bias=cst_t[:, 0:1]), reads=[ss_b, cst_b], writes=[rr_b])
                    S.op("dve", lambda: V.reciprocal(rr_t[:], rr_t[:]), reads=[rr_b], writes=[rr_b])
                    S.op("dve", lambda: V.scalar_tensor_tensor(out=x_t[:], in0=n_t[:], scalar=rr_t[:, 0:1], in1=gm2_t[:], op0=ALU.mult, op1=ALU.mult), reads=[n_b, rr_b, gm2_b], writes=[x_b])
                    S.op("pool", lambda: G.tensor_tensor(out=h_t[:], in0=x_t[:], in1=shF_t[:], op=ALU.add), reads=[x_b, shF_b], writes=[h_b])
                    for half in range(2):
                        (p_t, p_b) = pT[half]
                        for cc in range(8):
                            c = half * 8 + cc
                            S.op("pe", lambda: PE.transpose(p_t[:, cc * 128:(cc + 1) * 128], h_t[:, c * 128:(c + 1) * 128], idt[:]), reads=[h_b, idb], writes=[p_b])
                        dst = h2T_t[:, half * 8:(half + 1) * 8, :]
                        src_ = p_t[:].rearrange("p (c n) -> p c n", c=8)
                        if half == 0:
                            S.op("act", lambda: A.copy(dst, src_), reads=[p_b], writes=[h2T_b])
                        else:
                            S.op("dve", lambda: V.tensor_copy(dst, src_), reads=[p_b], writes=[h2T_b])
                    for c in range(16):
                        S.op("pe", lambda: PE.matmul(pR[0][:, 0:36], lhsT=h2T_t[:, c, :], rhs=wr_t[:, c, :], start=(c == 0), stop=(c == 15)), reads=[h2T_b, wr_b], writes=[pR[1]])
                    r = lambda n_: R[n_][0]
                    rb_ = lambda n_: R[n_][1]
                    def dv(fn, rd, wrn):
                        S.op("dve", fn, reads=[rb_(n_) if isinstance(n_, str) else n_ for n_ in rd], writes=[rb_(n_) if isinstance(n_, str) else n_ for n_ in wrn])
                    dv(lambda: V.tensor_tensor(out=r("lg")[:], in0=pR[0][:, 0:36], in1=br_t[:], op=ALU.add), [pR[1], br_b], ["lg"])
                    dv(lambda: V.reduce_max(out=r("gmax")[:], in_=r("lg")[:, 0:4], axis=AX.X), ["lg"], ["gmax"])
                    dv(lambda: V.tensor_scalar(out=r("goh")[:], in0=r("lg")[:, 0:4], scalar1=r("gmax")[:, 0:1], scalar2=None, op0=ALU.is_equal), ["lg", "gmax"], ["goh"])
                    dv(lambda: V.tensor_scalar(out=r("ngmax")[:], in0=r("gmax")[:], scalar1=-1.0, scalar2=None, op0=ALU.mult), ["gmax"], ["ngmax"])
                    S.op("act", lambda: A.activation(out=r("gex")[:], in_=r("lg")[:, 0:4], func=AF.Exp, bias=r("ngmax")[:, 0:1], accum_out=r("gsum")[:]), reads=[rb_("lg"), rb_("ngmax")], writes=[rb_("gex"), rb_("gsum")])
                    dv(lambda: V.reciprocal(r("gw")[:], r("gsum")[:]), ["gsum"], ["gw"])
                    dv(lambda: V.tensor_scalar(out=r("el")[:], in0=r("lg")[:, 4:12], scalar1=r("goh")[:, 0:1], scalar2=None, op0=ALU.mult), ["lg", "goh"], ["el"])
                    for g in range(1, 4):
                        dv(lambda: V.scalar_tensor_tensor(out=r("el")[:], in0=r("lg")[:, 4 + 8 * g:12 + 8 * g], scalar=r("goh")[:, g:g + 1], in1=r("el")[:], op0=ALU.mult, op1=ALU.add), ["lg", "goh", "el"], ["el"])
                    dv(lambda: V.reduce_max(out=r("m1")[:], in_=r("el")[:], axis=AX.X), ["el"], ["m1"])
                    dv(lambda: V.tensor_scalar(out=r("oh1")[:], in0=r("el")[:], scalar1=r("m1")[:, 0:1], scalar2=None, op0=ALU.is_equal), ["el", "m1"], ["oh1"])
                    dv(lambda: V.scalar_tensor_tensor(out=r("el2")[:], in0=r("oh1")[:], scalar=-1e30, in1=r("el")[:], op0=ALU.mult, op1=ALU.add), ["oh1", "el"], ["el2"])
                    dv(lambda: V.reduce_max(out=r("m2")[:], in_=r("el2")[:], axis=AX.X), ["el2"], ["m2"])
                    dv(lambda: V.tensor_scalar(out=r("oh2")[:], in0=r("el2")[:], scalar1=r("m2")[:, 0:1], scalar2=None, op0=ALU.is_equal), ["el2", "m2"], ["oh2"])
                    dv(lambda: V.tensor_tensor(out=r("dd")[:], in0=r("m2")[:], in1=r("m1")[:], op=ALU.subtract), ["m1", "m2"], ["dd"])
                    S.op("act", lambda: A.activation(out=r("s1")[:], in_=r("dd")[:], func=AF.Sigmoid, scale=-1.0), reads=[rb_("dd")], writes=[rb_("s1")])
                    S.op("act", lambda: A.activation(out=r("s2")[:], in_=r("dd")[:], func=AF.Sigmoid, scale=1.0), reads=[rb_("dd")], writes=[rb_("s2")])
                    dv(lambda: V.tensor_tensor(out=rt_t[:, ti, 2:3], in0=r("s1")[:], in1=r("gw")[:], op=ALU.mult), ["s1", "gw"], [rt_b])
                    dv(lambda: V.tensor_tensor(out=rt_t[:, ti, 3:4], in0=r("s2")[:], in1=r("gw")[:], op=ALU.mult), ["s2", "gw"], [rt_b])
                    gohb = r("goh")[:].unsqueeze(2).to_broadcast([128, 4, 8])
                    for (mn, ohn) in (("M1", "oh1"), ("M2", "oh2")):
                        ohb = r(ohn)[:].unsqueeze(1).to_broadcast([128, 4, 8])
                        dv(lambda: V.tensor_tensor(out=r(mn)[:].rearrange("p (g e) -> p g e", g=4), in0=gohb, in1=ohb, op=ALU.mult), ["goh", ohn], [mn])
                    dv(lambda: V.tensor_tensor(out=r("M")[:], in0=r("M1")[:], in1=r("M2")[:], op=ALU.add), ["M1", "M2"], ["M"])
                    S.op("pe", lambda: PE.matmul(pR[0][:, 64:64 + NE], lhsT=tri_t[:], rhs=r("M")[:], start=True, stop=False), reads=[tri_b, rb_("M")], writes=[pR[1]])
                    S.op("pe", lambda: PE.matmul(pR[0][:, 64:64 + NE], lhsT=onesf_t[:], rhs=mcum_t[:], start=False, stop=True), reads=[onesf_b, mcum_b], writes=[pR[1]])
                    dv(lambda: V.tensor_copy(r("po")[:], pR[0][:, 64:64 + NE]), [pR[1]], ["po"])
                    dv(lambda: V.tensor_tensor(out=mcum_t[:], in0=mcum_t[:], in1=r("M")[:], op=ALU.add), [mcum_b, "M"], [mcum_b])
                    for k_, mn in enumerate(("M1", "M2")):
                        dv(lambda: V.tensor_tensor(out=r("jk")[:], in0=r(mn)[:], in1=r("po")[:], op=ALU.mult), [mn, "po"], ["jk"])
                        dv(lambda: V.reduce_sum(out=rt_t[:, ti, k_:k_ + 1], in_=r("jk")[:], axis=AX.X), ["jk"], [rt_b])
                        dv(lambda: V.tensor_copy(Mall_t[:, k_, ti, :], r(mn)[:]), [mn], [Mall_b])
                    S.dma("sp", lambda: nc.sync.dma_start(out=H2_v[ti], in_=h_t[:]), reads=[h_b], writes=[H2_b])
                    ntile += 1
            cnt_b = S.buf("cnt")
            S.dma("sp", lambda: nc.sync.dma_start(out=cnt_o[:, :], in_=mcum_t[:]), reads=[mcum_b], writes=[cnt_b])
            S.barrier()
            S.es = es
        with ExitStack() as es2b:
            S.es = es2b
            H2_v = H2.rearrange("(n p) d -> n p d", p=128)
            pR = S.ps("pR2", [128, 512], F32)
            h2 = [S.sb(f"h2b_{i}", [128, D], BF16) for i in range(2)]
            T = {}
            for n_, shp in (("cnt", [128, NE]), ("ntl", [128, NE]), ("pend", [128, NE]), ("pst", [128, NE]), ("one", [128, NE]), ("thr", [128, 64]),
                            ("cmp", [128, NE, 64]), ("tau", [128, NTILE]), ("texp", [128, NTILE]), ("chg", [128, NTILE]), ("idf", [128, NTILE]),
                            ("pid", [128, 1]), ("sadd", [128, 2, NT])):
                T[n_] = S.sb("t_" + n_, shp, F32)
            tq = lambda n_: T[n_][0]
            tb = lambda n_: T[n_][1]
            cmp2_t, cmp2_b = S.sb("t_cmp2", [128, NTILE, NE], F32)
            mp_t, mp_b = S.sb("t_mp", [128, 2, NT, NE], F32)
            S.op("pe", lambda: PE.matmul(pR[0][:, 128:128 + NE], lhsT=onesf_t[:], rhs=mcum_t[:], start=True, stop=True), reads=[onesf_b, mcum_b], writes=[pR[1]])
            S.op("dve", lambda: V.tensor_copy(tq("cnt")[:], pR[0][:, 128:128 + NE]), reads=[pR[1]], writes=[tb("cnt")])
            S.op("pool", lambda: G.iota(tq("thr")[:], pattern=[[128, 64]], base=0, channel_multiplier=0, allow_small_or_imprecise_dtypes=True), writes=[tb("thr")])
            S.op("pool", lambda: G.iota(tq("tau")[:], pattern=[[128, NTILE]], base=0, channel_multiplier=0, allow_small_or_imprecise_dtypes=True), writes=[tb("tau")])
            S.op("pool", lambda: G.iota(tq("pid")[:], pattern=[[0, 1]], base=0, channel_multiplier=1, allow_small_or_imprecise_dtypes=True), writes=[tb("pid")])
            S.op("pool", lambda: G.memset(tq("one")[:], 1.0), writes=[tb("one")])
            S.op("dve", lambda: V.tensor_tensor(out=tq("cmp")[:], in0=tq("cnt")[:].unsqueeze(2).to_broadcast([128, NE, 64]), in1=tq("thr")[:].unsqueeze(1).to_broadcast([128, NE, 64]), op=ALU.is_gt), reads=[tb("cnt"), tb("thr")], writes=[tb("cmp")])
            S.op("dve", lambda: V.tensor_reduce(out=tq("ntl")[:], in_=tq("cmp")[:], op=ALU.add, axis=AX.X), reads=[tb("cmp")], writes=[tb("ntl")])
            S.op("dve", lambda: V.tensor_scalar(out=tq("ntl")[:], in0=tq("ntl")[:], scalar1=128.0, scalar2=None, op0=ALU.mult), reads=[tb("ntl")], writes=[tb("ntl")])
            S.op("dve", lambda: V.tensor_tensor_scan(out=tq("pend")[:], data0=tq("one")[:], data1=tq("ntl")[:], initial=0.0, op0=ALU.mult, op1=ALU.add), reads=[tb("one"), tb("ntl")], writes=[tb("pend")])
            S.op("dve", lambda: V.tensor_tensor(out=tq("pst")[:], in0=tq("pend")[:], in1=tq("ntl")[:], op=ALU.subtract), reads=[tb("pend"), tb("ntl")], writes=[tb("pst")])
            S.op("dve", lambda: V.tensor_tensor(out=mp_t[:].rearrange("p k n e -> p (k n) e"), in0=Mall_t[:].rearrange("p k n e -> p (k n) e"), in1=tq("pst")[:].unsqueeze(1).to_broadcast([128, 2 * NT, NE]), op=ALU.mult), reads=[Mall_b, tb("pst")], writes=[mp_b])
            S.op("dve", lambda: V.tensor_reduce(out=tq("sadd")[:].rearrange("p k n -> p (k n)"), in_=mp_t[:].rearrange("p k n e -> p (k n) e"), op=ALU.add, axis=AX.X), reads=[mp_b], writes=[tb("sadd")])
            for k_ in range(2):
                S.op("dve", lambda: V.tensor_tensor(out=rt_t[:, :, k_], in0=rt_t[:, :, k_], in1=tq("sadd")[:, k_, :], op=ALU.add), reads=[rt_b, tb("sadd")], writes=[rt_b])
            S.op("dve", lambda: V.tensor_copy(sl_t[:], rt_t[:, :, 0:2]), reads=[rt_b], writes=[sl_b])
            S.op("dve", lambda: V.tensor_tensor(out=cmp2_t[:], in0=tq("tau")[:].unsqueeze(2).to_broadcast([128, NTILE, NE]), in1=tq("pend")[:].unsqueeze(1).to_broadcast([128, NTILE, NE]), op=ALU.is_ge), reads=[tb("tau"), tb("pend")], writes=[cmp2_b])
            S.op("dve", lambda: V.tensor_reduce(out=tq("texp")[:], in_=cmp2_t[:], op=ALU.add, axis=AX.X), reads=[cmp2_b], writes=[tb("texp")])
            S.op("dve", lambda: V.tensor_scalar(out=tq("texp")[:], in0=tq("texp")[:], scalar1=float(NE - 1), scalar2=None, op0=ALU.min), reads=[tb("texp")], writes=[tb("texp")])
            S.op("pool", lambda: G.memset(tq("chg")[:, 0:1], 1.0), writes=[tb("chg")])
            S.op("dve", lambda: V.tensor_tensor(out=tq("chg")[:, 1:NTILE], in0=tq("texp")[:, 1:NTILE], in1=tq("texp")[:, 0:NTILE - 1], op=ALU.not_equal), reads=[tb("texp"), tb("chg")], writes=[tb("chg")])
            S.op("dve", lambda: V.tensor_scalar(out=tq("idf")[:], in0=tq("texp")[:], scalar1=128.0, scalar2=tq("pid")[:, 0:1], op0=ALU.mult, op1=ALU.add), reads=[tb("texp"), tb("pid")], writes=[tb("idf")])
            S.op("dve", lambda: V.scalar_tensor_tensor(out=tq("idf")[:], in0=tq("idf")[:], scalar=-BIG, in1=tq("chg")[:], op0=ALU.add, op1=ALU.mult), reads=[tb("idf"), tb("chg")], writes=[tb("idf")])
            S.op("dve", lambda: V.tensor_scalar(out=tq("idf")[:], in0=tq("idf")[:], scalar1=BIG, scalar2=None, op0=ALU.add), reads=[tb("idf")], writes=[tb("idf")])
            for a_ in range(4):
                S.op("dve", lambda: V.tensor_scalar(out=tq("tau")[:], in0=tq("idf")[:], scalar1=4.0, scalar2=float(a_), op0=ALU.mult, op1=ALU.add), reads=[tb("idf"), idxW_b], writes=[tb("tau")])
                S.op("dve", lambda: V.tensor_copy(idxW_t[:, a_, :], tq("tau")[:]), reads=[tb("tau")], writes=[idxW_b])
            for ti in range(NT):
                (h_t, h_b) = h2[ti % 2]
                S.dma("sp", lambda: nc.sync.dma_start(out=h_t[:], in_=H2_v[ti]), reads=[H2_b], writes=[h_b])
                for k_ in range(2):
                    S.dma("pool", lambda: G.indirect_dma_start(out=Xg[:, :], out_offset=bass.IndirectOffsetOnAxis(ap=sl_t[:, ti, k_:k_ + 1], axis=0), in_=h_t[:, :], in_offset=None), reads=[h_b, sl_b], writes=[Xg_b], track=h_b)
            S.barrier()
            S.es = es
        if stage < 2:
            S.finish([Xg_b, x1_b]); return nc
        with ExitStack() as es3:
            S.es = es3
            w1_t, w1_b = S.sb("w1", [128, 16 * 512], BF16)
            w3_t, w3_b = S.sb("w3", [128, 16 * 512], BF16)
            w2_t, w2_b = S.sb("w2", [128, 4 * D], BF16)
            xr = [S.sb(f"xr{i}", [128, D], BF16) for i in range(2)]
            XT = [S.sb(f"XT{i}", [128, 16, 128], BF16) for i in range(2)]
            sl_ = [S.sb(f"silu{i}", [128, 512], F32) for i in range(2)]
            ac = [S.sb(f"act{i}", [128, 512], BF16) for i in range(2)]
            aT = [S.sb(f"aT{i}", [128, 4, 128], BF16) for i in range(2)]
            yo = [S.sb(f"yo{i}", [128, D], F32) for i in range(2)]
            pT = [S.ps(f"eT{i}", [128, 1024], BF16) for i in range(2)]
            pH1 = [S.ps(f"eH1_{i}", [128, 512], F32) for i in range(2)]
            pH3 = [S.ps(f"eH3_{i}", [128, 512], F32) for i in range(2)]
            pY = [S.ps(f"eY{i}", [128, 512], F32) for i in range(2)]
            Xg_v = Xg.rearrange("(n p) d -> n p d", p=128)
            Yg_v = Yg.rearrange("(n p) d -> n p d", p=128)
            wg_v = wg.rearrange("e (p a c) n -> (e p a) (c n)", a=4, c=4)
            wu_v = wu.rearrange("e (p a c) n -> (e p a) (c n)", a=4, c=4)
            wd_v = wd.rearrange("e (p j) n -> (e p j) n", j=4)
            w1v = w1_t[:].rearrange("p (c n) -> p c n", c=16)
            w3v = w3_t[:].rearrange("p (c n) -> p c n", c=16)
            w2v = w2_t[:].rearrange("p (j n) -> p j n", j=4)
            ny = 0
            breg4 = G.to_reg(NE * 128 * 4 - 1)
            w1bs = [S.buf(f"w1b{a}") for a in range(4)]
            w3bs = [S.buf(f"w3b{a}") for a in range(4)]
            w2bs = [S.buf(f"w2b{a}") for a in range(4)]
            for tau in range(NTILE):
                (r_t, r_b) = xr[tau % 2]; (x_t, x_b) = XT[tau % 2]
                (h1_t, h1_b) = pH1[tau % 2]; (h3_t, h3_b) = pH3[tau % 2]
                (s_t, s_b) = sl_[tau % 2]; (a_t, a_b) = ac[tau % 2]; (at_t, at_b) = aT[tau % 2]
                (y_t, y_b) = yo[tau % 2]
                S.dma("sp", lambda: nc.sync.dma_start(out=r_t[:], in_=Xg_v[tau]), reads=[Xg_b], writes=[r_b])
                for a_ in range(4):
                    for (wt_, wbl_, src_) in ((w1_t, w1bs, wg_v), (w3_t, w3bs, wu_v)):
                        S.dma("pool", lambda: G.indirect_dma_start(out=wt_[:, a_ * 2048:(a_ + 1) * 2048], out_offset=None, in_=src_[:, :], in_offset=bass.IndirectOffsetOnAxis(ap=idxW_t[:, a_, tau:tau + 1], axis=0), bounds_check=breg4, oob_is_err=False), reads=[idxW_b], writes=[wbl_[a_]], track=wbl_[a_])
                for a_ in range(4):
                    S.dma("pool", lambda: G.indirect_dma_start(out=w2_t[:, a_ * 2048:(a_ + 1) * 2048], out_offset=None, in_=wd_v[:, :], in_offset=bass.IndirectOffsetOnAxis(ap=idxW_t[:, a_, tau:tau + 1], axis=0), bounds_check=breg4, oob_is_err=False), reads=[idxW_b], writes=[w2bs[a_]], track=w2bs[a_])
                rv = r_t[:].rearrange("s (p c) -> s c p", c=16)
                for half in range(2):
                    (p_t, p_b) = pT[half]
                    for cc in range(8):
                        c = half * 8 + cc
                        S.op("pe", lambda: PE.transpose(p_t[:, cc * 128:(cc + 1) * 128], rv[:, c, :], idt[:]), reads=[r_b, idb], writes=[p_b])
                    dst = x_t[:, half * 8:(half + 1) * 8, :]
                    src2 = p_t[:].rearrange("p (c n) -> p c n", c=8)
                    if half == 0:
                        S.op("act", lambda: A.copy(dst, src2), reads=[p_b], writes=[x_b])
                    else:
                        S.op("dve", lambda: V.tensor_copy(dst, src2), reads=[p_b], writes=[x_b])
                for c in range(16):
                    S.op("pe", lambda: PE.matmul(h1_t[:], lhsT=x_t[:, c, :], rhs=w1v[:, c, :], start=(c == 0), stop=(c == 15)), reads=[x_b, w1bs[c // 4]], writes=[h1_b])
                for c in range(16):
                    S.op("pe", lambda: PE.matmul(h3_t[:], lhsT=x_t[:, c, :], rhs=w3v[:, c, :], start=(c == 0), stop=(c == 15)), reads=[x_b, w3bs[c // 4]], writes=[h3_b])
                S.op("act", lambda: A.activation(out=s_t[:], in_=h1_t[:], func=AF.Silu), reads=[h1_b], writes=[s_b])
                S.op("dve", lambda: V.tensor_tensor(out=a_t[:], in0=h3_t[:], in1=s_t[:], op=ALU.mult), reads=[h3_b, s_b], writes=[a_b])
                av = a_t[:].rearrange("s (p j) -> s j p", j=4)
                (p_t, p_b) = pT[0]
                for j in range(4):
                    S.op("pe", lambda: PE.transpose(p_t[:, j * 128:(j + 1) * 128], av[:, j, :], idt[:]), reads=[a_b, idb], writes=[p_b])
                S.op("act", lambda: A.copy(at_t[:], p_t[:, 0:512].rearrange("p (j n) -> p j n", j=4)), reads=[p_b], writes=[at_b])
                for q in range(4):
                    (py_t, py_b) = pY[ny % 2]; ny += 1
                    for j in range(4):
                        S.op("pe", lambda: PE.matmul(py_t[:], lhsT=at_t[:, j, :], rhs=w2v[:, j, q * 512:(q + 1) * 512], start=(j == 0), stop=(j == 3)), reads=[at_b, w2bs[j]], writes=[py_b])
                    if q % 2 == 0:
                        S.op("act", lambda: A.copy(y_t[:, q * 512:(q + 1) * 512], py_t[:]), reads=[py_b], writes=[y_b])
                    else:
                        S.op("dve", lambda: V.tensor_copy(y_t[:, q * 512:(q + 1) * 512], py_t[:]), reads=[py_b], writes=[y_b])
                S.dma("sp", lambda: nc.sync.dma_start(out=Yg_v[tau], in_=y_t[:]), reads=[y_b], writes=[Yg_b])
            S.barrier()
            S.es = es
        if stage < 3:
            S.finish([Yg_b, x1_b]); return nc
        with ExitStack() as es4:
            S.es = es4
            y1 = [S.sb(f"y1_{i}", [128, D], F32) for i in range(2)]
            y2 = [S.sb(f"y2_{i}", [128, D], F32) for i in range(2)]
            xb = [S.sb(f"xb{i}", [128, D], F32) for i in range(2)]
            jq_t, jq_b = S.sb("jq", [128, D], BF16)
            st = [S.sb(f"fst{i}", [128, 1], F32) for i in range(4)]
            gfin_t, gfin_b = S.sb("gfin", [128, D], F32)
            if final:
                S.dma("sp", lambda: nc.sync.dma_start(out=gfin_t[:], in_=gfin[0:1, :].partition_broadcast(128)), writes=[gfin_b])
            x1_v = x1.rearrange("(n p) d -> n p d", p=128)
            xo_v = xo.rearrange("(n p) d -> n p d", p=128)
            nst = 0
            for ti in range(NT):
                (a_t, a_b) = y1[ti % 2]; (b_t, b_b) = y2[ti % 2]; (x_t, x_b) = xb[ti % 2]
                S.dma("pool", lambda: G.indirect_dma_start(out=a_t[:, :], out_offset=None, in_=Yg[:, :], in_offset=bass.IndirectOffsetOnAxis(ap=sl_t[:, ti, 0:1], axis=0)), reads=[Yg_b, sl_b], writes=[a_b])
                S.dma("pool", lambda: G.indirect_dma_start(out=b_t[:, :], out_offset=None, in_=Yg[:, :], in_offset=bass.IndirectOffsetOnAxis(ap=sl_t[:, ti, 1:2], axis=0)), reads=[Yg_b, sl_b], writes=[b_b])
                S.dma("sp", lambda: nc.sync.dma_start(out=x_t[:], in_=x1_v[ti]), reads=[x1_b], writes=[x_b])
                S.op("dve", lambda: V.tensor_scalar(out=a_t[:], in0=a_t[:], scalar1=rt_t[:, ti, 2:3], scalar2=None, op0=ALU.mult), reads=[a_b, rt_b], writes=[a_b])
                S.op("dve", lambda: V.scalar_tensor_tensor(out=a_t[:], in0=b_t[:], scalar=rt_t[:, ti, 3:4], in1=a_t[:], op0=ALU.mult, op1=ALU.add), reads=[a_b, b_b, rt_b], writes=[a_b])
                S.op("dve", lambda: V.tensor_tensor(out=a_t[:], in0=a_t[:], in1=gF_t[:], op=ALU.mult), reads=[a_b, gF_b], writes=[a_b])
                S.op("pool", lambda: G.tensor_tensor(out=x_t[:], in0=x_t[:], in1=a_t[:], op=ALU.add), reads=[a_b, x_b], writes=[x_b])
                if final:
                    (ss_t, ss_b) = st[nst % 4]; nst += 1
                    (rr_t, rr_b) = st[nst % 4]; nst += 1
                    S.op("act", lambda: A.activation(out=jq_t[:], in_=x_t[:], func=AF.Square, accum_out=ss_t[:]), reads=[x_b], writes=[jq_b, ss_b])
                    S.op("act", lambda: A.activation(out=rr_t[:], in_=ss_t[:], func=AF.Sqrt, scale=1.0 / D, bias=cst_t[:, 0:1]), reads=[ss_b, cst_b], writes=[rr_b])
                    S.op("dve", lambda: V.reciprocal(rr_t[:], rr_t[:]), reads=[rr_b], writes=[rr_b])
                    S.op("dve", lambda: V.scalar_tensor_tensor(out=x_t[:], in0=x_t[:], scalar=rr_t[:, 0:1], in1=gfin_t[:], op0=ALU.mult, op1=ALU.mult), reads=[x_b, rr_b, gfin_b], writes=[x_b])
                S.dma("sp", lambda: nc.sync.dma_start(out=xo_v[ti], in_=x_t[:]), reads=[x_b], writes=[xo_b])
            S.finish([xo_b])
            S.barrier()
            S.es = es
        print("C ninstr", S.ninstr)
    return nc

from concourse.bass_utils import run_bass_kernel_spmd
import os

B_, L_, NCORE = 2, 16384, 8
NTOK_ = 4096
CAP_ = 768
POOL_WINDOWS_ = (2, 4, 8, 16)
_prog_cache = {}


def _prog(key, fn):
    if key not in _prog_cache:
        _prog_cache[key] = fn()
    return _prog_cache[key]


def _c(a):
    return np.ascontiguousarray(a)


def _inv_freq():
    return np.power(np.float32(10000.0), -np.arange(0, 64, 2, dtype=np.float32) / np.float32(64)).astype(np.float32)


def _run(nc, in_maps):
    res = run_bass_kernel_spmd(nc, in_maps, core_ids=list(range(NCORE)))
    return res.results


def kernel(x, c, positions, w_ada, b_ada, norm1_g, w_in, pool_w, pool_scale,
           ssm_lam_re, ssm_lam_im, ssm_log_dt, ssm_b_re, ssm_b_im, ssm_c_re, ssm_c_im,
           ssm_d, ssm_w_glu, q_norm_g, kv_norm_g, w_uq, w_ukv, out_norm_g, w_out,
           norm2_g, router_w_group, router_b_group, router_w_expert, router_b_expert,
           w_gate, w_up, w_down, final_g):
    f32 = np.float32
    x = np.asarray(x, f32); c = np.asarray(c, f32); positions = np.asarray(positions, np.int32)
    depth = w_ada.shape[0]
    invf = _inv_freq()
    invf2 = np.concatenate([invf, invf]).reshape(64, 1).astype(f32)
    xs = [_c(x[k // 4, (k % 4) * NTOK_:(k % 4 + 1) * NTOK_, :]) for k in range(NCORE)]
    cTs = [_c(c[b].reshape(16, 128).T) for b in range(B_)]
    ncA = _prog("A", lambda: build_A(NTOK_))
    ncB = _prog("B", lambda: build_B(L_))
    for l in range(depth):
        wl = lambda a: np.asarray(a[l], f32)
        wada_l = wl(w_ada); bada_l = wl(b_ada)
        win_l = wl(w_in); wuq_l = wl(w_uq); wukv_l = wl(w_ukv)
        shared = {
            "wadaA": _c(wada_l[:, 0:4096]), "badaA": _c(bada_l[0:4096].reshape(1, -1)),
            "g1": wl(norm1_g).reshape(1, -1), "w_in": win_l,
            "w_in_sw": _c(np.concatenate([win_l[:, 1824:1856], win_l[:, 1792:1824]], axis=1)),
            "gq": _c(wl(q_norm_g).reshape(4, 128).T), "gkv": _c(wl(kv_norm_g).reshape(2, 128).T),
            "wq_n": _c(wuq_l[:, :, :128].reshape(512, 1024)),
            "wq_p": _c(wuq_l[:, :, 128:].reshape(512, 512)),
            "wq_ps": _c(np.concatenate([wuq_l[:, :, 160:192], wuq_l[:, :, 128:160]], axis=2).reshape(512, 512)),
            "wk": _c(wukv_l[:, :, :128].reshape(256, 1024)), "wv": _c(wukv_l[:, :, 128:].reshape(256, 1024)),
            "invf": invf2,
        }
        in_maps = []
        for k in range(NCORE):
            b, q = k // 4, k % 4
            m = dict(shared)
            m["x"] = xs[k]; m["cT"] = cTs[b]
            m["pos"] = _c(positions[b, q * NTOK_:(q + 1) * NTOK_].reshape(1, -1))
            in_maps.append(m)
        ra = _run(ncA, in_maps)
        cat = lambda name, b, ax: np.concatenate([ra[b * 4 + q][name] for q in range(4)], axis=ax)
        lam_re = wl(ssm_lam_re); lam_im = wl(ssm_lam_im); log_dt = wl(ssm_log_dt)
        b_re = wl(ssm_b_re); b_im = wl(ssm_b_im); c_re = wl(ssm_c_re); c_im = wl(ssm_c_im)
        dsk = wl(ssm_d); pw = wl(pool_w); psc = wl(pool_scale)
        in_maps = [None] * NCORE
        for b in range(B_):
            upT = cat("upT", b, 1); usT = cat("usT", b, 1); QT = cat("QT", b, 2); KT = cat("KT", b, 2)
            kpeT = cat("kpeT", b, 1); Vv = cat("V", b, 0).reshape(L_, 8, 128)
            for r in range(4):
                gs = slice(8 * r, 8 * r + 8)
                w = POOL_WINDOWS_[r]
                e = np.array([1.0 if (1 << kk) < w else 0.0 for kk in range(4)], f32)
                fix = np.array([1.0 / min(t + 1, w) for t in range(16)], f32)
                in_maps[b * 4 + r] = {
                    "QT": _c(QT[2 * r:2 * r + 2]), "KT": _c(KT[2 * r:2 * r + 2]), "kpeT": kpeT, "V": _c(Vv[:, 2 * r:2 * r + 2, :]),
                    "usT": _c(usT[128 * r:128 * (r + 1)]),
                    "lamre": _c(np.concatenate([lam_re[gs].T, lam_re[gs].T], 0)), "lamim": _c(np.concatenate([lam_im[gs].T, lam_im[gs].T], 0)),
                    "logdt": _c(np.broadcast_to(log_dt[gs][None, :], (128, 8))),
                    "bre": _c(b_re[gs].transpose(1, 0, 2).reshape(64, 128)), "bim": _c(b_im[gs].transpose(1, 0, 2).reshape(64, 128)),
                    "cA": _c(np.concatenate([c_re[gs], c_im[gs]], -1).reshape(128, 128)),
                    "cB": _c(np.concatenate([c_im[gs], c_re[gs]], -1).reshape(128, 128)),
                    "dsk": _c(dsk[128 * r:128 * (r + 1)].reshape(128, 1)),
                    "upT": _c(upT[128 * r:128 * (r + 1)]), "pw": _c(pw[r]), "psc": _c(psc[128 * r:128 * (r + 1)].reshape(128, 1)),
                    "pe": _c(np.broadcast_to(e, (128, 4))), "pinvw": np.full((128, 1), 1.0 / w, f32),
                    "pfix": _c(np.broadcast_to(fix, (128, 16))),
                }
        del ra
        rb = _run(ncB, in_maps)
        final = (l == depth - 1)
        ncC = _prog(("C", final), lambda: build_C(NTOK_, CAP_, final))
        sharedC = {
            "wadaC": _c(wada_l[:, 4096:]), "badaC": _c(bada_l[4096:].reshape(1, -1)),
            "wglu": wl(ssm_w_glu), "gn": _c(wl(out_norm_g).reshape(16, 128).T), "wout": wl(w_out),
            "g2": wl(norm2_g).reshape(1, -1),
            "wr": _c(np.concatenate([wl(router_w_group), wl(router_w_expert)], 1)),
            "br": _c(np.concatenate([wl(router_b_group), wl(router_b_expert)]).reshape(1, 36)),
            "wg": wl(w_gate), "wu": wl(w_up), "wd": wl(w_down), "gfin": np.asarray(final_g, f32).reshape(1, -1),
        }
        in_maps = []
        for k in range(NCORE):
            b, q = k // 4, k % 4
            ts = slice(q * NTOK_, (q + 1) * NTOK_)
            m = dict(sharedC)
            m["x"] = xs[k]; m["cT"] = cTs[b]
            m["ypT"] = _c(np.concatenate([rb[b * 4 + r]["ypT"][:, ts] for r in range(4)], 0))
            m["ysT"] = _c(np.concatenate([rb[b * 4 + r]["ysT"][:, ts] for r in range(4)], 0))
            m["ymT"] = _c(np.concatenate([rb[b * 4 + r]["ymT"][:, ts] for r in range(4)], 0))
            in_maps.append(m)
        del rb
        rc = _run(ncC, in_maps)
        xs = [np.asarray(rc[k]["xo"], f32) for k in range(NCORE)]
        if os.environ.get("KDEBUG"):
            cc = np.stack([np.asarray(rc[k]["cnt"]).sum(0) for k in range(NCORE)])
            print("KDEBUG layer", l, "expert counts per core: max", cc.max(), "min", cc.min(), "mean", cc.mean(), flush=True)
        del rc
    out = np.stack([np.concatenate(xs[b * 4:(b + 1) * 4], axis=0) for b in range(B_)], axis=0)
    return out.astype(f32)
```
